# Optimizing a Trainium2 kernel written in Bass

```python
import math
import jax
import jax.numpy as jnp
from jax import lax
import numpy as np

D_MODEL = 1024
BATCH = 8
SEQ = 4096
DEPTH = 2

GRID_W = 64
CTX_LEN = 256
EPS = 1e-6
F32 = jnp.float32

D_MIX = D_MODEL
N_MIXERS = 4
D_GROUP = D_MIX // N_MIXERS

HY_ORDER = 2
HY_HEADS = 4
HY_SHORT = 3
HY_EMB = 33
HY_BANDS = (HY_EMB - 1) // 2
HY_FFN = 64
HY_TARGET = 1e-2
HY_FAST_PCT = 0.3
HY_SLOW_PCT = 1.5

GM_CHUNK = 128
GM_HEADS = 4

CV_WIDTH = 31
CV_GROUPS = 4

MLA_HEADS = 4
MLA_NOPE = 64
MLA_ROPE = 32
MLA_V = 64
MLA_Q_RANK = 192
MLA_KV_RANK = 128
ROPE_BASE = 10000.0
ATTN_BLOCK = 128

N_EXPERTS = 16
N_EXPERT_GROUPS = 4
EXPERTS_PER_GROUP = N_EXPERTS // N_EXPERT_GROUPS
TOP_K = 2
GROUP_SCORE_TOPK = 2
D_EXPERT = 512

HY_COLS = (HY_ORDER + 1) * D_GROUP
GM_COLS = 2 * D_GROUP
CV_COLS = 2 * D_GROUP
MQ_COLS = MLA_Q_RANK
MKV_COLS = MLA_KV_RANK + MLA_ROPE
HY_OFF = 0
GM_OFF = HY_OFF + HY_COLS
CV_OFF = GM_OFF + GM_COLS
MQ_OFF = CV_OFF + CV_COLS
MKV_OFF = MQ_OFF + MQ_COLS
IN_COLS = MKV_OFF + MKV_COLS

kernel_name = "hybrid_hymba_flow_block"


def rms_norm(x, g):
    x32 = x.astype(F32)
    y = x32 * lax.rsqrt(jnp.mean(x32 * x32, axis=-1, keepdims=True) + EPS)
    return (y * g.astype(F32)).astype(x.dtype)


def layer_norm(x, g, b):
    x32 = x.astype(F32)
    mu = jnp.mean(x32, axis=-1, keepdims=True)
    var = jnp.mean(jnp.square(x32 - mu), axis=-1, keepdims=True)
    return ((x32 - mu) * lax.rsqrt(var + EPS) * g.astype(F32) + b.astype(F32)).astype(x.dtype)


def depthwise_conv(x, w, b):
    pad = (w.shape[0] - 1) // 2
    y = lax.conv_general_dilated(x, w[:, None, :].astype(x.dtype), window_strides=(1,),
                                 padding=[(pad, pad)], dimension_numbers=('NWC', 'WIO', 'NWC'),
                                 feature_group_count=x.shape[-1])
    return y + b.astype(x.dtype)


def modulation(cond, w, b):
    m = jax.nn.silu(cond) @ w + b
    return jnp.split(jnp.expand_dims(m, -2), 6, axis=-1)


def axial_rope_tables(n_lat):
    rows = n_lat // GRID_W
    row = jnp.repeat(jnp.arange(rows), GRID_W).astype(F32)
    col = jnp.tile(jnp.arange(GRID_W), rows).astype(F32)
    n_freq = MLA_ROPE // 4
    inv = ROPE_BASE ** (-jnp.arange(n_freq, dtype=F32) / n_freq)
    ang = jnp.concatenate([row[:, None] * inv, col[:, None] * inv], axis=-1)
    return jnp.cos(ang), jnp.sin(ang)


def apply_rope(x, cos, sin):
    x32 = x.astype(F32)
    x1, x2 = jnp.split(x32, 2, axis=-1)
    return jnp.concatenate([x1 * cos - x2 * sin, x1 * sin + x2 * cos], axis=-1).astype(x.dtype)


def hyena_filter_spectra(L, w1, b1, freq, w2, b2, w3):
    w1, b1, freq, w2, b2, w3 = (a.astype(F32) for a in (w1, b1, freq, w2, b2, w3))
    t = jnp.linspace(0.0, 1.0, L, dtype=F32)[:, None]
    w = 2.0 * math.pi * jnp.arange(L, dtype=F32)[:, None] / L
    f = jnp.linspace(1e-4, HY_BANDS - 1, HY_BANDS, dtype=F32)[None, :]
    z = jnp.concatenate([t, jnp.cos(f * w), -jnp.sin(f * w)], axis=-1)
    h = jnp.sin(freq * (z @ w1 + b1))
    h = jnp.sin(freq * (h @ w2 + b2))
    h = (h @ w3).reshape(L, HY_ORDER, 2, D_GROUP)
    deltas = jnp.abs(jnp.linspace(math.log(HY_TARGET) / HY_SLOW_PCT,
                                  math.log(HY_TARGET) / HY_FAST_PCT, D_GROUP, dtype=F32))
    h = h * jnp.exp(-t * deltas)[:, None, None, :]
    kf, kb = h[:, :, 0], h[:, :, 1]
    k2 = jnp.concatenate([kf, jnp.zeros_like(kf[:1]), kb[1:][::-1]], axis=0)
    k2 = k2 / jnp.sum(jnp.abs(k2), axis=0, keepdims=True)
    return jnp.fft.rfft(k2, n=2 * L, axis=0)


def long_conv(u, k_spec, bias):
    L = u.shape[1]
    u32 = u.astype(F32)
    y = jnp.fft.irfft(jnp.fft.rfft(u32, n=2 * L, axis=1) * k_spec[None], n=2 * L, axis=1)[:, :L]
    return (y + u32 * bias.astype(F32)).astype(u.dtype)


def hyena_mixer(p_hy, short_w, short_b, spectra, hy_bias):
    z = depthwise_conv(p_hy, short_w, short_b)
    x1, x2, v = jnp.split(z, HY_ORDER + 1, axis=-1)
    y = v
    for n, gate in enumerate((x1, x2)):
        y = gate * long_conv(y, spectra[:, n], hy_bias[n])
    return y


def gmlp_mixer(p_gm, ln_g, ln_b, ws, bs):
    B, L, _ = p_gm.shape
    z = jax.nn.gelu(p_gm)
    u, v = jnp.split(z, 2, axis=-1)
    v = layer_norm(v, ln_g, ln_b).reshape(B, L // GM_CHUNK, GM_CHUNK, GM_HEADS, D_GROUP // GM_HEADS)
    s = jnp.einsum('gij,bcjgd->bcigd', ws.astype(v.dtype), v) + bs.T.astype(v.dtype)[None, None, :, :, None]
    return u * s.reshape(B, L, D_GROUP)


def conv_mixer(p_cv, dw_w, dw_b, ln_g, ln_b):
    a, b = jnp.split(p_cv, 2, axis=-1)
    g = depthwise_conv(a * jax.nn.sigmoid(b), dw_w, dw_b)
    B, L, _ = g.shape
    g = layer_norm(g.reshape(B, L, CV_GROUPS, D_GROUP // CV_GROUPS),
                   ln_g.reshape(CV_GROUPS, -1), ln_b.reshape(CV_GROUPS, -1)).reshape(B, L, D_GROUP)
    return jax.nn.silu(g)


def mla_queries(cq, qa_norm, w_uq, rope):
    B, L, _ = cq.shape
    q = (rms_norm(cq, qa_norm) @ w_uq).reshape(B, L, MLA_HEADS, MLA_NOPE + MLA_ROPE)
    if rope is not None:
        cos, sin = rope
        q = jnp.concatenate([q[..., :MLA_NOPE],
                             apply_rope(q[..., MLA_NOPE:], cos[None, :, None], sin[None, :, None])], axis=-1)
    return q


def mla_keys_values(ckv, kva_norm, w_ukv, rope):
    B, L, _ = ckv.shape
    c_kv, k_rope = ckv[..., :MLA_KV_RANK], ckv[..., MLA_KV_RANK:]
    kv = (rms_norm(c_kv, kva_norm) @ w_ukv).reshape(B, L, MLA_HEADS, MLA_NOPE + MLA_V)
    k_nope, v = kv[..., :MLA_NOPE], kv[..., MLA_NOPE:]
    if rope is not None:
        cos, sin = rope
        k_rope = apply_rope(k_rope, cos[None], sin[None])
    k = jnp.concatenate([k_nope, jnp.broadcast_to(k_rope[:, :, None, :], (B, L, MLA_HEADS, MLA_ROPE))], axis=-1)
    return k, v


def block_attention(q, k, v):
    B, Lq, H, Dk = q.shape
    nb = Lq // ATTN_BLOCK
    qb = q.reshape(B, nb, ATTN_BLOCK, H, Dk).transpose(1, 0, 2, 3, 4)
    scale = 1.0 / math.sqrt(Dk)

    def attend(qblk):
        s = jnp.einsum('bqhd,bkhd->bhqk', qblk, k).astype(F32) * scale
        p = jax.nn.softmax(s, axis=-1).astype(v.dtype)
        return jnp.einsum('bhqk,bkhd->bqhd', p, v)

    o = lax.map(attend, qb)
    return o.transpose(1, 0, 2, 3, 4).reshape(B, Lq, H * v.shape[-1])


def mix_tokens(proj, k, v, rope, spectra, lp):
    B, L, _ = proj.shape
    y_hy = hyena_mixer(proj[..., HY_OFF:GM_OFF], lp['hy_short_w'], lp['hy_short_b'], spectra, lp['hy_bias'])
    y_gm = gmlp_mixer(proj[..., GM_OFF:CV_OFF], lp['gm_ln_g'], lp['gm_ln_b'], lp['gm_ws'], lp['gm_bs'])
    y_cv = conv_mixer(proj[..., CV_OFF:MQ_OFF], lp['cv_dw_w'], lp['cv_dw_b'], lp['cv_ln_g'], lp['cv_ln_b'])
    q = mla_queries(proj[..., MQ_OFF:MKV_OFF], lp['mla_qa_norm'], lp['w_uq'], rope)
    y_at = block_attention(q, k, v)
    y = jnp.stack([y_hy, y_gm, y_cv, y_at], axis=-2)
    y = rms_norm(y, lp['mix_norm_g'].reshape(N_MIXERS, D_GROUP)).reshape(B, L, D_MIX)
    return y @ lp['w_out']


def moe_ffn(h, w_router, router_bias, w_gate, w_up, w_down):
    s = jax.nn.sigmoid((h @ w_router).astype(F32))
    sel = s + router_bias.astype(F32)
    grp = sel.reshape(*sel.shape[:-1], N_EXPERT_GROUPS, EXPERTS_PER_GROUP)
    grp_score = jnp.sum(lax.top_k(grp, GROUP_SCORE_TOPK)[0], axis=-1)
    best = jnp.argmax(grp_score, axis=-1)
    in_group = (jnp.arange(N_EXPERTS) // EXPERTS_PER_GROUP) == best[..., None]
    _, idx = lax.top_k(jnp.where(in_group, sel, -jnp.inf), TOP_K)
    w = jnp.take_along_axis(s, idx, axis=-1)
    w = w / jnp.sum(w, axis=-1, keepdims=True)
    gates = jnp.sum(jax.nn.one_hot(idx, N_EXPERTS, dtype=F32) * w[..., None], axis=-2).astype(h.dtype)
    y = jnp.zeros_like(h)
    for e in range(N_EXPERTS):
        a = jax.nn.silu(h @ w_gate[e]) * (h @ w_up[e])
        y = y + gates[..., e:e + 1] * (a @ w_down[e])
    return y


def setup_inputs(seed: int = 0) -> dict:
    key = jax.random.key(seed)
    ks = iter(jax.random.split(key, 48))

    def nrm(shape, scale):
        return jax.random.normal(next(ks), shape, F32) * scale

    def gain(shape):
        return 1.0 + nrm(shape, 0.05)

    D, E, F = D_MODEL, N_EXPERTS, D_EXPERT
    return {
        "x": nrm((BATCH, SEQ, D), 1.0),
        "c": nrm((BATCH, D), 1.0),
        "ctx": nrm((BATCH, CTX_LEN, D), 1.0),
        "c_ctx": nrm((D,), 1.0),
        "ada_w": nrm((DEPTH, D, 6 * D), 0.5 * D ** -0.5),
        "ada_b": nrm((DEPTH, 6 * D), 0.02),
        "norm1_g": gain((DEPTH, D)),
        "norm2_g": gain((DEPTH, D)),
        "w_in": nrm((DEPTH, D, IN_COLS), D ** -0.5),
        "hy_short_w": nrm((DEPTH, HY_SHORT, HY_COLS), HY_SHORT ** -0.5),
        "hy_short_b": nrm((DEPTH, HY_COLS), 0.02),
        "hy_f_w1": nrm((DEPTH, HY_EMB, HY_FFN), HY_EMB ** -0.5),
        "hy_f_b1": nrm((DEPTH, HY_FFN), 0.02),
        "hy_f_freq": 1.0 + nrm((DEPTH, HY_FFN), 0.1),
        "hy_f_w2": nrm((DEPTH, HY_FFN, HY_FFN), HY_FFN ** -0.5),
        "hy_f_b2": nrm((DEPTH, HY_FFN), 0.02),
        "hy_f_w3": nrm((DEPTH, HY_FFN, HY_ORDER * 2 * D_GROUP), HY_FFN ** -0.5),
        "hy_bias": nrm((DEPTH, HY_ORDER, D_GROUP), 0.5),
        "gm_ln_g": gain((DEPTH, D_GROUP)),
        "gm_ln_b": nrm((DEPTH, D_GROUP), 0.02),
        "gm_ws": nrm((DEPTH, GM_HEADS, GM_CHUNK, GM_CHUNK), 0.5 * GM_CHUNK ** -0.5),
        "gm_bs": 1.0 + nrm((DEPTH, GM_HEADS, GM_CHUNK), 0.02),
        "cv_dw_w": nrm((DEPTH, CV_WIDTH, D_GROUP), CV_WIDTH ** -0.5),
        "cv_dw_b": nrm((DEPTH, D_GROUP), 0.02),
        "cv_ln_g": gain((DEPTH, D_GROUP)),
        "cv_ln_b": nrm((DEPTH, D_GROUP), 0.02),
        "mla_qa_norm": gain((DEPTH, MLA_Q_RANK)),
        "w_uq": nrm((DEPTH, MLA_Q_RANK, MLA_HEADS * (MLA_NOPE + MLA_ROPE)), MLA_Q_RANK ** -0.5),
        "mla_kva_norm": gain((DEPTH, MLA_KV_RANK)),
        "w_ukv": nrm((DEPTH, MLA_KV_RANK, MLA_HEADS * (MLA_NOPE + MLA_V)), MLA_KV_RANK ** -0.5),
        "mix_norm_g": gain((DEPTH, D_MIX)),
        "w_out": nrm((DEPTH, D_MIX, D), D_MIX ** -0.5),
        "w_router": nrm((D, E), D ** -0.5),
        "router_bias": nrm((E,), 0.01),
        "exp_w_gate": nrm((DEPTH, E, D, F), D ** -0.5),
        "exp_w_up": nrm((DEPTH, E, D, F), D ** -0.5),
        "exp_w_down": nrm((DEPTH, E, F, D), F ** -0.5),
        "final_norm_g": gain((D,)),
    }


def reference(x, c, ctx, c_ctx, ada_w, ada_b, norm1_g, norm2_g, w_in, hy_short_w, hy_short_b,
              hy_f_w1, hy_f_b1, hy_f_freq, hy_f_w2, hy_f_b2, hy_f_w3, hy_bias, gm_ln_g, gm_ln_b,
              gm_ws, gm_bs, cv_dw_w, cv_dw_b, cv_ln_g, cv_ln_b, mla_qa_norm, w_uq, mla_kva_norm,
              w_ukv, mix_norm_g, w_out, w_router, router_bias, exp_w_gate, exp_w_up, exp_w_down,
              final_norm_g):
    n_lat = x.shape[1]
    n_ctx = ctx.shape[1]
    rope = axial_rope_tables(n_lat)
    for l in range(DEPTH):
        last = l == DEPTH - 1
        lp = {
            'hy_short_w': hy_short_w[l], 'hy_short_b': hy_short_b[l], 'hy_bias': hy_bias[l],
            'gm_ln_g': gm_ln_g[l], 'gm_ln_b': gm_ln_b[l], 'gm_ws': gm_ws[l], 'gm_bs': gm_bs[l],
            'cv_dw_w': cv_dw_w[l], 'cv_dw_b': cv_dw_b[l], 'cv_ln_g': cv_ln_g[l], 'cv_ln_b': cv_ln_b[l],
            'mla_qa_norm': mla_qa_norm[l], 'w_uq': w_uq[l],
            'mix_norm_g': mix_norm_g[l], 'w_out': w_out[l],
        }
        filt = (hy_f_w1[l], hy_f_b1[l], hy_f_freq[l], hy_f_w2[l], hy_f_b2[l], hy_f_w3[l])
        sh1, sc1, g1, sh2, sc2, g2 = modulation(c, ada_w[l], ada_b[l])
        csh1, csc1, cg1, csh2, csc2, cg2 = modulation(c_ctx, ada_w[l], ada_b[l])

        h_lat = rms_norm(x, norm1_g[l]) * (1.0 + sc1) + sh1
        h_ctx = rms_norm(ctx, norm1_g[l]) * (1.0 + csc1) + csh1
        proj_lat = h_lat @ w_in[l]
        proj_ctx = h_ctx @ (w_in[l][:, MKV_OFF:] if last else w_in[l])
        k_ctx, v_ctx = mla_keys_values(proj_ctx[..., -MKV_COLS:], mla_kva_norm[l], w_ukv[l], None)
        k_lat, v_lat = mla_keys_values(proj_lat[..., MKV_OFF:], mla_kva_norm[l], w_ukv[l], rope)
        k_all = jnp.concatenate([k_lat, k_ctx], axis=1)
        v_all = jnp.concatenate([v_lat, v_ctx], axis=1)
        spectra_lat = hyena_filter_spectra(n_lat, *filt)
        x_mid = x + g1 * mix_tokens(proj_lat, k_all, v_all, rope, spectra_lat, lp)

        hf = rms_norm(x_mid, norm2_g[l]) * (1.0 + sc2) + sh2
        x = x_mid + g2 * moe_ffn(hf, w_router, router_bias, exp_w_gate[l], exp_w_up[l], exp_w_down[l])

        if not last:
            spectra_ctx = hyena_filter_spectra(n_ctx, *filt)
            ctx_mid = ctx + cg1 * mix_tokens(proj_ctx, k_ctx, v_ctx, None, spectra_ctx, lp)
            hc = rms_norm(ctx_mid, norm2_g[l]) * (1.0 + csc2) + csh2
            ctx = ctx_mid + cg2 * moe_ffn(hc, w_router, router_bias, exp_w_gate[l], exp_w_up[l], exp_w_down[l])
    return rms_norm(x, final_norm_g)
```

```python
import os
import numpy as np
import ml_dtypes
from contextlib import ExitStack
import concourse.bass as bass
import concourse.mybir as mybir
from concourse.bass_utils import run_bass_kernel_spmd

F32 = mybir.dt.float32
BF16 = mybir.dt.bfloat16
I32 = mybir.dt.int32
AF = mybir.ActivationFunctionType
ALU = mybir.AluOpType
AX = mybir.AxisListType

D = 1024
L = 4096
LC = 256
NTOK = L + LC
DEPTH = 2
EPS = 1e-6
HY_OFF, GM_OFF, CV_OFF, MQ_OFF, MKV_OFF, KR_OFF, IN_COLS = 0, 768, 1280, 1792, 1984, 2112, 2144
NE = 16
FE = 512
PI = float(np.pi)


class Buf:
    __slots__ = ("name", "w", "r")

    def __init__(self, name):
        self.name = name
        self.w = None
        self.r = []


class Eng:
    def __init__(self, name, eng, sem):
        self.name = name
        self.eng = eng
        self.sem = sem
        self.count = 0
        self.waited = {}


class Prog:
    def __init__(self, nc, stack, n_dma_sems=32):
        self.nc = nc
        self.engs = {}
        for name, e in (("pe", nc.tensor), ("act", nc.scalar), ("dve", nc.vector),
                        ("pool", nc.gpsimd), ("sp", nc.sync)):
            sem = stack.enter_context(nc.semaphore("s_" + name))
            self.engs[name] = Eng(name, e, sem)
        self.dma_sems = []
        for i in range(n_dma_sems):
            sem = stack.enter_context(nc.semaphore("s_dma%d" % i))
            self.dma_sems.append([sem, 0, "dma%d" % i])
        self.dma_rr = 0
        self.nbuf = 0

    def buf_id(self):
        self.nbuf += 1
        return self.nbuf

    def buf(self, name=None):
        self.nbuf += 1
        return Buf(name or "b%d" % self.nbuf)

    def bufs(self, n, name="b"):
        return [self.buf("%s%d" % (name, i)) for i in range(n)]

    def _wait(self, E, tok):
        if tok is None:
            return
        key, sem, val = tok
        if key == E.name and (E.name == "pe" or val > E.count):
            return
        if E.waited.get(key, 0) >= val:
            return
        E.eng.wait_ge(sem, val)
        E.waited[key] = val

    def _deps(self, E, reads, writes):
        for b in reads:
            self._wait(E, b.w)
        for b in writes:
            self._wait(E, b.w)
            for t in b.r:
                self._wait(E, t)

    def _commit(self, tok, reads, writes):
        for b in reads:
            b.r.append(tok)
            if len(b.r) > 16:
                latest = {}
                for t in b.r:
                    if t[0] not in latest or latest[t[0]][2] < t[2]:
                        latest[t[0]] = t
                b.r = list(latest.values())
        for b in writes:
            b.w = tok
            b.r = []

    def op(self, en, fn, reads=(), writes=(), inc=True):
        E = self.engs[en]
        self._deps(E, reads, writes)
        ins = fn(E.eng)
        if inc:
            E.count += 1
            ins.then_inc(E.sem, 1)
            tok = (E.name, E.sem, E.count)
        else:
            tok = (E.name, E.sem, E.count + 1)
        self._commit(tok, reads, writes)
        return tok

    def dma(self, en, out, in_, reads=(), writes=(), **kw):
        E = self.engs[en]
        slot = self.dma_sems[self.dma_rr]
        self.dma_rr = (self.dma_rr + 1) % len(self.dma_sems)
        sem, cnt, key = slot
        if cnt > 0:
            self._wait(E, (key, sem, cnt))
        self._deps(E, reads, writes)
        ins = E.eng.dma_start(out=out, in_=in_, **kw)
        cnt += 16
        slot[1] = cnt
        ins.then_inc(sem, 16)
        tok = (key, sem, cnt)
        self._commit(tok, reads, writes)
        return tok

    def idma(self, out, in_, out_off, in_off, bounds, reads=(), writes=(), breg=None):
        E = self.engs["pool"]
        slot = self.dma_sems[self.dma_rr]
        self.dma_rr = (self.dma_rr + 1) % len(self.dma_sems)
        sem, cnt, key = slot
        if cnt > 0:
            self._wait(E, (key, sem, cnt))
        self._deps(E, reads, writes)
        if breg is not None:
            ins = E.eng.indirect_dma_start(out=out, out_offset=out_off, in_=in_, in_offset=in_off, bounds_check=breg, oob_is_err=False)
        else:
            ins = E.eng.indirect_dma_start(out=out, out_offset=out_off, in_=in_, in_offset=in_off)
        cnt += 16
        slot[1] = cnt
        ins.then_inc(sem, 16)
        tok = (key, sem, cnt)
        self._commit(tok, reads, writes)
        return tok

    def barrier(self):
        toks = [(E.name, E.sem, E.count) for E in self.engs.values() if E.count > 0]
        toks += [(k, s, c) for s, c, k in self.dma_sems if c > 0]
        for E in self.engs.values():
            for t in toks:
                self._wait(E, t)


_CONST = {}


def _bf(a):
    return np.ascontiguousarray(a.astype(ml_dtypes.bfloat16))


def _dft_slabs(Lh):
    N = 2 * Lh
    NT = Lh // 128
    t = np.arange(Lh, dtype=np.int64)
    F = np.empty((2 * Lh, Lh), np.float32)
    for r0 in range(0, Lh, 256):
        r = np.arange(r0, r0 + 256, dtype=np.int64)[:, None]
        ang = ((r * t[None, :]) % N).astype(np.float64) * (2 * np.pi / N)
        F[r0:r0 + 256] = np.cos(ang)
        F[Lh + r0:Lh + r0 + 256] = -np.sin(ang)
    F[Lh] = np.where(t % 2 == 0, 1.0, -1.0)
    Fb = F.astype(ml_dtypes.bfloat16)
    del F
    NS = max(NT // 2, 1)
    fwd = np.empty((NS, 128, NT, 512), ml_dtypes.bfloat16)
    for s in range(NS):
        rcs = [2 * s, 2 * s + 1, NT + 2 * s, NT + 2 * s + 1]
        for j, rc in enumerate(rcs):
            blk = Fb[rc * 128:(rc + 1) * 128, :]
            fwd[s, :, :, j * 128:(j + 1) * 128] = blk.T.reshape(NT, 128, 128).transpose(1, 0, 2)
    inv = np.empty((NS, 128, 2 * NT, 256), ml_dtypes.bfloat16)
    for s in range(NS):
        blk = Fb[:, s * 256:(s + 1) * 256]
        inv[s] = blk.reshape(2 * NT, 128, 256).transpose(1, 0, 2)
    rs = np.full((2 * Lh,), 2.0 / N, np.float32)
    rs[0] = 1.0 / N
    rs[Lh] = 1.0 / N
    rowscale = np.ascontiguousarray(rs.reshape(2 * NT, 128).T)
    return fwd, inv, rowscale


def _hy_tables(Lh):
    t = np.linspace(0.0, 1.0, Lh, dtype=np.float32)[:, None]
    w = (2.0 * np.pi * np.arange(Lh, dtype=np.float32)[:, None] / Lh).astype(np.float32)
    f = np.linspace(1e-4, 15, 16, dtype=np.float32)[None, :]
    z = np.concatenate([t, np.cos(f * w), -np.sin(f * w)], axis=-1).astype(np.float32)
    deltas = np.abs(np.linspace(np.log(1e-2) / 1.5, np.log(1e-2) / 0.3, 256, dtype=np.float32))
    decay = np.exp(-t * deltas[None, :]).astype(np.float32)
    NT = Lh // 128
    zT = np.ascontiguousarray(z.T)
    dec = np.ascontiguousarray(decay.reshape(NT, 128, 256).transpose(1, 0, 2))
    return zT, dec


def _consts():
    if _CONST:
        return _CONST
    c = {}
    c["ident"] = np.eye(128, dtype=np.float32)
    c["onesf"] = np.ones((128, 128), np.float32)
    bo = np.zeros((128, 128), np.float32)
    bo[:64, :64] = 1.0 / 64
    bo[64:, 64:] = 1.0 / 64
    c["blk64"] = bo
    row = np.repeat(np.arange(L // 64), 64).astype(np.float32)
    col = np.tile(np.arange(64), L // 64).astype(np.float32)
    inv = (10000.0 ** (-np.arange(8, dtype=np.float32) / 8)).astype(np.float32)
    ang = np.concatenate([row[:, None] * inv, col[:, None] * inv], axis=-1).astype(np.float32)
    c["ropec"] = np.ascontiguousarray(np.cos(ang).T.astype(np.float32))
    c["ropes"] = np.ascontiguousarray(np.sin(ang).T.astype(np.float32))
    sel = np.zeros((16, 16, 128), np.float32)
    for e in range(16):
        sel[e, e, :] = 1.0
    c["sel"] = sel
    c["tri"] = np.triu(np.ones((128, 128), np.float32), k=1)
    c["k9"] = np.ascontiguousarray(np.broadcast_to((512.0 * np.arange(9, dtype=np.float32))[None, None, :], (128, 16, 9)))
    c["k32"] = np.ascontiguousarray(np.broadcast_to((512.0 * np.arange(32, dtype=np.float32))[None, :, None], (128, 32, 16)))
    c["pidx"] = np.arange(128, dtype=np.float32).reshape(128, 1)
    c["tokidx"] = np.ascontiguousarray((np.arange(34, dtype=np.int32)[None, :] * 128 + np.arange(128, dtype=np.int32)[:, None]).astype(np.int32))
    c["tblfill"] = np.zeros((32 * 512, 1), np.int32)
    for Lh, tag in ((L, "L"), (LC, "C")):
        fwd, invs, rsc = _dft_slabs(Lh)
        c["dftf" + tag] = fwd
        c["dfti" + tag] = invs
        c["rsc" + tag] = rsc
        zT, dec = _hy_tables(Lh)
        c["hyz" + tag] = zT
        c["hydec" + tag] = dec
    _CONST.update(c)
    return _CONST


def _cols(v):
    v = np.asarray(v, np.float32)
    return np.ascontiguousarray(v.reshape(-1, 128).T)


def _rep(v):
    v = np.asarray(v, np.float32).reshape(1, -1)
    return np.ascontiguousarray(np.broadcast_to(v, (128, v.shape[1])))


def prep_shared(inp):
    s = {}
    s.update(_consts())
    for k in ("ada_w", "w_in", "w_out", "exp_w_gate", "exp_w_up", "exp_w_down", "hy_f_w1", "hy_f_w2", "hy_f_w3"):
        s[k] = np.ascontiguousarray(inp[k], dtype=np.float32)
    s["w_router"] = np.ascontiguousarray(inp["w_router"], dtype=np.float32)
    s["ada_bT"] = np.stack([_cols(inp["ada_b"][l]) for l in range(DEPTH)])
    s["n1gT"] = np.stack([_cols(inp["norm1_g"][l]) for l in range(DEPTH)])
    s["n2gT"] = np.stack([_cols(inp["norm2_g"][l]) for l in range(DEPTH)])
    s["hsw_bc"] = np.stack([np.stack([_rep(inp["hy_short_w"][l, k]) for k in range(3)], axis=1) for l in range(DEPTH)])
    s["hsb_bc"] = np.stack([_rep(inp["hy_short_b"][l]) for l in range(DEPTH)])
    s["hyb_bc"] = np.stack([_rep(inp["hy_bias"][l].reshape(-1)) for l in range(DEPTH)])
    s["hy_b1c"] = np.ascontiguousarray(np.asarray(inp["hy_f_b1"], np.float32)[:, :, None])
    s["hy_b2c"] = np.ascontiguousarray(np.asarray(inp["hy_f_b2"], np.float32)[:, :, None])
    s["hy_frc"] = np.ascontiguousarray(np.asarray(inp["hy_f_freq"], np.float32)[:, :, None])
    s["gm_lng"] = np.stack([_rep(inp["gm_ln_g"][l]) for l in range(DEPTH)])
    s["gm_lnb"] = np.stack([_rep(inp["gm_ln_b"][l]) for l in range(DEPTH)])
    s["gm_wsT"] = np.ascontiguousarray(np.asarray(inp["gm_ws"], np.float32).transpose(0, 3, 1, 2))
    gb = np.asarray(inp["gm_bs"], np.float32)
    s["gm_bsbc"] = np.ascontiguousarray(np.repeat(gb.transpose(0, 2, 1), 64, axis=2))
    s["cv_w"] = np.ascontiguousarray(np.asarray(inp["cv_dw_w"], np.float32).transpose(0, 2, 1).reshape(DEPTH, 2, 128, 31).transpose(0, 2, 1, 3))
    s["cv_bT"] = np.stack([_cols(inp["cv_dw_b"][l]) for l in range(DEPTH)])
    s["cv_lgT"] = np.stack([_cols(inp["cv_ln_g"][l]) for l in range(DEPTH)])
    s["cv_lbT"] = np.stack([_cols(inp["cv_ln_b"][l]) for l in range(DEPTH)])
    wuq = np.asarray(inp["w_uq"], np.float32).reshape(DEPTH, 192, 4, 96)
    wq_n = np.zeros((DEPTH, 192, 4, 128), np.float32)
    wq_n[:, :, :, 64:128] = wuq[:, :, :, 0:64]
    wq_r = np.zeros((DEPTH, 192, 4, 2, 16), np.float32)
    wq_r[:, :, :, 0, :] = wuq[:, :, :, 64:80]
    wq_r[:, :, :, 1, :] = wuq[:, :, :, 80:96]
    s["wq_n"] = wq_n.reshape(DEPTH, 192, 512)
    s["wq_r"] = wq_r.reshape(DEPTH, 192, 128)
    s["qan"] = np.ascontiguousarray(np.asarray(inp["mla_qa_norm"], np.float32)[:, :, None])
    wukv = np.asarray(inp["w_ukv"], np.float32).reshape(DEPTH, 128, 4, 128)
    wk = np.zeros((DEPTH, 128, 4, 128), np.float32)
    wk[:, :, :, 64:128] = wukv[:, :, :, 0:64]
    s["wk_n"] = wk.reshape(DEPTH, 128, 512)
    s["wv"] = np.ascontiguousarray(wukv[:, :, :, 64:128].reshape(DEPTH, 128, 256))
    s["kvn"] = np.ascontiguousarray(np.asarray(inp["mla_kva_norm"], np.float32)[:, :, None])
    s["mixg_bc"] = np.stack([_rep(inp["mix_norm_g"][l]) for l in range(DEPTH)])
    s["rb_bc"] = _rep(inp["router_bias"])
    s["fng_bc"] = _rep(inp["final_norm_g"])
    return s


def build_program(shared_shapes, debug=()):
    nc = bass.Bass("TRN2", target_bir_lowering=False)
    dbg = set(debug)
    class _Lazy(dict):
        def __missing__(self, k):
            shape, dt = shared_shapes[k]
            v = nc.dram_tensor(k, list(shape), dt, kind="ExternalInput").ap()
            self[k] = v
            return v
    I = _Lazy()
    x_in = nc.dram_tensor("x", [L, D], F32, kind="ExternalInput").ap()
    ctx_in = nc.dram_tensor("ctx", [LC, D], F32, kind="ExternalInput").ap()
    ccols = nc.dram_tensor("ccols", [128, 8, 2], F32, kind="ExternalInput").ap()
    out = nc.dram_tensor("out", [L, D], F32, kind="ExternalOutput").ap()

    def scratch(name, shape, dt):
        kind = "ExternalOutput" if name in dbg else "Internal"
        return nc.dram_tensor(name, list(shape), dt, kind=kind).ap()

    xm = scratch("xm", [NTOK, D], F32)
    xs1 = scratch("xs1", [NTOK, D], F32)
    zs = scratch("zs", [NTOK, 768], F32)
    ymix = scratch("ymix", [NTOK, D], F32)
    glu = scratch("glu", [256, NTOK], BF16)
    qT = scratch("qT", [4, 128, NTOK], BF16)
    kT = scratch("kT", [4, 128, NTOK], BF16)
    vv = scratch("vv", [NTOK, 4 * 65], BF16)
    kspec = scratch("kspec", [2 * L, 512], F32)
    kspecC = scratch("kspecC", [2 * LC, 512], F32)
    dbg_mod = scratch("dbg_mod", [128, 48, 2], F32)
    hfD = scratch("hfD", [NTOK, D], BF16)
    ypair = scratch("ypair", [32 * 512, D], F32)
    tbl = scratch("tbl", [32 * 512, 1], I32)
    dbg_sort = scratch("dbg_sort", [128, 184], F32)
    dbg_hT = scratch("dbg_hT", [128, 8, L + 2], BF16)

    with ExitStack() as top:
        P = Prog(nc, top)
        sbt = lambda st, name, shape, dt: st.enter_context(nc.sbuf_tensor("sb_%s_%d" % (name, P.buf_id()), shape, dt))
        psb = [top.enter_context(nc.psum_tensor("ps%d" % i, [128, 512], F32)) for i in range(8)]
        pB = P.bufs(8, "ps")
        ident = sbt(top, "ident", [128, 128], F32)
        identb = sbt(top, "identb", [128, 128], BF16)
        onesf = sbt(top, "onesf", [128, 128], F32)
        blk64 = sbt(top, "blk64", [128, 128], F32)
        modT = sbt(top, "modT", [128, 48, 2], F32)
        gain1 = sbt(top, "gain1", [128, 8, 2], F32)
        gain2 = sbt(top, "gain2", [128, 8, 2], F32)
        gbc = sbt(top, "gbc", [128, 4, 1024], F32)
        epsT = sbt(top, "epsT", [128, 1], F32)
        junk2 = sbt(top, "junk2", [128, 1024], BF16)
        bJ2 = P.buf("junk2")
        bC = P.buf("consts")
        bOut = P.buf("out_dram")
        bXs1 = P.buf("xs1_dram")
        P.op("pool", lambda e: e.memset(epsT[:], EPS), writes=[bC])
        bMod = P.buf("mod")
        bGbc = P.buf("gbc")
        P.dma("sp", ident[:], I["ident"], writes=[bC])
        P.dma("sp", onesf[:], I["onesf"], writes=[bC])
        P.dma("sp", blk64[:], I["blk64"], writes=[bC])
        P.op("dve", lambda e: e.tensor_copy(out=identb[:], in_=ident[:]), reads=[bC], writes=[bC])

        def xin_ap(l, t0, n):
            if l == 0:
                if t0 < L:
                    return x_in[t0:t0 + n, :]
                return ctx_in[t0 - L:t0 - L + n, :]
            return xs1[t0:t0 + n, :]

        BLOCKS = [(b * 512, 4, 0) for b in range(8)] + [(L, 2, 1)]

        for l in range(DEPTH):
            last = (l == DEPTH - 1)
            with ExitStack() as st:
                aw = [sbt(st, "aw%d" % i, [128, 6144], F32) for i in range(2)]
                bAw = P.bufs(2, "aw")
                scs = sbt(st, "scs", [128, 8, 2], F32)
                abT = sbt(st, "abT", [128, 48], F32)
                n1g = sbt(st, "n1g", [128, 8], F32)
                n2g = sbt(st, "n2g", [128, 8], F32)
                tmp = sbt(st, "tmpA", [128, 8, 2], F32)
                diag = [sbt(st, "diag%d" % i, [128, 128], F32) for i in range(2)]
                bDiag = P.bufs(2, "diag")
                bS = P.buf("scs")
                P.dma("sp", scs[:], ccols, writes=[bS])
                P.dma("sp", abT[:], I["ada_bT"][l], writes=[bS])
                P.dma("sp", n1g[:], I["n1gT"][l], writes=[bS])
                P.dma("sp", n2g[:], I["n2gT"][l], writes=[bS])
                P.op("act", lambda e: e.activation(out=scs[:], in_=scs[:], func=AF.Silu), reads=[bS], writes=[bS])
                for kc in range(8):
                    sl = kc % 2
                    P.dma("sp" if kc % 2 == 0 else "pool", aw[sl][:], I["ada_w"][l, kc * 128:(kc + 1) * 128, :], writes=[bAw[sl]])
                    for n in range(48):
                        P.op("pe", lambda e: e.matmul(psb[0][:, 2 * n:2 * n + 2], lhsT=aw[sl][:, n * 128:(n + 1) * 128],
                                                      rhs=scs[:, kc, :], start=(kc == 0 and n == 0), stop=(kc == 7)),
                             reads=[bAw[sl], bS], writes=[pB[0]], inc=(n == 47))
                P.op("dve", lambda e: e.tensor_tensor(out=modT[:], in0=psb[0][:, 0:96].rearrange("p (a b) -> p a b", b=2),
                                                       in1=abT[:].unsqueeze(2).to_broadcast([128, 48, 2]), op=ALU.add),
                     reads=[pB[0], bS], writes=[bMod])
                for (gain, ng, off) in ((gain1, n1g, 8), (gain2, n2g, 32)):
                    P.op("dve", lambda e: e.tensor_scalar(out=tmp[:], in0=modT[:, off:off + 8, :], scalar1=1.0, scalar2=None, op0=ALU.add),
                         reads=[bMod], writes=[bS])
                    P.op("dve", lambda e: e.tensor_tensor(out=gain[:], in0=tmp[:], in1=ng[:].unsqueeze(2).to_broadcast([128, 8, 2]), op=ALU.mult),
                         reads=[bS], writes=[bMod])
                k = 0
                for gi, (off, w) in enumerate(((16, 0), (16, 1), (40, 0), (40, 1))):
                    for half in range(2):
                        bank = 1 + (k % 2)
                        for j in range(4):
                            dc = half * 4 + j
                            dsl = (k * 4 + j) % 2
                            P.op("dve", lambda e: e.tensor_scalar(out=diag[dsl][:], in0=ident[:], scalar1=modT[:, off + dc, w:w + 1], scalar2=None, op0=ALU.mult),
                                 reads=[bMod, bC], writes=[bDiag[dsl]])
                            P.op("pe", lambda e: e.matmul(psb[bank][:, j * 128:(j + 1) * 128], lhsT=onesf[:], rhs=diag[dsl][:], start=(j == 0), stop=True),
                                 reads=[bDiag[dsl], bC], writes=[pB[bank]], inc=True)
                        P.op("act", lambda e: e.activation(out=gbc[:, gi, half * 512:(half + 1) * 512], in_=psb[bank][:], func=AF.Identity),
                             reads=[pB[bank]], writes=[bGbc])
                        k += 1
                if "dbg_mod" in dbg and l == 0:
                    P.dma("sp", dbg_mod, modT[:], reads=[bMod])
                P.barrier()
            if os.environ.get("MK_STOP") == "A":
                break
            with ExitStack() as st:
                hT = [sbt(st, "hTl", [128, 8, L + 2], BF16), sbt(st, "hTc", [128, 8, LC + 2], BF16)]
                bHl = P.bufs(8, "hTl")
                bHc = P.buf("hTc")
                bPad = P.buf("hTpad")
                wb = sbt(st, "wb", [128, 8, IN_COLS - 768], BF16)
                wh = sbt(st, "wh", [128, 8, 3, 768], BF16)
                bW = P.buf("w_in_b")
                for (tt, n) in ((hT[0], L), (hT[1], LC)):
                    P.op("pool", lambda e: e.memset(tt[:, :, 0:1], 0.0), writes=[bPad])
                    P.op("pool", lambda e: e.memset(tt[:, :, n + 1:n + 2], 0.0), writes=[bPad])

                def hbufs(w, tl, n):
                    if w == 1:
                        return [bHc, bPad]
                    lo = max(tl - 1, 0) // 512
                    hi = min(tl + n, L - 1) // 512
                    return [bHl[i] for i in range(lo, hi + 1)] + [bPad]

                with ExitStack() as s1:
                    xt = [sbt(s1, "xt%d" % i, [128, 1024], F32) for i in range(2)]
                    bXt = P.bufs(2, "xt")
                    xn = [sbt(s1, "xn%d" % i, [128, 4, 1024], BF16) for i in range(2)]
                    bXn = P.bufs(2, "xn")
                    junk = sbt(s1, "junk", [128, 1024], BF16)
                    bJ = P.buf("junk")
                    ssq = [sbt(s1, "ssq%d" % i, [128, 1], F32) for i in range(2)]
                    bSs = P.bufs(2, "ssq")
                    wst = [sbt(s1, "wst%d" % i, [128, IN_COLS], F32) for i in range(2)]
                    bWst = P.bufs(2, "wst")
                    hsw = sbt(s1, "hsw", [128, 3, 768], F32)
                    bHsw = P.buf("hsw")
                    P.dma("pool", hsw[:], I["hsw_bc"][l], writes=[bHsw])
                    for dc in range(8):
                        sl = dc % 2
                        P.dma("pool", wst[sl][:], I["w_in"][l, dc * 128:(dc + 1) * 128, :], writes=[bWst[sl]])
                        P.op("pool", lambda e: e.tensor_copy(out=wb[:, dc, :], in_=wst[sl][:, 768:IN_COLS]), reads=[bWst[sl]], writes=[bW])
                        for k in range(3):
                            P.op("pool", lambda e: e.tensor_tensor(out=wh[:, dc, k, :], in0=wst[sl][:, 0:768], in1=hsw[:, k, :], op=ALU.mult),
                                 reads=[bWst[sl], bHsw], writes=[bW])
                    ti = 0
                    for bi, (t0, nt, w) in enumerate(BLOCKS):
                        if last and w == 1 and False:
                            continue
                        xs_ = bi % 2
                        for t in range(nt):
                            sl = ti % 2
                            ti += 1
                            P.dma("sp", xt[sl][:], xin_ap(l, t0 + t * 128, 128), writes=[bXt[sl]])
                            P.op("act", lambda e: e.activation(out=junk[:], in_=xt[sl][:], func=AF.Square, accum_out=ssq[sl][:]),
                                 reads=[bXt[sl]], writes=[bJ, bSs[sl]])
                            P.op("act", lambda e: e.activation(out=ssq[sl][:], in_=ssq[sl][:], func=AF.Sqrt, bias=epsT[:, 0:1], scale=1.0 / D),
                                 reads=[bSs[sl], bC], writes=[bSs[sl]])
                            P.op("dve", lambda e: e.reciprocal(out=ssq[sl][:], in_=ssq[sl][:]), reads=[bSs[sl]], writes=[bSs[sl]])
                            P.op("dve", lambda e: e.tensor_scalar(out=xn[xs_][:, t, :], in0=xt[sl][:], scalar1=ssq[sl][:, 0:1], scalar2=None, op0=ALU.mult),
                                 reads=[bXt[sl], bSs[sl]], writes=[bXn[xs_]])
                        tl0 = t0 if w == 0 else 0
                        hb = [bHl[bi]] if w == 0 else [bHc]
                        for dc in range(8):
                            bank = 6 + dc % 2
                            pT = psb[bank][:].bitcast(BF16)
                            for t in range(nt):
                                P.op("pe", lambda e: e.transpose(out=pT[:, t * 128:(t + 1) * 128], in_=xn[xs_][:, t, dc * 128:(dc + 1) * 128], identity=identb[:]),
                                     reads=[bXn[xs_], bC], writes=[pB[bank]], inc=(t == nt - 1))
                            dst = hT[w][:, dc, 1 + tl0:1 + tl0 + nt * 128]
                            if dc % 2 == 0:
                                P.op("act", lambda e: e.activation(out=dst, in_=pT[:, 0:nt * 128], func=AF.Identity,
                                                                   bias=modT[:, dc, w:w + 1], scale=gain1[:, dc, w:w + 1]),
                                     reads=[pB[bank], bMod], writes=hb)
                            else:
                                P.op("dve", lambda e: e.tensor_scalar(out=dst, in0=pT[:, 0:nt * 128], scalar1=gain1[:, dc, w:w + 1],
                                                                      scalar2=modT[:, dc, w:w + 1], op0=ALU.mult, op1=ALU.add),
                                     reads=[pB[bank], bMod], writes=hb)
                    if "dbg_hT" in dbg and l == 0:
                        P.dma("sp", dbg_hT, hT[0][:], reads=bHl + [bPad])
                    P.barrier()
                if os.environ.get("MK_STOP") == "B1":
                    break
                with ExitStack() as s2:
                    F_ = lambda name, shape, dt=F32: sbt(s2, name, shape, dt)
                    hsb = F_("hsb", [128, 768]); lng = F_("lng", [128, 256]); lnb = F_("lnb", [128, 256]); bsbc = F_("bsbc", [128, 256])
                    wsT = F_("wsT", [128, 4, 128], BF16)
                    wqn = F_("wqn", [128, 2, 512], BF16); wqr = F_("wqr", [128, 2, 128], BF16)
                    wkn = F_("wkn", [128, 512], BF16); wvb = F_("wvb", [128, 256], BF16)
                    wtmp = F_("wtmp", [128, 640]); gcol = F_("gcol", [128, 3]); wsTf = wtmp[:, 0:512].rearrange("p (g i) -> p g i", g=4)
                    bS2 = P.buf("b2consts")
                    P.dma("sp", hsb[:], I["hsb_bc"][l], writes=[bS2])
                    P.dma("sp", lng[:], I["gm_lng"][l], writes=[bS2])
                    P.dma("sp", lnb[:], I["gm_lnb"][l], writes=[bS2])
                    P.dma("sp", bsbc[:], I["gm_bsbc"][l], writes=[bS2])
                    bWt = P.buf("wtmp")
                    P.dma("sp", wsTf, I["gm_wsT"][l], writes=[bWt])
                    P.op("pool", lambda e: e.tensor_copy(out=wsT[:], in_=wsTf), reads=[bWt], writes=[bS2])
                    P.dma("sp", gcol[:, 0:1], I["qan"][l, 0:128, :], writes=[bS2])
                    P.dma("sp", gcol[0:64, 1:2], I["qan"][l, 128:192, :], writes=[bS2])
                    P.dma("sp", gcol[:, 2:3], I["kvn"][l], writes=[bS2])
                    for rc, rows in ((0, 128), (1, 64)):
                        P.dma("sp", wtmp[0:rows, 0:512], I["wq_n"][l, rc * 128:rc * 128 + rows, :], writes=[bWt])
                        P.dma("sp", wtmp[0:rows, 512:640], I["wq_r"][l, rc * 128:rc * 128 + rows, :], writes=[bWt])
                        P.op("dve", lambda e: e.tensor_scalar(out=wqn[0:rows, rc, :], in0=wtmp[0:rows, 0:512], scalar1=gcol[0:rows, rc:rc + 1], scalar2=None, op0=ALU.mult),
                             reads=[bWt, bS2], writes=[bS2])
                        P.op("dve", lambda e: e.tensor_scalar(out=wqr[0:rows, rc, :], in0=wtmp[0:rows, 512:640], scalar1=gcol[0:rows, rc:rc + 1], scalar2=None, op0=ALU.mult),
                             reads=[bWt, bS2], writes=[bS2])
                    P.dma("sp", wtmp[:, 0:512], I["wk_n"][l], writes=[bWt])
                    P.op("dve", lambda e: e.tensor_scalar(out=wkn[:], in0=wtmp[:, 0:512], scalar1=gcol[:, 2:3], scalar2=None, op0=ALU.mult), reads=[bWt, bS2], writes=[bS2])
                    P.dma("sp", wtmp[:, 0:256], I["wv"][l], writes=[bWt])
                    P.op("dve", lambda e: e.tensor_scalar(out=wvb[:], in0=wtmp[:, 0:256], scalar1=gcol[:, 2:3], scalar2=None, op0=ALU.mult), reads=[bWt, bS2], writes=[bS2])
                    zt0 = F_("zt0", [128, 768]); zt = [zt0, zt0]; bZt0 = P.buf("zt"); bZt = [bZt0, bZt0]
                    zg0 = F_("zg0", [128, 512]); zg = [zg0, zg0]; bZg0 = P.buf("zg"); bZg = [bZg0, bZg0]
                    st1 = [F_("st1%d" % i, [128, 2]) for i in range(2)]; bSt = P.bufs(2, "st1")
                    vc = [F_("vc%d" % i, [128, 256]) for i in range(2)]; bVc = P.bufs(2, "vc")
                    vnb = [F_("vnb%d" % i, [128, 256], BF16) for i in range(2)]; bVn = P.bufs(2, "vnb")
                    ygm = [F_("ygm%d" % i, [128, 256]) for i in range(2)]; bYg = P.bufs(2, "ygm")

                    glt = [F_("glt%d" % i, [128, 512], BF16) for i in range(2)]; bGl = P.bufs(2, "glt")
                    cqb = F_("cqb", [128, 2, 512], BF16); sq0 = F_("sq0", [128, 2, 512]); bCq = P.buf("cqb")
                    sig = sq0[:, 1, :]; bSig = bCq
                    rrep = F_("rrep", [128, 512]); bRr = P.buf("rrep")
                    qt = F_("qt", [128, 4, 512], BF16); bQt = P.buf("qt")
                    kt = F_("kt", [128, 4, 512], BF16); bKt = P.buf("kt")
                    vt = F_("vt", [128, 4, 4, 65], BF16); bVt = P.buf("vt")
                    ckvb = cqb[:, 0, :]; sqk = sq0[:, 0, :]; bCk = bCq
                    rrk = rrep; bRk = bRr
                    rcol = F_("rcol", [128, 4]); bRc = P.buf("rcol")
                    rcs = [F_("ropec", [16, 512]), F_("ropes", [16, 512])]; bRope = P.buf("rope")
                    ra = F_("ra", [16, 512]); rb = F_("rb", [16, 512]); rt1 = F_("rt1", [16, 512]); rt2 = F_("rt2", [16, 512]); bR = P.buf("ropetmp")

                    P.op("pool", lambda e: e.memset(qt[:], 0.0), writes=[bQt])
                    P.op("pool", lambda e: e.memset(kt[:], 0.0), writes=[bKt])
                    P.op("pool", lambda e: e.memset(vt[:], 1.0), writes=[bVt])
                    bZs = P.buf("zs_dram"); bYm = P.buf("ymix_dram"); bGlu = P.buf("glu_dram"); bQd = P.buf("q_dram"); bKd = P.buf("k_dram"); bVd = P.buf("v_dram")

                    def rope_apply(src1, src2, dst1, dst2, nb, do_rope, rd, wr):
                        if do_rope:
                            P.op("dve", lambda e: e.tensor_tensor(out=rt1[:, :nb], in0=src1, in1=rcs[0][:, :nb], op=ALU.mult), reads=rd + [bRope], writes=[bR])
                            P.op("pool", lambda e: e.tensor_tensor(out=rt2[:, :nb], in0=src2, in1=rcs[1][:, :nb], op=ALU.mult), reads=rd + [bRope], writes=[bR])
                            P.op("dve", lambda e: e.tensor_tensor(out=dst1, in0=rt1[:, :nb], in1=rt2[:, :nb], op=ALU.subtract), reads=[bR], writes=wr)
                            P.op("dve", lambda e: e.tensor_tensor(out=rt1[:, :nb], in0=src1, in1=rcs[1][:, :nb], op=ALU.mult), reads=rd + [bRope] + wr, writes=[bR])
                            P.op("pool", lambda e: e.tensor_tensor(out=rt2[:, :nb], in0=src2, in1=rcs[0][:, :nb], op=ALU.mult), reads=rd + [bRope] + wr, writes=[bR])
                            P.op("dve", lambda e: e.tensor_tensor(out=dst2, in0=rt1[:, :nb], in1=rt2[:, :nb], op=ALU.add), reads=[bR], writes=wr)
                        else:
                            P.op("dve", lambda e: e.tensor_copy(out=dst1, in_=src1), reads=rd, writes=wr)
                            P.op("dve", lambda e: e.tensor_copy(out=dst2, in_=src2), reads=rd, writes=wr)

                    tix = 0
                    for bi, (t0, nt, w) in enumerate(BLOCKS):
                        nb = nt * 128
                        tl0 = t0 if w == 0 else 0
                        HT = hT[w]
                        full = not (last and w == 1)
                        hb = hbufs(w, tl0, nb)
                        rhs = lambda dc: HT[:, dc, 1 + tl0:1 + tl0 + nb]
                        if w == 0:
                            P.dma("sp", rcs[0][:, :nb], I["ropec"][:, t0:t0 + nb], writes=[bRope])
                            P.dma("sp", rcs[1][:, :nb], I["ropes"][:, t0:t0 + nb], writes=[bRope])
                        if full:
                            for j in range(2):
                                for (bank, c0) in ((4, 512 + j * 128), (5, 768 + j * 128)):
                                    for dc in range(8):
                                        P.op("pe", lambda e: e.matmul(psb[bank][:, :nb], lhsT=wb[:, dc, c0:c0 + 128], rhs=rhs(dc), start=(dc == 0), stop=(dc == 7)),
                                             reads=hb + [bW], writes=[pB[bank]], inc=(dc == 7))
                                P.op("act", lambda e: e.activation(out=sig[:, :nb], in_=psb[5][:, :nb], func=AF.Sigmoid), reads=[pB[5]], writes=[bSig])
                                P.op("dve", lambda e: e.tensor_tensor(out=glt[j][:, :nb], in0=psb[4][:, :nb], in1=sig[:, :nb], op=ALU.mult),
                                     reads=[pB[4], bSig], writes=[bGl[j]])
                                P.dma("pool", glu[j * 128:(j + 1) * 128, t0:t0 + nb], glt[j][:, :nb], reads=[bGl[j]], writes=[bGlu])
                            for (bank, c0, rows, rc) in ((4, 1024, 128, 0), (5, 1152, 64, 1)):
                                for dc in range(8):
                                    P.op("pe", lambda e: e.matmul(psb[bank][0:rows, :nb], lhsT=wb[:, dc, c0:c0 + rows], rhs=rhs(dc), start=(dc == 0), stop=(dc == 7)),
                                         reads=hb + [bW], writes=[pB[bank]], inc=(dc == 7))
                                P.op("act", lambda e: e.activation(out=cqb[0:rows, rc, :nb], in_=psb[bank][0:rows, :nb], func=AF.Identity), reads=[pB[bank]], writes=[bCq])
                                P.op("act", lambda e: e.activation(out=sq0[0:rows, rc, :nb], in_=psb[bank][0:rows, :nb], func=AF.Square), reads=[pB[bank]], writes=[bCq])
                            P.op("pe", lambda e: e.matmul(psb[6][:, :nb], lhsT=onesf[:, :], rhs=sq0[:, 0, :nb], start=True, stop=False), reads=[bCq, bC], writes=[pB[6]], inc=False)
                            P.op("pe", lambda e: e.matmul(psb[6][:, :nb], lhsT=onesf[0:64, :], rhs=sq0[0:64, 1, :nb], start=False, stop=True), reads=[bCq, bC], writes=[pB[6]])
                            P.op("act", lambda e: e.activation(out=rrep[:, :nb], in_=psb[6][:, :nb], func=AF.Sqrt, bias=epsT[:, 0:1], scale=1.0 / 192), reads=[pB[6], bC], writes=[bRr])
                            P.op("dve", lambda e: e.reciprocal(out=rrep[:, :nb], in_=rrep[:, :nb]), reads=[bRr], writes=[bRr])
                            for h in range(4):
                                P.op("pe", lambda e: e.matmul(psb[7][:, :nb], lhsT=wqn[:, 0, h * 128:(h + 1) * 128], rhs=cqb[:, 0, :nb], start=True, stop=False), reads=[bCq, bS2], writes=[pB[7]], inc=False)
                                P.op("pe", lambda e: e.matmul(psb[7][:, :nb], lhsT=wqn[0:64, 1, h * 128:(h + 1) * 128], rhs=cqb[0:64, 1, :nb], start=False, stop=True), reads=[bCq, bS2], writes=[pB[7]])
                                P.op("dve", lambda e: e.tensor_tensor(out=qt[64:128, h, :nb], in0=psb[7][64:128, :nb], in1=rrep[64:128, :nb], op=ALU.mult),
                                     reads=[pB[7], bRr], writes=[bQt])
                                for (bank, c0) in ((4, h * 32), (5, h * 32 + 16)):
                                    P.op("pe", lambda e: e.matmul(psb[bank][0:16, :nb], lhsT=wqr[:, 0, c0:c0 + 16], rhs=cqb[:, 0, :nb], start=True, stop=False), reads=[bCq, bS2], writes=[pB[bank]], inc=False)
                                    P.op("pe", lambda e: e.matmul(psb[bank][0:16, :nb], lhsT=wqr[0:64, 1, c0:c0 + 16], rhs=cqb[0:64, 1, :nb], start=False, stop=True), reads=[bCq, bS2], writes=[pB[bank]])
                                P.op("dve", lambda e: e.tensor_tensor(out=ra[:, :nb], in0=psb[4][0:16, :nb], in1=rrep[0:16, :nb], op=ALU.mult), reads=[pB[4], bRr], writes=[bR])
                                P.op("dve", lambda e: e.tensor_tensor(out=rb[:, :nb], in0=psb[5][0:16, :nb], in1=rrep[0:16, :nb], op=ALU.mult), reads=[pB[5], bRr], writes=[bR])
                                rope_apply(ra[:, :nb], rb[:, :nb], qt[0:16, h, :nb], qt[32:48, h, :nb], nb, w == 0, [bR], [bQt])
                            P.dma("pool", qT[:, :, t0:t0 + nb].rearrange("h p t -> p h t"), qt[:, :, :nb], reads=[bQt], writes=[bQd])
                        for dc in range(8):
                            P.op("pe", lambda e: e.matmul(psb[6][:, :nb], lhsT=wb[:, dc, 1216:1344], rhs=rhs(dc), start=(dc == 0), stop=(dc == 7)),
                                 reads=hb + [bW], writes=[pB[6]], inc=(dc == 7))
                        P.op("act", lambda e: e.activation(out=ckvb[:, :nb], in_=psb[6][:, :nb], func=AF.Identity), reads=[pB[6]], writes=[bCk])
                        P.op("act", lambda e: e.activation(out=sqk[:, :nb], in_=psb[6][:, :nb], func=AF.Square), reads=[pB[6]], writes=[bCk])
                        P.op("pe", lambda e: e.matmul(psb[7][:, :nb], lhsT=onesf[:, :], rhs=sqk[:, :nb], start=True, stop=True), reads=[bCk, bC], writes=[pB[7]])
                        P.op("act", lambda e: e.activation(out=rrk[:, :nb], in_=psb[7][:, :nb], func=AF.Sqrt, bias=epsT[:, 0:1], scale=1.0 / 128), reads=[pB[7], bC], writes=[bRk])
                        P.op("dve", lambda e: e.reciprocal(out=rrk[:, :nb], in_=rrk[:, :nb]), reads=[bRk], writes=[bRk])
                        for t in range(nt):
                            P.op("pe", lambda e: e.matmul(psb[5][:, 32 + t:33 + t], lhsT=sqk[:, t * 128:(t + 1) * 128], rhs=onesf[:, 0:1], start=(t == 0), stop=True),
                                 reads=[bCk, bC], writes=[pB[5]], inc=(t == nt - 1))
                        P.op("act", lambda e: e.activation(out=rcol[:, 0:nt], in_=psb[5][:, 32:32 + nt], func=AF.Sqrt, bias=epsT[:, 0:1], scale=1.0 / 128), reads=[pB[5], bC], writes=[bRc])
                        P.op("dve", lambda e: e.reciprocal(out=rcol[:, 0:nt], in_=rcol[:, 0:nt]), reads=[bRc], writes=[bRc])
                        for h in range(4):
                            P.op("pe", lambda e: e.matmul(psb[7][:, :nb], lhsT=wkn[:, h * 128:(h + 1) * 128], rhs=ckvb[:, :nb], start=True, stop=True), reads=[bCk, bS2], writes=[pB[7]])
                            P.op("dve", lambda e: e.tensor_tensor(out=kt[64:128, h, :nb], in0=psb[7][64:128, :nb], in1=rrk[64:128, :nb], op=ALU.mult),
                                 reads=[pB[7], bRk], writes=[bKt])
                        for (bank, c0) in ((4, 1344), (5, 1360)):
                            for dc in range(8):
                                P.op("pe", lambda e: e.matmul(psb[bank][0:16, :nb], lhsT=wb[:, dc, c0:c0 + 16], rhs=rhs(dc), start=(dc == 0), stop=(dc == 7)),
                                     reads=hb + [bW], writes=[pB[bank]], inc=(dc == 7))
                        P.op("act", lambda e: e.activation(out=ra[:, :nb], in_=psb[4][0:16, :nb], func=AF.Identity), reads=[pB[4]], writes=[bR])
                        P.op("act", lambda e: e.activation(out=rb[:, :nb], in_=psb[5][0:16, :nb], func=AF.Identity), reads=[pB[5]], writes=[bR])
                        rope_apply(ra[:, :nb], rb[:, :nb], kt[0:16, 0, :nb], kt[32:48, 0, :nb], nb, w == 0, [bR], [bKt])
                        for h in range(1, 4):
                            P.op("pool", lambda e: e.tensor_copy(out=kt[0:16, h, :nb], in_=kt[0:16, 0, :nb]), reads=[bKt], writes=[bKt])
                            P.op("pool", lambda e: e.tensor_copy(out=kt[32:48, h, :nb], in_=kt[32:48, 0, :nb]), reads=[bKt], writes=[bKt])
                        P.dma("pool", kT[:, :, t0:t0 + nb].rearrange("h p t -> p h t"), kt[:, :, :nb], reads=[bKt], writes=[bKd])
                        for t in range(nt):
                            P.op("pe", lambda e: e.matmul(psb[7][:, 0:256], lhsT=ckvb[:, t * 128:(t + 1) * 128], rhs=wvb[:, :], start=True, stop=True), reads=[bCk, bS2], writes=[pB[7]])
                            P.op("act", lambda e: e.activation(out=vt[:, t, :, 0:64], in_=psb[7][:, 0:256].rearrange("p (h d) -> p h d", h=4), func=AF.Identity, scale=rcol[:, t:t + 1]),
                                 reads=[pB[7], bRc], writes=[bVt])
                        P.dma("pool", vv[t0:t0 + nb, :].rearrange("(t p) c -> p t c", p=128), vt[:, 0:nt, :, :].rearrange("p t h d -> p t (h d)"), reads=[bVt], writes=[bVd])
                        if not full:
                            continue
                        for t in range(nt):
                            sl = tix % 2
                            tix += 1
                            tl = tl0 + t * 128
                            tg = t0 + t * 128
                            hbt = hbufs(w, tl, 128)
                            for half in range(2):
                                bank = half
                                n = 0
                                for k in range(3):
                                    for dc in range(8):
                                        P.op("pe", lambda e: e.matmul(psb[bank][:, 0:384], lhsT=HT[:, dc, tl + k:tl + k + 128], rhs=wh[:, dc, k, half * 384:(half + 1) * 384],
                                                                      start=(n == 0), stop=(n == 23)), reads=hbt + [bW], writes=[pB[bank]], inc=(n == 23))
                                        n += 1
                                P.op("dve", lambda e: e.tensor_tensor(out=zt[sl][:, half * 384:(half + 1) * 384], in0=psb[bank][:, 0:384], in1=hsb[:, half * 384:(half + 1) * 384], op=ALU.add),
                                     reads=[pB[bank], bS2], writes=[bZt[sl]])
                            P.dma("pool", zs[tg:tg + 128, :], zt[sl][:], reads=[bZt[sl]], writes=[bZs])
                            for dc in range(8):
                                P.op("pe", lambda e: e.matmul(psb[2][:, :], lhsT=HT[:, dc, tl + 1:tl + 129], rhs=wb[:, dc, 0:512], start=(dc == 0), stop=(dc == 7)),
                                     reads=hbt + [bW], writes=[pB[2]], inc=(dc == 7))
                            P.op("act", lambda e: e.activation(out=zg[sl][:, 0:256], in_=psb[2][:, 0:256], func=AF.Gelu), reads=[pB[2]], writes=[bZg[sl]])
                            P.op("act", lambda e: e.activation(out=zg[sl][:, 256:512], in_=psb[2][:, 256:512], func=AF.Gelu, accum_out=st1[sl][:, 0:1]), reads=[pB[2]], writes=[bZg[sl], bSt[sl]])
                            P.op("dve", lambda e: e.tensor_scalar(out=st1[sl][:, 0:1], in0=st1[sl][:, 0:1], scalar1=-1.0 / 256, scalar2=None, op0=ALU.mult), reads=[bSt[sl]], writes=[bSt[sl]])
                            P.op("dve", lambda e: e.tensor_scalar(out=vc[sl][:], in0=zg[sl][:, 256:512], scalar1=st1[sl][:, 0:1], scalar2=None, op0=ALU.add), reads=[bZg[sl], bSt[sl]], writes=[bVc[sl]])
                            P.op("act", lambda e: e.activation(out=junk2[:, 0:256], in_=vc[sl][:], func=AF.Square, accum_out=st1[sl][:, 1:2]), reads=[bVc[sl]], writes=[bJ2, bSt[sl]])
                            P.op("act", lambda e: e.activation(out=st1[sl][:, 1:2], in_=st1[sl][:, 1:2], func=AF.Sqrt, bias=epsT[:, 0:1], scale=1.0 / 256), reads=[bSt[sl], bC], writes=[bSt[sl]])
                            P.op("dve", lambda e: e.reciprocal(out=st1[sl][:, 1:2], in_=st1[sl][:, 1:2]), reads=[bSt[sl]], writes=[bSt[sl]])
                            P.op("dve", lambda e: e.scalar_tensor_tensor(out=vc[sl][:], in0=vc[sl][:], scalar=st1[sl][:, 1:2], in1=lng[:], op0=ALU.mult, op1=ALU.mult), reads=[bVc[sl], bSt[sl], bS2], writes=[bVc[sl]])
                            P.op("pool", lambda e: e.tensor_tensor(out=vnb[sl][:], in0=vc[sl][:], in1=lnb[:], op=ALU.add), reads=[bVc[sl], bS2], writes=[bVn[sl]])
                            for g in range(4):
                                P.op("pe", lambda e: e.matmul(psb[3][:, g * 64:(g + 1) * 64], lhsT=wsT[:, g, :], rhs=vnb[sl][:, g * 64:(g + 1) * 64], start=(g == 0), stop=True),
                                     reads=[bVn[sl], bS2], writes=[pB[3]], inc=(g == 3))
                            P.op("dve", lambda e: e.tensor_tensor(out=ygm[sl][:], in0=psb[3][:, 0:256], in1=bsbc[:], op=ALU.add), reads=[pB[3], bS2], writes=[bYg[sl]])
                            P.op("pool", lambda e: e.tensor_tensor(out=ygm[sl][:], in0=ygm[sl][:], in1=zg[sl][:, 0:256], op=ALU.mult), reads=[bYg[sl], bZg[sl]], writes=[bYg[sl]])
                            P.dma("pool", ymix[tg:tg + 128, 256:512], ygm[sl][:], reads=[bYg[sl]], writes=[bYm])
                    P.barrier()
            if os.environ.get("MK_STOP") == "B":
                break
            with ExitStack() as st:
                F_ = lambda name, shape, dt=F32: sbt(st, name, shape, dt)
                kTs = F_("kTs", [128, 4, NTOK], BF16); bK = P.buf("kTs")
                vs = F_("vs", [128, 34, 260], BF16); bV = P.buf("vs")
                P.dma("sp", kTs[:, 0:2, :], kT[0:2].rearrange("h p t -> p h t"), reads=[bKd], writes=[bK])
                P.dma("sp", kTs[:, 2:4, :], kT[2:4].rearrange("h p t -> p h t"), reads=[bKd], writes=[bK])
                for c in range(0, 34, 8):
                    n = min(8, 34 - c)
                    P.dma("pool", vs[:, c:c + n, :], vv[c * 128:(c + n) * 128, :].rearrange("(t p) c -> p t c", p=128), reads=[bVd], writes=[bV])
                qs = [F_("qs%d" % i, [128, 4, 512], BF16) for i in range(2)]; bQs = P.bufs(2, "qs")
                pt = [F_("pt%d" % i, [128, 512], BF16) for i in range(3)]; bPt = P.bufs(3, "pt")
                oat = [F_("oat%d" % i, [128, 4, 256]) for i in range(2)]; bOa = P.bufs(2, "oat")
                rden = [F_("rden%d" % i, [128, 4]) for i in range(2)]; bRd = P.bufs(2, "rden")
                SC = 1.0 / float(np.sqrt(96.0))
                ablocks = [(bi, t0, nt, w) for bi, (t0, nt, w) in enumerate(BLOCKS) if not (last and w == 1)]
                iters = []
                for ai, (bi, t0, nt, w) in enumerate(ablocks):
                    kts = list(range(34)) if w == 0 else [32, 33]
                    for h in range(4):
                        for ki, kb in enumerate(kts):
                            iters.append((ai, t0, nt, w, h, ki, kb, len(kts)))

                def load_q(ai):
                    bi, t0, nt, w = ablocks[ai]
                    nb = nt * 128
                    P.dma("sp", qs[ai % 2][:, :, :nb], qT[:, :, t0:t0 + nb].rearrange("h p t -> p h t"), reads=[bQd], writes=[bQs[ai % 2]])

                def issue_qk(i):
                    ai, t0, nt, w, h, ki, kb, nk = iters[i]
                    nb = nt * 128
                    sl = ai % 2
                    sb_ = i % 3
                    if h == 0 and ki == 0 and ai + 1 < len(ablocks):
                        load_q(ai + 1)
                    P.op("pe", lambda e: e.matmul(psb[sb_][:, :nb], lhsT=kTs[:, h, kb * 128:(kb + 1) * 128], rhs=qs[sl][:, h, :nb], start=True, stop=True),
                         reads=[bK, bQs[sl]], writes=[pB[sb_]])
                    P.op("act", lambda e: e.activation(out=pt[sb_][:, :nb], in_=psb[sb_][:, :nb], func=AF.Exp, scale=SC), reads=[pB[sb_]], writes=[bPt[sb_]])

                def issue_pv(i):
                    ai, t0, nt, w, h, ki, kb, nk = iters[i]
                    nb = nt * 128
                    sl = ai % 2
                    sb_ = i % 3
                    ob = 4 + (h % 2)
                    for q_ in range(nt):
                        P.op("pe", lambda e: e.matmul(psb[ob][:, q_ * 65:(q_ + 1) * 65], lhsT=pt[sb_][:, q_ * 128:(q_ + 1) * 128], rhs=vs[:, kb, h * 65:(h + 1) * 65],
                                                      start=(ki == 0 and q_ == 0), stop=(ki == nk - 1)),
                             reads=[bPt[sb_], bV], writes=[pB[ob]], inc=(q_ == nt - 1))
                    if ki == nk - 1:
                        rs_ = h % 2
                        P.op("dve", lambda e: e.reciprocal(out=rden[rs_][:, 0:nt], in_=psb[ob][:, 0:nt * 65].rearrange("p (q c) -> p q c", c=65)[:, :, 64]),
                             reads=[pB[ob]], writes=[bRd[rs_]])
                        for q_ in range(nt):
                            P.op("dve", lambda e: e.tensor_scalar(out=oat[sl][:, q_, h * 64:(h + 1) * 64], in0=psb[ob][:, q_ * 65:q_ * 65 + 64], scalar1=rden[rs_][:, q_:q_ + 1], scalar2=None, op0=ALU.mult),
                                 reads=[pB[ob], bRd[rs_]], writes=[bOa[sl]])
                        if h == 3:
                            P.dma("pool", ymix[t0:t0 + nb, 768:1024].rearrange("(t p) c -> p t c", p=128), oat[sl][:, 0:nt, :], reads=[bOa[sl]], writes=[bYm])

                LA = 2
                load_q(0)
                for i in range(len(iters) + LA):
                    if i < len(iters):
                        issue_qk(i)
                    if i - LA >= 0:
                        issue_pv(i - LA)
                P.barrier()
            if os.environ.get("MK_STOP") == "AT":
                break
            with ExitStack() as st:
                F_ = lambda name, shape, dt=F32: sbt(st, name, shape, dt)
                gl = [F_("gll", [128, 2, L + 30], BF16), F_("glc", [128, 2, LC + 30], BF16)]; bG = P.bufs(2, "gl")
                for (w, n, c0) in ((0, L, 0), (1, LC, L)):
                    if last and w == 1:
                        continue
                    P.op("pool", lambda e: e.memset(gl[w][:, :, 0:15], 0.0), writes=[bG[w]])
                    P.op("pool", lambda e: e.memset(gl[w][:, :, n + 15:n + 30], 0.0), writes=[bG[w]])
                    for j in range(2):
                        P.dma("sp", gl[w][:, j, 15:15 + n], glu[j * 128:(j + 1) * 128, c0:c0 + n], reads=[bGlu], writes=[bG[w]])
                cvw = F_("cvw", [128, 2, 31]); cvb = F_("cvb", [128, 2]); clg = F_("clg", [128, 2]); clb = F_("clb", [128, 2]); bCv = P.buf("cvconst")
                P.dma("sp", cvw[:], I["cv_w"][l], writes=[bCv])
                P.dma("sp", cvb[:], I["cv_bT"][l], writes=[bCv])
                P.dma("sp", clg[:], I["cv_lgT"][l], writes=[bCv])
                P.dma("sp", clb[:], I["cv_lbT"][l], writes=[bCv])
                dg = F_("dg", [128, 2, 31, 128], BF16); bDg = P.buf("dg")
                for j in range(2):
                    for k in range(31):
                        P.op("dve" if k % 2 == 0 else "pool", lambda e: e.tensor_scalar(out=dg[:, j, k, :], in0=ident[:], scalar1=cvw[:, j, k:k + 1], scalar2=None, op0=ALU.mult),
                             reads=[bCv, bC], writes=[bDg])
                gsb = [F_("gsb%d" % i, [128, 512]) for i in range(2)]; bGs = P.bufs(2, "gsb")
                cen = [F_("cen%d" % i, [128, 512]) for i in range(2)]; bCe = P.bufs(2, "cen")
                sqs = [F_("sqs%d" % i, [128, 512]) for i in range(2)]; bSq = P.bufs(2, "sqs")
                ycT = [F_("ycT%d" % i, [128, 512]) for i in range(2)]; bYc = P.bufs(2, "ycT")
                ysb = [F_("ysb%d" % i, [128, 256]) for i in range(2)]; bYs = P.bufs(2, "ysb")
                tix = 0
                for bi, (t0, nt, w) in enumerate(BLOCKS):
                    if last and w == 1:
                        continue
                    nb = nt * 128
                    tl0 = t0 if w == 0 else 0
                    for j in range(2):
                        for k in range(31):
                            P.op("pe", lambda e: e.matmul(psb[j][:, :nb], lhsT=dg[:, j, k, :], rhs=gl[w][:, j, tl0 + k:tl0 + k + nb], start=(k == 0), stop=(k == 30)),
                                 reads=[bDg, bG[w]], writes=[pB[j]], inc=(k == 30))
                        P.op("act", lambda e: e.activation(out=gsb[j][:, :nb], in_=psb[j][:, :nb], func=AF.Identity, bias=cvb[:, j:j + 1]), reads=[pB[j], bCv], writes=[bGs[j]])
                        P.op("pe", lambda e: e.matmul(psb[2 + j][:, :nb], lhsT=blk64[:], rhs=gsb[j][:, :nb], start=True, stop=True), reads=[bGs[j], bC], writes=[pB[2 + j]])
                        P.op("dve", lambda e: e.tensor_tensor(out=cen[j][:, :nb], in0=gsb[j][:, :nb], in1=psb[2 + j][:, :nb], op=ALU.subtract), reads=[bGs[j], pB[2 + j]], writes=[bCe[j]])
                        P.op("act", lambda e: e.activation(out=sqs[j][:, :nb], in_=cen[j][:, :nb], func=AF.Square), reads=[bCe[j]], writes=[bSq[j]])
                        P.op("pe", lambda e: e.matmul(psb[2 + j][:, :nb], lhsT=blk64[:], rhs=sqs[j][:, :nb], start=True, stop=True), reads=[bSq[j], bC], writes=[pB[2 + j]])
                        P.op("act", lambda e: e.activation(out=sqs[j][:, :nb], in_=psb[2 + j][:, :nb], func=AF.Sqrt, bias=epsT[:, 0:1]), reads=[pB[2 + j], bC], writes=[bSq[j]])
                        P.op("dve", lambda e: e.reciprocal(out=sqs[j][:, :nb], in_=sqs[j][:, :nb]), reads=[bSq[j]], writes=[bSq[j]])
                        P.op("dve", lambda e: e.tensor_tensor(out=cen[j][:, :nb], in0=cen[j][:, :nb], in1=sqs[j][:, :nb], op=ALU.mult), reads=[bCe[j], bSq[j]], writes=[bCe[j]])
                        P.op("act", lambda e: e.activation(out=ycT[j][:, :nb], in_=cen[j][:, :nb], func=AF.Silu, bias=clb[:, j:j + 1], scale=clg[:, j:j + 1]), reads=[bCe[j], bCv], writes=[bYc[j]])
                    for t in range(nt):
                        sl = tix % 2
                        tix += 1
                        bank = 4 + sl
                        for j in range(2):
                            P.op("pe", lambda e: e.transpose(out=psb[bank][:, j * 128:(j + 1) * 128], in_=ycT[j][:, t * 128:(t + 1) * 128], identity=ident[:]),
                                 reads=[bYc[j], bC], writes=[pB[bank]], inc=(j == 1))
                        P.op("act", lambda e: e.activation(out=ysb[sl][:], in_=psb[bank][:, 0:256], func=AF.Identity), reads=[pB[bank]], writes=[bYs[sl]])
                        tg = t0 + t * 128
                        P.dma("pool", ymix[tg:tg + 128, 512:768], ysb[sl][:], reads=[bYs[sl]], writes=[bYm])
                P.barrier()
            if os.environ.get("MK_STOP") == "CV":
                break
            def hyena(w, Lh, tag, c0, ksp):
                NT = Lh // 128
                NR = 2 * NT
                NS = max(NT // 2, 1)
                TB = min(512, Lh)
                with ExitStack() as st:
                    F_ = lambda name, shape, dt=F32: sbt(st, name, shape, dt)
                    slab = [F_("slab%d" % i, [128, 32 * 512 if Lh == L else NT * 512], BF16) for i in range(2)]; bSl = P.bufs(2, "slab"); bSlB = P.bufs(2, "slabB")

                    def load_slab(sl, view3, src3, n0):
                        h = max(n0 // 2, 1)
                        P.dma("sp", view3[:, 0:h, :], src3[:, 0:h, :], writes=[bSl[sl]])
                        if h < n0:
                            P.dma("act", view3[:, h:n0, :], src3[:, h:n0, :], writes=[bSlB[sl]])
                    invn = F_("invn", [128, 512]); bIn = P.buf("invn")
                    rsc = F_("rsc", [128, NR]); bRs = P.buf("rsc")
                    P.dma("sp", rsc[:], I["rsc" + tag], writes=[bRs])
                    bKs = P.buf("kspec_dram")
                    with ExitStack() as s1:
                        G_ = lambda name, shape, dt=F32: sbt(s1, name, shape, dt)
                        w1 = G_("w1", [33, 64]); w2 = G_("w2", [64, 64]); w3 = G_("w3", [64, 1024]); cb = G_("cb", [64, 6]); bFw = P.buf("fw")
                        P.dma("sp", w1[:], I["hy_f_w1"][l], writes=[bFw])
                        P.dma("sp", w2[:], I["hy_f_w2"][l], writes=[bFw])
                        P.dma("sp", w3[:], I["hy_f_w3"][l], writes=[bFw])
                        P.dma("sp", cb[:, 0:1], I["hy_b1c"][l], writes=[bFw])
                        P.dma("sp", cb[:, 1:2], I["hy_b2c"][l], writes=[bFw])
                        P.dma("sp", cb[:, 2:3], I["hy_frc"][l], writes=[bFw])
                        P.op("dve", lambda e: e.tensor_tensor(out=cb[:, 3:4], in0=cb[:, 0:1], in1=cb[:, 2:3], op=ALU.mult), reads=[bFw], writes=[bFw])
                        P.op("dve", lambda e: e.tensor_tensor(out=cb[:, 4:5], in0=cb[:, 1:2], in1=cb[:, 2:3], op=ALU.mult), reads=[bFw], writes=[bFw])
                        zT = G_("zT", [33, TB]); bZ = P.buf("zT")
                        arg = G_("arg", [64, TB]); msk_ = G_("mskf", [64, TB]); h1 = G_("h1", [64, TB]); bAr = P.buf("arg"); bH1 = P.buf("h1")
                        h2T = G_("h2T", [64, Lh]); bH2 = P.buf("h2T")
                        rre = G_("rre", [128, NT, 512], BF16); rim = G_("rim", [128, NT, 512], BF16); bRe = P.buf("rre")
                        dect = [G_("dect%d" % i, [128, 256]) for i in range(2)]; bDe = P.bufs(2, "dect")
                        kk = G_("kk", [128, 1024]); ak = G_("ak", [128, 1024]); bKk = P.buf("kk"); bAk = P.buf("ak")
                        spo = [G_("spo%d" % i, [128, 512]) for i in range(2)]; bSp = P.bufs(2, "spo")

                        def sin_layer(ps_ap, bcol, dst, nbk, rd, wr):
                            P.op("act", lambda e: e.activation(out=arg[:, :nbk], in_=ps_ap, func=AF.Identity, bias=cb[:, bcol:bcol + 1], scale=cb[:, 2:3]), reads=rd + [bFw], writes=[bAr])
                            for _ in range(2):
                                P.op("dve", lambda e: e.tensor_single_scalar(out=msk_[:, :nbk], in_=arg[:, :nbk], scalar=PI, op=ALU.is_gt), reads=[bAr], writes=[bAr])
                                P.op("dve", lambda e: e.scalar_tensor_tensor(out=arg[:, :nbk], in0=msk_[:, :nbk], scalar=-2.0 * PI, in1=arg[:, :nbk], op0=ALU.mult, op1=ALU.add), reads=[bAr], writes=[bAr])
                                P.op("dve", lambda e: e.tensor_single_scalar(out=msk_[:, :nbk], in_=arg[:, :nbk], scalar=-PI, op=ALU.is_lt), reads=[bAr], writes=[bAr])
                                P.op("dve", lambda e: e.scalar_tensor_tensor(out=arg[:, :nbk], in0=msk_[:, :nbk], scalar=2.0 * PI, in1=arg[:, :nbk], op0=ALU.mult, op1=ALU.add), reads=[bAr], writes=[bAr])
                            P.op("act", lambda e: e.activation(out=dst, in_=arg[:, :nbk], func=AF.Sin), reads=[bAr], writes=wr)

                        for b0 in range(0, Lh, TB):
                            P.dma("sp", zT[:, :], I["hyz" + tag][:, b0:b0 + TB], writes=[bZ])
                            P.op("pe", lambda e: e.matmul(psb[0][0:64, :TB], lhsT=w1[:, :], rhs=zT[:, :], start=True, stop=True), reads=[bZ, bFw], writes=[pB[0]])
                            sin_layer(psb[0][0:64, :TB], 3, h1[:, :TB], TB, [pB[0]], [bH1])
                            P.op("pe", lambda e: e.matmul(psb[1][0:64, :TB], lhsT=w2[:, :], rhs=h1[:, :TB], start=True, stop=True), reads=[bH1, bFw], writes=[pB[1]])
                            sin_layer(psb[1][0:64, :TB], 4, h2T[:, b0:b0 + TB], TB, [pB[1]], [bH2])
                        for ti in range(NT):
                            sl = ti % 2
                            P.dma("sp", dect[sl][:], I["hydec" + tag][:, ti, :], writes=[bDe[sl]])
                            for o in range(2):
                                bank = 2 + o
                                P.op("pe", lambda e: e.matmul(psb[bank][:, :], lhsT=h2T[:, ti * 128:(ti + 1) * 128], rhs=w3[:, o * 512:(o + 1) * 512], start=True, stop=True), reads=[bH2, bFw], writes=[pB[bank]])
                                P.op("dve", lambda e: e.tensor_tensor(out=kk[:, o * 512:(o + 1) * 512].rearrange("p (a c) -> p a c", c=256), in0=psb[bank][:, :].rearrange("p (a c) -> p a c", c=256),
                                                                       in1=dect[sl][:].unsqueeze(1).to_broadcast([128, 2, 256]), op=ALU.mult), reads=[pB[bank], bDe[sl]], writes=[bKk])
                            if ti == 0:
                                for o in range(2):
                                    P.op("dve", lambda e: e.memset(kk[0:1, o * 512 + 256:o * 512 + 512], 0.0), reads=[bKk], writes=[bKk])
                            P.op("dve", lambda e: e.scalar_tensor_tensor(out=ak[:], in0=kk[:], scalar=-1.0, in1=kk[:], op0=ALU.mult, op1=ALU.max), reads=[bKk], writes=[bAk])
                            for o in range(2):
                                P.op("pe", lambda e: e.matmul(psb[6 + o][:, :], lhsT=onesf[:], rhs=ak[:, o * 512:(o + 1) * 512], start=(ti == 0), stop=(ti == NT - 1)), reads=[bAk, bC], writes=[pB[6 + o]])
                            kv_ = kk[:].rearrange("p (o d c) -> p o d c", o=2, d=2)
                            P.op("pool", lambda e: e.tensor_tensor(out=rre[:, ti, :].rearrange("p (o c) -> p o c", o=2), in0=kv_[:, :, 0, :], in1=kv_[:, :, 1, :], op=ALU.add), reads=[bKk], writes=[bRe])
                            P.op("pool", lambda e: e.tensor_tensor(out=rim[:, ti, :].rearrange("p (o c) -> p o c", o=2), in0=kv_[:, :, 0, :], in1=kv_[:, :, 1, :], op=ALU.subtract), reads=[bKk], writes=[bRe])
                        for o in range(2):
                            P.op("act", lambda e: e.activation(out=invn[:, o * 256:(o + 1) * 256], in_=psb[6 + o][:, 0:256], func=AF.Identity), reads=[pB[6 + o]], writes=[bIn])
                            P.op("dve", lambda e: e.tensor_tensor(out=invn[:, o * 256:(o + 1) * 256], in0=psb[6 + o][:, 256:512], in1=invn[:, o * 256:(o + 1) * 256], op=ALU.add), reads=[pB[6 + o], bIn], writes=[bIn])
                        P.op("dve", lambda e: e.reciprocal(out=invn[:], in_=invn[:]), reads=[bIn], writes=[bIn])
                        k = 0
                        for s_ in range(NS):
                            sl = s_ % 2
                            sv = slab[sl][:, 0:NT * 512].rearrange("p (t c) -> p t c", c=512)
                            load_slab(sl, sv, I["dftf" + tag][s_], NT)
                            rcs_ = [2 * s_, 2 * s_ + 1, NT + 2 * s_, NT + 2 * s_ + 1] if NT >= 2 else None
                            for j, rc in enumerate(rcs_):
                                src = rim if j >= 2 else rre
                                bank = k % 2
                                for tc in range(NT):
                                    P.op("pe", lambda e: e.matmul(psb[bank][:, :], lhsT=sv[:, tc, j * 128:(j + 1) * 128], rhs=src[:, tc, :], start=(tc == 0), stop=(tc == NT - 1)),
                                         reads=[bSl[sl], bSlB[sl], bRe], writes=[pB[bank]], inc=(tc == NT - 1))
                                osl = k % 2
                                P.op("dve", lambda e: e.scalar_tensor_tensor(out=spo[osl][:], in0=psb[bank][:, :], scalar=rsc[:, rc:rc + 1], in1=invn[:], op0=ALU.mult, op1=ALU.mult),
                                     reads=[pB[bank], bRs, bIn], writes=[bSp[osl]])
                                if rc == NT:
                                    for tc in range(NT):
                                        P.op("pe", lambda e: e.matmul(psb[2][0:1, :], lhsT=sv[:, tc, j * 128:j * 128 + 1], rhs=rre[:, tc, :], start=(tc == 0), stop=(tc == NT - 1)),
                                             reads=[bSl[sl], bSlB[sl], bRe], writes=[pB[2]], inc=(tc == NT - 1))
                                    P.op("dve", lambda e: e.scalar_tensor_tensor(out=spo[osl][0:1, :], in0=psb[2][0:1, :], scalar=rsc[0:1, rc:rc + 1], in1=invn[0:1, :], op0=ALU.mult, op1=ALU.mult),
                                         reads=[pB[2], bRs, bIn, bSp[osl]], writes=[bSp[osl]])
                                P.dma("pool", ksp[rc * 128:(rc + 1) * 128, :], spo[osl][:], reads=[bSp[osl]], writes=[bKs])
                                k += 1
                        P.barrier()
                    with ExitStack() as s2:
                        G_ = lambda name, shape, dt=F32: sbt(s2, name, shape, dt)
                        ub = G_("ub", [128, NT, 256], BF16); bUb = P.buf("ub")
                        Yf = G_("Yf", [128, NR, 256], BF16); bYf = P.buf("Yf")
                        y1f = G_("y1f", [128, NT, 256]); bY1 = P.buf("y1f")
                        hyb = G_("hyb", [128, 512]); bHb = P.buf("hyb")
                        P.dma("sp", hyb[:], I["hyb_bc"][l], writes=[bHb])
                        vst = [G_("vst%d" % i, [128, 8, 256]) for i in range(2)]; bVs = P.bufs(2, "vst")
                        for c in range(0, NT, 8):
                            n = min(8, NT - c)
                            sl = (c // 8) % 2
                            P.dma("sp", vst[sl][:, 0:n, :], zs[c0 + c * 128:c0 + (c + n) * 128, 512:768].rearrange("(t p) c -> p t c", p=128), reads=[bZs], writes=[bVs[sl]])
                            P.op("pool", lambda e: e.tensor_copy(out=ub[:, c:c + n, :], in_=vst[sl][:, 0:n, :]), reads=[bVs[sl]], writes=[bUb])
                        kt_ = [G_("ktab%d" % i, [128, 2, 256]) for i in range(2)]; bKt_ = P.bufs(2, "ktab")
                        tm = [G_("tm%d" % i, [128, 256]) for i in range(4)]; bTm = P.bufs(4, "tm")
                        zt_ = [G_("hzt%d" % i, [128, 768]) for i in range(2)]; bZt_ = P.bufs(2, "hzt")
                        yo = [G_("yo%d" % i, [128, 256]) for i in range(2)]; bYo = P.bufs(2, "yo")
                        kq = 0
                        for o in range(2):
                            for s_ in range(NS):
                                sl = kq % 2
                                kq += 1
                                sv = slab[sl][:, 0:NT * 512].rearrange("p (t c) -> p t c", c=512)
                                load_slab(sl, sv, I["dftf" + tag][s_], NT)
                                for j in range(4):
                                    bank = (s_ % 2) * 2 + (j % 2)
                                    cs = (j // 2) * 256
                                    for tc in range(NT):
                                        P.op("pe", lambda e: e.matmul(psb[bank][:, cs:cs + 256], lhsT=sv[:, tc, j * 128:(j + 1) * 128], rhs=ub[:, tc, :], start=(tc == 0), stop=(tc == NT - 1)),
                                             reads=[bSl[sl], bSlB[sl], bUb], writes=[pB[bank]], inc=(tc == NT - 1))
                                for jj in range(2):
                                    fc = 2 * s_ + jj
                                    bank = (s_ % 2) * 2 + jj
                                    ks_ = fc % 2
                                    P.dma("sp", kt_[ks_][:, 0, :], ksp[fc * 128:(fc + 1) * 128, o * 256:(o + 1) * 256], reads=[bKs], writes=[bKt_[ks_]])
                                    P.dma("sp", kt_[ks_][:, 1, :], ksp[(NT + fc) * 128:(NT + fc + 1) * 128, o * 256:(o + 1) * 256], reads=[bKs], writes=[bKt_[ks_]])
                                    Ure = psb[bank][:, 0:256]
                                    Uim = psb[bank][:, 256:512]
                                    Kre = kt_[ks_][:, 0, :]
                                    Kim = kt_[ks_][:, 1, :]
                                    P.op("dve", lambda e: e.tensor_tensor(out=tm[0][:], in0=Ure, in1=Kre, op=ALU.mult), reads=[pB[bank], bKt_[ks_]], writes=[bTm[0]])
                                    P.op("dve", lambda e: e.tensor_tensor(out=tm[1][:], in0=Uim, in1=Kim, op=ALU.mult), reads=[pB[bank], bKt_[ks_]], writes=[bTm[1]])
                                    P.op("pool", lambda e: e.tensor_tensor(out=Yf[:, fc, :], in0=tm[0][:], in1=tm[1][:], op=ALU.subtract), reads=[bTm[0], bTm[1]], writes=[bYf])
                                    P.op("dve", lambda e: e.tensor_tensor(out=tm[2][:], in0=Ure, in1=Kim, op=ALU.mult), reads=[pB[bank], bKt_[ks_]], writes=[bTm[2]])
                                    P.op("dve", lambda e: e.tensor_tensor(out=tm[3][:], in0=Uim, in1=Kre, op=ALU.mult), reads=[pB[bank], bKt_[ks_]], writes=[bTm[3]])
                                    P.op("pool", lambda e: e.tensor_tensor(out=Yf[:, NT + fc, :], in0=tm[2][:], in1=tm[3][:], op=ALU.add), reads=[bTm[2], bTm[3]], writes=[bYf])
                                    if fc == 0:
                                        P.op("dve", lambda e: e.tensor_tensor(out=Yf[0:1, 0, :], in0=Ure[0:1, :], in1=Kre[0:1, :], op=ALU.mult), reads=[pB[bank], bKt_[ks_], bYf], writes=[bYf])
                                        P.op("dve", lambda e: e.tensor_tensor(out=Yf[0:1, NT, :], in0=Uim[0:1, :], in1=Kim[0:1, :], op=ALU.mult), reads=[pB[bank], bKt_[ks_], bYf], writes=[bYf])
                            for s_ in range(NS):
                                sl = kq % 2
                                kq += 1
                                iv = slab[sl][:, 0:NR * 256].rearrange("p (r c) -> p r c", c=256)
                                load_slab(sl, iv, I["dfti" + tag][s_], NR)
                                for tt in range(2 if NT >= 2 else 1):
                                    ti = 2 * s_ + tt
                                    bank = 4 + (ti % 2)
                                    for rc in range(NR):
                                        P.op("pe", lambda e: e.matmul(psb[bank][:, 0:256], lhsT=iv[:, rc, tt * 128:(tt + 1) * 128], rhs=Yf[:, rc, :], start=(rc == 0), stop=(rc == NR - 1)),
                                             reads=[bSl[sl], bSlB[sl], bYf], writes=[pB[bank]], inc=(rc == NR - 1))
                                    zsl = ti % 2
                                    tg = c0 + ti * 128
                                    P.dma("sp", zt_[zsl][:], zs[tg:tg + 128, :], reads=[bZs], writes=[bZt_[zsl]])
                                    u32 = zt_[zsl][:, 512:768] if o == 0 else y1f[:, ti, :]
                                    gate = zt_[zsl][:, o * 256:(o + 1) * 256]
                                    P.op("dve", lambda e: e.tensor_tensor(out=yo[zsl][:], in0=u32, in1=hyb[:, o * 256:(o + 1) * 256], op=ALU.mult), reads=[bZt_[zsl], bY1, bHb], writes=[bYo[zsl]])
                                    P.op("dve", lambda e: e.tensor_tensor(out=yo[zsl][:], in0=psb[bank][:, 0:256], in1=yo[zsl][:], op=ALU.add), reads=[pB[bank], bYo[zsl]], writes=[bYo[zsl]])
                                    if o == 0:
                                        P.op("pool", lambda e: e.tensor_tensor(out=y1f[:, ti, :], in0=yo[zsl][:], in1=gate, op=ALU.mult), reads=[bYo[zsl], bZt_[zsl]], writes=[bY1])
                                        P.op("pool", lambda e: e.tensor_copy(out=ub[:, ti, :], in_=y1f[:, ti, :]), reads=[bY1], writes=[bUb])
                                    else:
                                        P.op("pool", lambda e: e.tensor_tensor(out=yo[zsl][:], in0=yo[zsl][:], in1=gate, op=ALU.mult), reads=[bYo[zsl], bZt_[zsl]], writes=[bYo[zsl]])
                                        P.dma("pool", ymix[tg:tg + 128, 0:256], yo[zsl][:], reads=[bYo[zsl]], writes=[bYm])
                        P.barrier()
                P.barrier()

            hyena(0, L, "L", 0, kspec)
            if not last:
                hyena(1, LC, "C", L, kspecC)
            if os.environ.get("MK_STOP") == "HY":
                break

            with ExitStack() as st:
                F_ = lambda name, shape, dt=F32: sbt(st, name, shape, dt)
                wo = F_("wo", [128, 8, 1024], BF16); bWo = P.buf("wo")
                wst = [F_("wost%d" % i, [128, 1024]) for i in range(2)]; bWs = P.bufs(2, "wost")
                mixg = F_("mixg", [128, 1024]); bMg = P.buf("mixg")
                P.dma("sp", mixg[:], I["mixg_bc"][l], writes=[bMg])
                for kc in range(8):
                    sl = kc % 2
                    P.dma("sp", wst[sl][:], I["w_out"][l, kc * 128:(kc + 1) * 128, :], writes=[bWs[sl]])
                    P.op("pool", lambda e: e.tensor_copy(out=wo[:, kc, :], in_=wst[sl][:]), reads=[bWs[sl]], writes=[bWo])
                yt = [F_("yt%d" % i, [128, 1024]) for i in range(3)]; bYt = P.bufs(3, "yt")
                xt = [F_("oxt%d" % i, [128, 1024]) for i in range(3)]; bXt = P.bufs(3, "oxt")
                yn = [F_("yn%d" % i, [128, 1024], BF16) for i in range(3)]; bYn = P.bufs(3, "yn")
                ynT = [F_("ynT%d" % i, [128, 8, 128], BF16) for i in range(3)]; bYT = P.bufs(3, "ynT")
                xo = [F_("xo%d" % i, [128, 1024]) for i in range(3)]; bXo = P.bufs(3, "xo")
                s4 = [F_("s4%d" % i, [128, 4]) for i in range(3)]; bS4 = P.bufs(3, "s4")
                bXm = P.buf("xm_dram")
                ntile = 32 if last else 34
                for ti in range(ntile):
                    sl = ti % 3
                    w = 0 if ti < 32 else 1
                    tg = ti * 128
                    P.dma("sp", yt[sl][:], ymix[tg:tg + 128, :], reads=[bYm], writes=[bYt[sl]])
                    P.dma("sp", xt[sl][:], xin_ap(l, tg, 128), writes=[bXt[sl]])
                    for g in range(4):
                        P.op("act", lambda e: e.activation(out=junk2[:, 0:256], in_=yt[sl][:, g * 256:(g + 1) * 256], func=AF.Square, accum_out=s4[sl][:, g:g + 1]),
                             reads=[bYt[sl]], writes=[bJ2, bS4[sl]])
                    P.op("act", lambda e: e.activation(out=s4[sl][:], in_=s4[sl][:], func=AF.Sqrt, bias=epsT[:, 0:1], scale=1.0 / 256), reads=[bS4[sl], bC], writes=[bS4[sl]])
                    P.op("dve", lambda e: e.reciprocal(out=s4[sl][:], in_=s4[sl][:]), reads=[bS4[sl]], writes=[bS4[sl]])
                    for g in range(4):
                        P.op("dve", lambda e: e.scalar_tensor_tensor(out=yn[sl][:, g * 256:(g + 1) * 256], in0=yt[sl][:, g * 256:(g + 1) * 256], scalar=s4[sl][:, g:g + 1],
                                                                     in1=mixg[:, g * 256:(g + 1) * 256], op0=ALU.mult, op1=ALU.mult),
                             reads=[bYt[sl], bS4[sl], bMg], writes=[bYn[sl]])
                    for half in range(2):
                        bank = 6 + half
                        pT = psb[bank][:].bitcast(BF16)
                        for j in range(4):
                            kc = half * 4 + j
                            P.op("pe", lambda e: e.transpose(out=pT[:, j * 128:(j + 1) * 128], in_=yn[sl][:, kc * 128:(kc + 1) * 128], identity=identb[:]),
                                 reads=[bYn[sl], bC], writes=[pB[bank]], inc=(j == 3))
                        P.op("act", lambda e: e.activation(out=ynT[sl][:, half * 4:(half + 1) * 4, :], in_=pT[:, 0:512].rearrange("p (a b) -> p a b", a=4), func=AF.Identity),
                             reads=[pB[bank]], writes=[bYT[sl]])
                    for half in range(2):
                        for kc in range(8):
                            P.op("pe", lambda e: e.matmul(psb[half][:, :], lhsT=ynT[sl][:, kc, :], rhs=wo[:, kc, half * 512:(half + 1) * 512], start=(kc == 0), stop=(kc == 7)),
                                 reads=[bYT[sl], bWo], writes=[pB[half]], inc=(kc == 7))
                        P.op("dve", lambda e: e.tensor_tensor(out=xo[sl][:, half * 512:(half + 1) * 512], in0=psb[half][:, :], in1=gbc[:, w, half * 512:(half + 1) * 512], op=ALU.mult),
                             reads=[pB[half], bGbc], writes=[bXo[sl]])
                        P.op("pool", lambda e: e.tensor_tensor(out=xo[sl][:, half * 512:(half + 1) * 512], in0=xo[sl][:, half * 512:(half + 1) * 512], in1=xt[sl][:, half * 512:(half + 1) * 512], op=ALU.add),
                             reads=[bXo[sl], bXt[sl]], writes=[bXo[sl]])
                    P.dma("pool", xm[tg:tg + 128, :], xo[sl][:], reads=[bXo[sl]], writes=[bXm])
                P.barrier()
            if os.environ.get("MK_STOP") == "O":
                break
            with ExitStack() as st:
                F_ = lambda name, shape, dt=F32: sbt(st, name, shape, dt)
                NTL = 31 if last else 32
                NSL = 32 * 512
                BIG = 32768.0
                ntile = 32 if last else 34
                sel_all = F_("sel_all", [128, 34, 16]); g_all = F_("g_all", [128, 34, 16]); bSel = P.buf("sel_all"); bGa = P.buf("g_all")
                slotA_i = F_("slotA_i", [128, 34], I32); slotB_i = F_("slotB_i", [128, 34], I32); gA = F_("gA", [128, 34]); gB = F_("gB", [128, 34])
                widx_i = F_("widx_i", [128, 32], I32); bSlots = P.buf("slots")
                tokidx = F_("tokidx", [128, 34], I32); bTok = P.buf("tokidx")
                P.dma("sp", tokidx[:], I["tokidx"], writes=[bTok])
                P.op("pool", lambda e: e.memset(sel_all[:], 0.0), writes=[bSel])
                P.op("pool", lambda e: e.memset(g_all[:], 0.0), writes=[bGa])
                bTbl = P.buf("tbl_dram"); bHfD = P.buf("hfD_dram"); bYp = P.buf("ypair_dram")
                P.dma("sp", tbl, I["tblfill"], writes=[bTbl])
                with ExitStack() as s1:
                    G_ = lambda name, shape, dt=F32: sbt(s1, name, shape, dt)
                    wr = G_("wr", [128, 8, 16]); rbb = G_("rbb", [128, 16]); bMc = P.buf("mconst")
                    P.dma("sp", wr[:], I["w_router"].rearrange("(c p) e -> p c e", p=128), writes=[bMc])
                    P.dma("sp", rbb[:], I["rb_bc"], writes=[bMc])
                    bc2 = G_("bc2", [128, 4, 1024]); bBc2 = P.buf("bc2")
                    diag = [G_("mdiag%d" % i, [128, 128]) for i in range(2)]; bDiag = P.bufs(2, "mdiag")
                    k = 0
                    for gi, (srct, off, w) in enumerate(((modT, 24, 0), (modT, 24, 1), (gain2, 0, 0), (gain2, 0, 1))):
                        for half in range(2):
                            bank = 1 + (k % 2)
                            for j in range(4):
                                dc = half * 4 + j
                                dsl = (k * 4 + j) % 2
                                P.op("dve", lambda e: e.tensor_scalar(out=diag[dsl][:], in0=ident[:], scalar1=srct[:, off + dc, w:w + 1], scalar2=None, op0=ALU.mult),
                                     reads=[bMod, bC], writes=[bDiag[dsl]])
                                P.op("pe", lambda e: e.matmul(psb[bank][:, j * 128:(j + 1) * 128], lhsT=onesf[:], rhs=diag[dsl][:], start=(j == 0), stop=True),
                                     reads=[bDiag[dsl], bC], writes=[pB[bank]], inc=True)
                            P.op("act", lambda e: e.activation(out=bc2[:, gi, half * 512:(half + 1) * 512], in_=psb[bank][:], func=AF.Identity),
                                 reads=[pB[bank]], writes=[bBc2])
                            k += 1
                    mx = [G_("mx%d" % i, [128, 1024]) for i in range(4)]; bMx = P.bufs(4, "mx")
                    xn4 = G_("xn4", [128, 4, 1024]); bXn = P.bufs(4, "xn4")
                    hf32 = [G_("hf32_%d" % i, [128, 512]) for i in range(2)]; bH32 = P.bufs(2, "hf32")
                    htm = [G_("htm%d" % i, [128, 1024]) for i in range(4)]; bHtm = P.bufs(4, "htm")
                    hfb = [G_("hfb%d" % i, [128, 1024], BF16) for i in range(4)]; bHfb = P.bufs(4, "hfb")
                    ms = [G_("ms%d" % i, [128, 1]) for i in range(4)]; bMs = P.bufs(4, "ms")
                    sg = G_("sg", [128, 4, 16]); sl_ = G_("selv", [128, 4, 16]); p6 = G_("p6", [128, 16, 6]); gs = G_("gs", [128, 16]); gmx = G_("gmx", [128, 4])
                    isb = G_("isb", [128, 16]); thr = G_("thr", [128, 16]); msk = G_("msk", [128, 4, 16]); den = G_("den", [128, 4]); bRt = P.buf("router")
                    mti = 0
                    for bidx, (t0, nt, w) in enumerate(BLOCKS):
                        if last and w == 1:
                            continue
                        nb = nt * 128
                        T0 = t0 // 128
                        for t in range(nt):
                            sl = mti % 4
                            mti += 1
                            P.dma("sp", mx[sl][:], xm[t0 + t * 128:t0 + (t + 1) * 128, :], reads=[bXm], writes=[bMx[sl]])
                            P.op("act", lambda e: e.activation(out=junk2[:], in_=mx[sl][:], func=AF.Square, accum_out=ms[sl][:]), reads=[bMx[sl]], writes=[bJ2, bMs[sl]])
                            P.op("act", lambda e: e.activation(out=ms[sl][:], in_=ms[sl][:], func=AF.Sqrt, bias=epsT[:, 0:1], scale=1.0 / D), reads=[bMs[sl], bC], writes=[bMs[sl]])
                            P.op("dve", lambda e: e.reciprocal(out=ms[sl][:], in_=ms[sl][:]), reads=[bMs[sl]], writes=[bMs[sl]])
                            P.op("dve", lambda e: e.tensor_scalar(out=xn4[:, t, :], in0=mx[sl][:], scalar1=ms[sl][:, 0:1], scalar2=None, op0=ALU.mult), reads=[bMx[sl], bMs[sl]], writes=[bXn[t]])
                            P.op("dve", lambda e: e.tensor_tensor(out=htm[sl][:], in0=xn4[:, t, :], in1=bc2[:, 2 + w, :], op=ALU.mult), reads=[bXn[t], bBc2], writes=[bHtm[sl]])
                            P.op("pool", lambda e: e.tensor_tensor(out=hfb[sl][:], in0=htm[sl][:], in1=bc2[:, w, :], op=ALU.add), reads=[bHtm[sl], bBc2], writes=[bHfb[sl]])
                            P.dma("pool", hfD[t0 + t * 128:t0 + (t + 1) * 128, :], hfb[sl][:], reads=[bHfb[sl]], writes=[bHfD])
                        def m1_T(dc):
                            bank = 6 + dc % 2
                            for t in range(nt):
                                P.op("pe", lambda e: e.transpose(out=psb[bank][:, t * 128:(t + 1) * 128], in_=xn4[:, t, dc * 128:(dc + 1) * 128], identity=ident[:]),
                                     reads=[bXn[t], bC], writes=[pB[bank]], inc=(t == nt - 1))
                            P.op("act", lambda e: e.activation(out=hf32[dc % 2][:, :nb], in_=psb[bank][:, :nb], func=AF.Identity, bias=modT[:, 24 + dc, w:w + 1], scale=gain2[:, dc, w:w + 1]),
                                 reads=[pB[bank], bMod], writes=[bH32[dc % 2]])

                        def m1_R(dc):
                            for t in range(nt):
                                P.op("pe", lambda e: e.matmul(psb[5][:, t * 16:(t + 1) * 16], lhsT=hf32[dc % 2][:, t * 128:(t + 1) * 128], rhs=wr[:, dc, :], start=(dc == 0 and t == 0), stop=(dc == 7)),
                                     reads=[bH32[dc % 2], bMc], writes=[pB[5]], inc=(t == nt - 1))

                        m1_T(0)
                        for dc in range(8):
                            if dc + 1 < 8:
                                m1_T(dc + 1)
                            m1_R(dc)
                        G = nt * 4
                        P.op("act", lambda e: e.activation(out=sg[:, 0:nt, :], in_=psb[5][:, 0:nt * 16].rearrange("p (t e) -> p t e", e=16), func=AF.Sigmoid), reads=[pB[5]], writes=[bRt])
                        P.op("dve", lambda e: e.tensor_tensor(out=sl_[:, 0:nt, :], in0=sg[:, 0:nt, :], in1=rbb[:].unsqueeze(1).to_broadcast([128, nt, 16]), op=ALU.add), reads=[bRt, bMc], writes=[bRt])
                        sv = sl_[:, 0:nt, :].rearrange("p t (g k) -> p (t g) k", k=4)
                        for (opx, dst) in ((ALU.add, gs), (ALU.min, thr)):
                            P.op("dve", lambda e: e.tensor_tensor(out=p6[:, 0:G, 0:2], in0=sv[:, :, 0:2], in1=sv[:, :, 2:4], op=opx), reads=[bRt], writes=[bRt])
                            P.op("dve", lambda e: e.tensor_tensor(out=p6[:, 0:G, 2:5], in0=sv[:, :, 0:3], in1=sv[:, :, 1:4], op=opx), reads=[bRt], writes=[bRt])
                            P.op("dve", lambda e: e.tensor_tensor(out=p6[:, 0:G, 5:6], in0=sv[:, :, 0:1], in1=sv[:, :, 3:4], op=opx), reads=[bRt], writes=[bRt])
                            P.op("dve", lambda e: e.tensor_reduce(out=dst[:, 0:G], in_=p6[:, 0:G, :], axis=AX.X, op=ALU.max), reads=[bRt], writes=[bRt])
                        P.op("dve", lambda e: e.tensor_reduce(out=gmx[:, 0:nt], in_=gs[:, 0:G].rearrange("p (t g) -> p t g", g=4), axis=AX.X, op=ALU.max), reads=[bRt], writes=[bRt])
                        P.op("dve", lambda e: e.tensor_tensor(out=isb[:, 0:G].rearrange("p (t g) -> p t g", g=4), in0=gs[:, 0:G].rearrange("p (t g) -> p t g", g=4),
                                                               in1=gmx[:, 0:nt].unsqueeze(2).to_broadcast([128, nt, 4]), op=ALU.is_ge), reads=[bRt], writes=[bRt])
                        mv = msk[:, 0:nt, :].rearrange("p t (g k) -> p (t g) k", k=4)
                        P.op("dve", lambda e: e.tensor_tensor(out=mv, in0=sv, in1=thr[:, 0:G].unsqueeze(2).to_broadcast([128, G, 4]), op=ALU.is_ge), reads=[bRt], writes=[bRt])
                        P.op("dve", lambda e: e.tensor_tensor(out=mv, in0=mv, in1=isb[:, 0:G].unsqueeze(2).to_broadcast([128, G, 4]), op=ALU.mult), reads=[bRt], writes=[bRt])
                        P.op("dve", lambda e: e.tensor_copy(out=sel_all[:, T0:T0 + nt, :], in_=msk[:, 0:nt, :]), reads=[bRt], writes=[bSel])
                        P.op("dve", lambda e: e.tensor_tensor(out=msk[:, 0:nt, :], in0=msk[:, 0:nt, :], in1=sg[:, 0:nt, :], op=ALU.mult), reads=[bRt], writes=[bRt])
                        P.op("dve", lambda e: e.tensor_reduce(out=den[:, 0:nt], in_=msk[:, 0:nt, :], axis=AX.X, op=ALU.add), reads=[bRt], writes=[bRt])
                        P.op("dve", lambda e: e.reciprocal(out=den[:, 0:nt], in_=den[:, 0:nt]), reads=[bRt], writes=[bRt])
                        P.op("dve", lambda e: e.tensor_tensor(out=g_all[:, T0:T0 + nt, :], in0=msk[:, 0:nt, :], in1=den[:, 0:nt].unsqueeze(2).to_broadcast([128, nt, 16]), op=ALU.mult), reads=[bRt], writes=[bGa])
                    P.barrier()
                with ExitStack() as s2:
                    G_ = lambda name, shape, dt=F32: sbt(s2, name, shape, dt)
                    tri = G_("tri", [128, 128]); k9 = G_("k9", [128, 16, 9]); k32 = G_("k32", [128, 32, 16]); pidx = G_("pidx", [128, 1]); bK = P.buf("sortconst")
                    P.dma("sp", tri[:], I["tri"], writes=[bK])
                    P.dma("sp", k9[:], I["k9"], writes=[bK])
                    P.dma("sp", k32[:], I["k32"], writes=[bK])
                    P.dma("sp", pidx[:], I["pidx"], writes=[bK])
                    cs = G_("cs", [128, 35, 16]); bCs = P.buf("cs")
                    rk = G_("rk", [128, 34, 16]); slot = G_("slot", [128, 34, 16]); s3 = G_("s3", [128, 34, 16]); bSm = P.buf("sortmath")
                    cnt = G_("cnt", [128, 16]); c9 = G_("c9", [128, 16, 9]); nte = G_("nte", [128, 16]); padc = G_("padc", [128, 16]); offend = G_("offend", [128, 16]); offm = G_("offm", [128, 16])
                    c32 = G_("c32", [128, 32, 16]); ek = G_("ek", [128, 32]); sA = G_("sA", [128, 34]); sB = G_("sB", [128, 34]); gsum = G_("gsum", [128, 34])
                    P.op("dve", lambda e: e.memset(cs[:, 0, :], 0.0), writes=[bCs])
                    for t in range(1, 35):
                        P.op("dve", lambda e: e.tensor_tensor(out=cs[:, t, :], in0=cs[:, t - 1, :], in1=sel_all[:, t - 1, :], op=ALU.add), reads=[bSel, bCs], writes=[bCs])
                    fl = lambda ap: ap.rearrange("p t e -> p (t e)")
                    P.op("pe", lambda e: e.matmul(psb[0][:, 0:512], lhsT=tri[:], rhs=fl(sel_all[:, 0:32, :]), start=True, stop=False), reads=[bK, bSel], writes=[pB[0]], inc=False)
                    P.op("pe", lambda e: e.matmul(psb[0][:, 0:512], lhsT=onesf[:], rhs=fl(cs[:, 0:32, :]), start=False, stop=True), reads=[bC, bCs], writes=[pB[0]])
                    P.op("pe", lambda e: e.matmul(psb[1][:, 0:32], lhsT=tri[:], rhs=fl(sel_all[:, 32:34, :]), start=True, stop=False), reads=[bK, bSel], writes=[pB[1]], inc=False)
                    P.op("pe", lambda e: e.matmul(psb[1][:, 0:32], lhsT=onesf[:], rhs=fl(cs[:, 32:34, :]), start=False, stop=True), reads=[bC, bCs], writes=[pB[1]])
                    P.op("pe", lambda e: e.matmul(psb[2][:, 0:16], lhsT=onesf[:], rhs=cs[:, 34, :], start=True, stop=True), reads=[bC, bCs], writes=[pB[2]])
                    P.op("act", lambda e: e.activation(out=fl(rk[:, 0:32, :]), in_=psb[0][:, 0:512], func=AF.Identity), reads=[pB[0]], writes=[bSm])
                    P.op("act", lambda e: e.activation(out=fl(rk[:, 32:34, :]), in_=psb[1][:, 0:32], func=AF.Identity), reads=[pB[1]], writes=[bSm])
                    P.op("act", lambda e: e.activation(out=cnt[:], in_=psb[2][:, 0:16], func=AF.Identity), reads=[pB[2]], writes=[bSm])
                    D_ = lambda fn, rd=(): P.op("dve", fn, reads=[bSm, bK, bSel, bGa] + list(rd), writes=[bSm])
                    D_(lambda e: e.tensor_tensor(out=c9[:], in0=cnt[:].unsqueeze(2).to_broadcast([128, 16, 9]), in1=k9[:], op=ALU.is_gt))
                    D_(lambda e: e.tensor_reduce(out=nte[:], in_=c9[:], axis=AX.X, op=ALU.add))
                    D_(lambda e: e.tensor_scalar(out=padc[:], in0=nte[:], scalar1=512.0, scalar2=None, op0=ALU.mult))
                    D_(lambda e: e.tensor_copy(out=offend[:, 0:1], in_=padc[:, 0:1]))
                    for ex in range(1, 16):
                        D_(lambda e: e.tensor_tensor(out=offend[:, ex:ex + 1], in0=offend[:, ex - 1:ex], in1=padc[:, ex:ex + 1], op=ALU.add))
                    D_(lambda e: e.tensor_tensor(out=offm[:], in0=offend[:], in1=padc[:], op=ALU.subtract))
                    D_(lambda e: e.tensor_tensor(out=s3[:], in0=rk[:], in1=offm[:].unsqueeze(1).to_broadcast([128, 34, 16]), op=ALU.add))
                    D_(lambda e: e.tensor_tensor(out=s3[:], in0=s3[:], in1=sel_all[:], op=ALU.mult))
                    D_(lambda e: e.tensor_reduce(out=sB[:], in_=s3[:], axis=AX.X, op=ALU.max))
                    D_(lambda e: e.tensor_scalar(out=slot[:], in0=sel_all[:], scalar1=-BIG, scalar2=BIG, op0=ALU.mult, op1=ALU.add))
                    D_(lambda e: e.tensor_tensor(out=slot[:], in0=slot[:], in1=s3[:], op=ALU.add))
                    D_(lambda e: e.tensor_reduce(out=sA[:], in_=slot[:], axis=AX.X, op=ALU.min))
                    D_(lambda e: e.tensor_tensor(out=slot[:], in0=slot[:], in1=sA[:].unsqueeze(2).to_broadcast([128, 34, 16]), op=ALU.is_equal))
                    D_(lambda e: e.tensor_tensor(out=slot[:], in0=slot[:], in1=g_all[:], op=ALU.mult))
                    P.op("dve", lambda e: e.tensor_reduce(out=gA[:], in_=slot[:], axis=AX.X, op=ALU.add), reads=[bSm], writes=[bSlots])
                    D_(lambda e: e.tensor_reduce(out=gsum[:], in_=g_all[:], axis=AX.X, op=ALU.add))
                    P.op("dve", lambda e: e.tensor_tensor(out=gB[:], in0=gsum[:], in1=gA[:], op=ALU.subtract), reads=[bSm, bSlots], writes=[bSlots])
                    P.op("dve", lambda e: e.tensor_copy(out=slotA_i[:], in_=sA[:]), reads=[bSm], writes=[bSlots])
                    P.op("dve", lambda e: e.tensor_copy(out=slotB_i[:], in_=sB[:]), reads=[bSm], writes=[bSlots])
                    D_(lambda e: e.tensor_tensor(out=c32[:], in0=k32[:], in1=offend[:].unsqueeze(1).to_broadcast([128, 32, 16]), op=ALU.is_ge))
                    D_(lambda e: e.tensor_reduce(out=ek[:], in_=c32[:], axis=AX.X, op=ALU.add))
                    D_(lambda e: e.tensor_scalar(out=ek[:], in0=ek[:], scalar1=15.0, scalar2=None, op0=ALU.min))
                    D_(lambda e: e.tensor_scalar(out=ek[:], in0=ek[:], scalar1=128.0, scalar2=pidx[:, 0:1], op0=ALU.mult, op1=ALU.add))
                    D_(lambda e: e.tensor_scalar(out=ek[:], in0=ek[:], scalar1=float(l * 2048), scalar2=None, op0=ALU.add))
                    P.op("dve", lambda e: e.tensor_copy(out=widx_i[:], in_=ek[:]), reads=[bSm], writes=[bSlots])
                    for t in range(ntile):
                        for si in (slotA_i, slotB_i):
                            P.idma(tbl[:, :], tokidx[:, t:t + 1], bass.IndirectOffsetOnAxis(ap=si[:, t:t + 1], axis=0), None, NSL - 1, reads=[bSlots, bTok, bTbl], writes=[])
                    if "dbg_sort" in dbg:
                        P.dma("sp", dbg_sort[:, 0:34], sA[:], reads=[bSm])
                        P.dma("sp", dbg_sort[:, 34:68], sB[:], reads=[bSm])
                        P.dma("sp", dbg_sort[:, 68:102], gA[:], reads=[bSlots])
                        P.dma("sp", dbg_sort[:, 102:136], gB[:], reads=[bSlots])
                        P.dma("sp", dbg_sort[:, 136:168], ek[:], reads=[bSm])
                        P.dma("sp", dbg_sort[:, 168:184], cnt[:], reads=[bSm])
                    P.barrier()
                with ExitStack() as s3_:
                    G_ = lambda name, shape, dt=F32: sbt(s3_, name, shape, dt)
                    stg = [G_("stg%d" % i, [128, 4096]) for i in range(3)]; bStg = P.bufs(3, "stg")
                    wgb = [G_("wgb%d" % i, [128, 8, 4, 128], BF16) for i in range(2)]; bWg = P.bufs(2, "wgb"); bWgD = P.bufs(2, "wgbD"); bWgP = P.bufs(2, "wgbP")
                    wub = [G_("wub%d" % i, [128, 8, 4, 128], BF16) for i in range(2)]; bWu = P.bufs(2, "wub"); bWuP = P.bufs(2, "wubP")
                    wdb = [G_("wdb%d" % i, [128, 4, 1024], BF16) for i in range(2)]; bWdA = P.bufs(2, "wdbA"); bWdB = P.bufs(2, "wdbB")
                    idxk = [G_("idxk%d" % i, [128, 4], I32) for i in range(2)]; bIdx = P.bufs(2, "idxk")
                    xg = [G_("xg%d" % i, [128, 4, 1024], BF16) for i in range(2)]; bXg = [P.bufs(4, "xg%d_" % i) for i in range(2)]
                    hT_ = [G_("hTm%d" % i, [128, 8, 512], BF16) for i in range(2)]; bHT = [P.bufs(4, "hTm%d_" % i) for i in range(2)]
                    aT = [G_("aT%d" % i, [128, 4, 512], BF16) for i in range(2)]; bAT = P.bufs(2, "aT")
                    sgu = [G_("sgu%d" % i, [128, 512]) for i in range(2)]; bSg = P.bufs(2, "sgu")
                    yp = [G_("yp%d" % i, [128, 1024]) for i in range(2)]; bYpA = P.bufs(2, "ypA"); bYpB = P.bufs(2, "ypB")
                    for i in range(2):
                        P.op("pool", lambda e: e.memset(xg[i][:], 0.0), writes=bXg[i])
                    wviews = (I["exp_w_gate"].rearrange("l e (p j) f -> (l e p) (j f)", j=8),
                              I["exp_w_up"].rearrange("l e (p j) f -> (l e p) (j f)", j=8),
                              I["exp_w_down"].rearrange("l e (p j) d -> (l e p) (j d)", j=4))

                    def prefetch(k):
                        ks = k % 2
                        P.dma("sp", idxk[ks][:], tbl[k * 512:(k + 1) * 512, :].rearrange("(p j) o -> p (j o)", p=128), reads=[bTbl], writes=[bIdx[ks]])
                        for j in range(4):
                            P.idma(xg[ks][:, j, :], hfD[:, :], None, bass.IndirectOffsetOnAxis(ap=idxk[ks][:, j:j + 1], axis=0), NTOK - 1, reads=[bIdx[ks], bHfD], writes=[bXg[ks][j]])
                        for m in range(3):
                            P.idma(stg[m][:], wviews[m], None, bass.IndirectOffsetOnAxis(ap=widx_i[:, k:k + 1], axis=0), 4095, reads=[bSlots], writes=[bStg[m]])

                    def casts(k):
                        ks = k % 2
                        gv = stg[0][:].rearrange("p (jd m jf) -> p jd m jf", jd=8, jf=4)
                        uv = stg[1][:].rearrange("p (jd m jf) -> p jd m jf", jd=8, jf=4)
                        dv = stg[2][:].rearrange("p (jf d) -> p jf d", jf=4)
                        for jf in (0, 1):
                            P.op("act", lambda e: e.activation(out=wgb[ks][:, :, jf, :], in_=gv[:, :, :, jf], func=AF.Identity), reads=[bStg[0]], writes=[bWg[ks]])
                        P.op("dve", lambda e: e.tensor_copy(out=wgb[ks][:, :, 2, :], in_=gv[:, :, :, 2]), reads=[bStg[0]], writes=[bWgD[ks]])
                        P.op("pool", lambda e: e.tensor_copy(out=wgb[ks][:, :, 3, :], in_=gv[:, :, :, 3]), reads=[bStg[0]], writes=[bWgP[ks]])
                        for jf in (0, 1, 2):
                            P.op("dve", lambda e: e.tensor_copy(out=wub[ks][:, :, jf, :], in_=uv[:, :, :, jf]), reads=[bStg[1]], writes=[bWu[ks]])
                        P.op("pool", lambda e: e.tensor_copy(out=wub[ks][:, :, 3, :], in_=uv[:, :, :, 3]), reads=[bStg[1]], writes=[bWuP[ks]])
                        P.op("act", lambda e: e.activation(out=wdb[ks][:, 0:2, :], in_=dv[:, 0:2, :], func=AF.Identity), reads=[bStg[2]], writes=[bWdA[ks]])
                        P.op("dve", lambda e: e.tensor_copy(out=wdb[ks][:, 2:4, :], in_=dv[:, 2:4, :]), reads=[bStg[2]], writes=[bWdB[ks]])

                    prefetch(0)
                    tcount = 0
                    for k in range(NTL):
                        ks = k % 2
                        for s in range(4):
                            bank = 4 + (tcount % 2)
                            pT = psb[bank][:].bitcast(BF16)
                            xv = xg[ks][:, s, :].rearrange("p (m j) -> p j m", j=8)
                            for jd in range(8):
                                P.op("pe", lambda e: e.transpose(out=pT[:, jd * 128:(jd + 1) * 128], in_=xv[:, jd, :], identity=identb[:]),
                                     reads=[bXg[ks][s], bC], writes=[pB[bank]], inc=(jd == 7))
                            dst = hT_[ks][:, :, s * 128:(s + 1) * 128]
                            src = pT[:, 0:1024].rearrange("p (j m) -> p j m", j=8)
                            if tcount % 2 == 0:
                                P.op("act", lambda e: e.activation(out=dst, in_=src, func=AF.Identity), reads=[pB[bank]], writes=[bHT[ks][s]])
                            else:
                                P.op("dve", lambda e: e.tensor_copy(out=dst, in_=src), reads=[pB[bank]], writes=[bHT[ks][s]])
                            tcount += 1
                        casts(k)
                        if k + 1 < NTL:
                            prefetch(k + 1)
                        for jf in range(4):
                            pg = (jf % 2) * 2
                            for (bank, wt, bw) in ((pg, wgb, [bWg[ks], bWgD[ks], bWgP[ks]]), (pg + 1, wub, [bWu[ks], bWuP[ks]])):
                                for jd in range(8):
                                    P.op("pe", lambda e: e.matmul(psb[bank][:, :], lhsT=wt[ks][:, jd, jf, :], rhs=hT_[ks][:, jd, :], start=(jd == 0), stop=(jd == 7)),
                                         reads=bw + bHT[ks], writes=[pB[bank]], inc=(jd == 7))
                            ssl = jf % 2
                            P.op("act", lambda e: e.activation(out=sgu[ssl][:], in_=psb[pg][:, :], func=AF.Silu), reads=[pB[pg]], writes=[bSg[ssl]])
                            P.op("dve", lambda e: e.tensor_tensor(out=aT[ks][:, jf, :], in0=psb[pg + 1][:, :], in1=sgu[ssl][:], op=ALU.mult), reads=[pB[pg + 1], bSg[ssl]], writes=[bAT[ks]])
                        for s in range(4):
                            ys = s % 2
                            b0 = 6 if s % 2 == 0 else 0
                            for dh in range(2):
                                bank = b0 + dh
                                for jf in range(4):
                                    P.op("pe", lambda e: e.matmul(psb[bank][:, :], lhsT=aT[ks][:, jf, s * 128:(s + 1) * 128], rhs=wdb[ks][:, jf, dh * 512:(dh + 1) * 512], start=(jf == 0), stop=(jf == 3)),
                                         reads=[bAT[ks], bWdA[ks], bWdB[ks]], writes=[pB[bank]], inc=(jf == 3))
                            P.op("act", lambda e: e.activation(out=yp[ys][:, 0:512], in_=psb[b0][:, :], func=AF.Identity), reads=[pB[b0]], writes=[bYpA[ys]])
                            P.op("dve", lambda e: e.tensor_copy(out=yp[ys][:, 512:1024], in_=psb[b0 + 1][:, :]), reads=[pB[b0 + 1]], writes=[bYpB[ys]])
                            P.dma("sp", ypair[k * 512:(k + 1) * 512, :].rearrange("(p j) d -> p j d", j=4)[:, s, :], yp[ys][:], reads=[bYpA[ys], bYpB[ys]], writes=[bYp])
                    P.barrier()
                with ExitStack() as s4_:
                    G_ = lambda name, shape, dt=F32: sbt(s4_, name, shape, dt)
                    ya = [G_("ya%d" % i, [128, 1024]) for i in range(3)]; bYa = P.bufs(3, "ya")
                    yb = [G_("yb%d" % i, [128, 1024]) for i in range(3)]; bYb = P.bufs(3, "yb")
                    mx = [G_("emx%d" % i, [128, 1024]) for i in range(3)]; bMx = P.bufs(3, "emx")
                    ms = [G_("ems%d" % i, [128, 1]) for i in range(3)]; bMs = P.bufs(3, "ems")
                    fng = G_("fng", [128, 1024]); bFn = P.buf("fng")
                    if last:
                        P.dma("sp", fng[:], I["fng_bc"], writes=[bFn])
                    for t in range(ntile):
                        sl = t % 3
                        w = 0 if t < 32 else 1
                        tg = t * 128
                        P.idma(ya[sl][:], ypair[:, :], None, bass.IndirectOffsetOnAxis(ap=slotA_i[:, t:t + 1], axis=0), NSL - 1, reads=[bSlots, bYp], writes=[bYa[sl]])
                        P.idma(yb[sl][:], ypair[:, :], None, bass.IndirectOffsetOnAxis(ap=slotB_i[:, t:t + 1], axis=0), NSL - 1, reads=[bSlots, bYp], writes=[bYb[sl]])
                        P.dma("sp", mx[sl][:], xm[tg:tg + 128, :], reads=[bXm], writes=[bMx[sl]])
                        P.op("dve", lambda e: e.tensor_scalar(out=ya[sl][:], in0=ya[sl][:], scalar1=gA[:, t:t + 1], scalar2=None, op0=ALU.mult), reads=[bYa[sl], bSlots], writes=[bYa[sl]])
                        P.op("dve", lambda e: e.scalar_tensor_tensor(out=ya[sl][:], in0=yb[sl][:], scalar=gB[:, t:t + 1], in1=ya[sl][:], op0=ALU.mult, op1=ALU.add), reads=[bYa[sl], bYb[sl], bSlots], writes=[bYa[sl]])
                        P.op("dve", lambda e: e.tensor_tensor(out=ya[sl][:], in0=ya[sl][:], in1=gbc[:, 2 + w, :], op=ALU.mult), reads=[bYa[sl], bGbc], writes=[bYa[sl]])
                        P.op("dve", lambda e: e.tensor_tensor(out=ya[sl][:], in0=ya[sl][:], in1=mx[sl][:], op=ALU.add), reads=[bYa[sl], bMx[sl]], writes=[bYa[sl]])
                        if last:
                            P.op("act", lambda e: e.activation(out=junk2[:], in_=ya[sl][:], func=AF.Square, accum_out=ms[sl][:]), reads=[bYa[sl]], writes=[bJ2, bMs[sl]])
                            P.op("act", lambda e: e.activation(out=ms[sl][:], in_=ms[sl][:], func=AF.Sqrt, bias=epsT[:, 0:1], scale=1.0 / D), reads=[bMs[sl], bC], writes=[bMs[sl]])
                            P.op("dve", lambda e: e.reciprocal(out=ms[sl][:], in_=ms[sl][:]), reads=[bMs[sl]], writes=[bMs[sl]])
                            P.op("dve", lambda e: e.scalar_tensor_tensor(out=ya[sl][:], in0=ya[sl][:], scalar=ms[sl][:, 0:1], in1=fng[:], op0=ALU.mult, op1=ALU.mult), reads=[bYa[sl], bMs[sl], bFn], writes=[bYa[sl]])
                            P.dma("act", out[tg:tg + 128, :], ya[sl][:], reads=[bYa[sl]], writes=[bOut])
                        else:
                            P.dma("act", xs1[tg:tg + 128, :], ya[sl][:], reads=[bYa[sl]], writes=[bXs1])
                    P.barrier()
                P.barrier()
            if os.environ.get("MK_STOP") == "M":
                break
        P.barrier()
    return nc, list(I.keys())


_PROG = {}


def _dt_of(a):
    if a.dtype == np.int32:
        return I32
    return BF16 if a.dtype == ml_dtypes.bfloat16 else F32


def kernel(**inputs):
    inp = {k: np.asarray(v) for k, v in inputs.items()}
    shared = prep_shared(inp)
    debug = tuple(os.environ.get("MK_DEBUG", "").split(",")) if os.environ.get("MK_DEBUG") else ()
    key = (debug, os.environ.get("MK_STOP"))
    if key not in _PROG:
        shapes = {k: (v.shape, _dt_of(v)) for k, v in shared.items()}
        _PROG[key] = build_program(shapes, debug)
    nc, used = _PROG[key]
    x = np.ascontiguousarray(inp["x"], dtype=np.float32)
    ctx = np.ascontiguousarray(inp["ctx"], dtype=np.float32)
    c = np.asarray(inp["c"], np.float32)
    cc = _cols(inp["c_ctx"])
    in_maps = []
    for b in range(8):
        m = {k: shared[k] for k in used}
        m["x"] = x[b]
        m["ctx"] = ctx[b]
        m["ccols"] = np.ascontiguousarray(np.stack([_cols(c[b]), cc], axis=-1))
        in_maps.append(m)
    res = run_bass_kernel_spmd(nc, in_maps, core_ids=list(range(8)))
    if debug:
        kernel.last = res.results
    return np.stack([np.asarray(r["out"], dtype=np.float32) for r in res.results], axis=0)
```

```python
import os
import numpy as np
import ml_dtypes
from contextlib import ExitStack
import concourse.bass as bass
import concourse.mybir as mybir
from concourse.bass_utils import run_bass_kernel_spmd

F32 = mybir.dt.float32
BF16 = mybir.dt.bfloat16
I32 = mybir.dt.int32
AF = mybir.ActivationFunctionType
ALU = mybir.AluOpType
AX = mybir.AxisListType

D = 1024
L = 4096
LC = 256
NTOK = L + LC
DEPTH = 2
EPS = 1e-6
HY_OFF, GM_OFF, CV_OFF, MQ_OFF, MKV_OFF, KR_OFF, IN_COLS = 0, 768, 1280, 1792, 1984, 2112, 2144
NE = 16
FE = 512
PI = float(np.pi)


class Buf:
    __slots__ = ("name", "w", "r")

    def __init__(self, name):
        self.name = name
        self.w = None
        self.r = []


class Eng:
    def __init__(self, name, eng, sem):
        self.name = name
        self.eng = eng
        self.sem = sem
        self.count = 0
        self.waited = {}


class Prog:
    def __init__(self, nc, stack, n_dma_sems=32):
        self.nc = nc
        self.engs = {}
        for name, e in (("pe", nc.tensor), ("act", nc.scalar), ("dve", nc.vector),
                        ("pool", nc.gpsimd), ("sp", nc.sync)):
            sem = stack.enter_context(nc.semaphore("s_" + name))
            self.engs[name] = Eng(name, e, sem)
        self.dma_sems = []
        for i in range(n_dma_sems):
            sem = stack.enter_context(nc.semaphore("s_dma%d" % i))
            self.dma_sems.append([sem, 0, "dma%d" % i])
        self.dma_rr = 0
        self.nbuf = 0

    def buf_id(self):
        self.nbuf += 1
        return self.nbuf

    def buf(self, name=None):
        self.nbuf += 1
        return Buf(name or "b%d" % self.nbuf)

    def bufs(self, n, name="b"):
        return [self.buf("%s%d" % (name, i)) for i in range(n)]

    def _wait(self, E, tok):
        if tok is None:
            return
        key, sem, val = tok
        if key == E.name and (E.name == "pe" or val > E.count):
            return
        if E.waited.get(key, 0) >= val:
            return
        E.eng.wait_ge(sem, val)
        E.waited[key] = val

    def _deps(self, E, reads, writes):
        for b in reads:
            self._wait(E, b.w)
        for b in writes:
            self._wait(E, b.w)
            for t in b.r:
                self._wait(E, t)

    def _commit(self, tok, reads, writes):
        for b in reads:
            b.r.append(tok)
            if len(b.r) > 16:
                latest = {}
                for t in b.r:
                    if t[0] not in latest or latest[t[0]][2] < t[2]:
                        latest[t[0]] = t
                b.r = list(latest.values())
        for b in writes:
            b.w = tok
            b.r = []

    def op(self, en, fn, reads=(), writes=(), inc=True):
        E = self.engs[en]
        self._deps(E, reads, writes)
        ins = fn(E.eng)
        if inc:
            E.count += 1
            ins.then_inc(E.sem, 1)
            tok = (E.name, E.sem, E.count)
        else:
            tok = (E.name, E.sem, E.count + 1)
        self._commit(tok, reads, writes)
        return tok

    def dma(self, en, out, in_, reads=(), writes=(), **kw):
        E = self.engs[en]
        slot = self.dma_sems[self.dma_rr]
        self.dma_rr = (self.dma_rr + 1) % len(self.dma_sems)
        sem, cnt, key = slot
        if cnt > 0:
            self._wait(E, (key, sem, cnt))
        self._deps(E, reads, writes)
        ins = E.eng.dma_start(out=out, in_=in_, **kw)
        cnt += 16
        slot[1] = cnt
        ins.then_inc(sem, 16)
        tok = (key, sem, cnt)
        self._commit(tok, reads, writes)
        return tok

    def idma(self, out, in_, out_off, in_off, bounds, reads=(), writes=(), breg=None):
        E = self.engs["pool"]
        slot = self.dma_sems[self.dma_rr]
        self.dma_rr = (self.dma_rr + 1) % len(self.dma_sems)
        sem, cnt, key = slot
        if cnt > 0:
            self._wait(E, (key, sem, cnt))
        self._deps(E, reads, writes)
        if breg is not None:
            ins = E.eng.indirect_dma_start(out=out, out_offset=out_off, in_=in_, in_offset=in_off, bounds_check=breg, oob_is_err=False)
        else:
            ins = E.eng.indirect_dma_start(out=out, out_offset=out_off, in_=in_, in_offset=in_off)
        cnt += 16
        slot[1] = cnt
        ins.then_inc(sem, 16)
        tok = (key, sem, cnt)
        self._commit(tok, reads, writes)
        return tok

    def barrier(self):
        toks = [(E.name, E.sem, E.count) for E in self.engs.values() if E.count > 0]
        toks += [(k, s, c) for s, c, k in self.dma_sems if c > 0]
        for E in self.engs.values():
            for t in toks:
                self._wait(E, t)


_CONST = {}


def _bf(a):
    return np.ascontiguousarray(a.astype(ml_dtypes.bfloat16))


def _dft_slabs(Lh):
    N = 2 * Lh
    NT = Lh // 128
    t = np.arange(Lh, dtype=np.int64)
    F = np.empty((2 * Lh, Lh), np.float32)
    for r0 in range(0, Lh, 256):
        r = np.arange(r0, r0 + 256, dtype=np.int64)[:, None]
        ang = ((r * t[None, :]) % N).astype(np.float64) * (2 * np.pi / N)
        F[r0:r0 + 256] = np.cos(ang)
        F[Lh + r0:Lh + r0 + 256] = -np.sin(ang)
    F[Lh] = np.where(t % 2 == 0, 1.0, -1.0)
    Fb = F.astype(ml_dtypes.bfloat16)
    del F
    NS = max(NT // 2, 1)
    fwd = np.empty((NS, 128, NT, 512), ml_dtypes.bfloat16)
    for s in range(NS):
        rcs = [2 * s, 2 * s + 1, NT + 2 * s, NT + 2 * s + 1]
        for j, rc in enumerate(rcs):
            blk = Fb[rc * 128:(rc + 1) * 128, :]
            fwd[s, :, :, j * 128:(j + 1) * 128] = blk.T.reshape(NT, 128, 128).transpose(1, 0, 2)
    inv = np.empty((NS, 128, 2 * NT, 256), ml_dtypes.bfloat16)
    for s in range(NS):
        blk = Fb[:, s * 256:(s + 1) * 256]
        inv[s] = blk.reshape(2 * NT, 128, 256).transpose(1, 0, 2)
    rs = np.full((2 * Lh,), 2.0 / N, np.float32)
    rs[0] = 1.0 / N
    rs[Lh] = 1.0 / N
    rowscale = np.ascontiguousarray(rs.reshape(2 * NT, 128).T)
    return fwd, inv, rowscale


def _hy_tables(Lh):
    t = np.linspace(0.0, 1.0, Lh, dtype=np.float32)[:, None]
    w = (2.0 * np.pi * np.arange(Lh, dtype=np.float32)[:, None] / Lh).astype(np.float32)
    f = np.linspace(1e-4, 15, 16, dtype=np.float32)[None, :]
    z = np.concatenate([t, np.cos(f * w), -np.sin(f * w)], axis=-1).astype(np.float32)
    deltas = np.abs(np.linspace(np.log(1e-2) / 1.5, np.log(1e-2) / 0.3, 256, dtype=np.float32))
    decay = np.exp(-t * deltas[None, :]).astype(np.float32)
    NT = Lh // 128
    zT = np.ascontiguousarray(z.T)
    dec = np.ascontiguousarray(decay.reshape(NT, 128, 256).transpose(1, 0, 2))
    return zT, dec


def _consts():
    if _CONST:
        return _CONST
    c = {}
    c["ident"] = np.eye(128, dtype=np.float32)
    c["onesf"] = np.ones((128, 128), np.float32)
    bo = np.zeros((128, 128), np.float32)
    bo[:64, :64] = 1.0 / 64
    bo[64:, 64:] = 1.0 / 64
    c["blk64"] = bo
    row = np.repeat(np.arange(L // 64), 64).astype(np.float32)
    col = np.tile(np.arange(64), L // 64).astype(np.float32)
    inv = (10000.0 ** (-np.arange(8, dtype=np.float32) / 8)).astype(np.float32)
    ang = np.concatenate([row[:, None] * inv, col[:, None] * inv], axis=-1).astype(np.float32)
    c["ropec"] = np.ascontiguousarray(np.cos(ang).T.astype(np.float32))
    c["ropes"] = np.ascontiguousarray(np.sin(ang).T.astype(np.float32))
    sel = np.zeros((16, 16, 128), np.float32)
    for e in range(16):
        sel[e, e, :] = 1.0
    c["sel"] = sel
    c["tri"] = np.triu(np.ones((128, 128), np.float32), k=1)
    c["k9"] = np.ascontiguousarray(np.broadcast_to((512.0 * np.arange(9, dtype=np.float32))[None, None, :], (128, 16, 9)))
    c["k32"] = np.ascontiguousarray(np.broadcast_to((512.0 * np.arange(32, dtype=np.float32))[None, :, None], (128, 32, 16)))
    c["pidx"] = np.arange(128, dtype=np.float32).reshape(128, 1)
    c["tokidx"] = np.ascontiguousarray((np.arange(34, dtype=np.int32)[None, :] * 128 + np.arange(128, dtype=np.int32)[:, None]).astype(np.int32))
    c["tblfill"] = np.zeros((32 * 512, 1), np.int32)
    for Lh, tag in ((L, "L"), (LC, "C")):
        fwd, invs, rsc = _dft_slabs(Lh)
        c["dftf" + tag] = fwd
        c["dfti" + tag] = invs
        c["rsc" + tag] = rsc
        zT, dec = _hy_tables(Lh)
        c["hyz" + tag] = zT
        c["hydec" + tag] = dec
    _CONST.update(c)
    return _CONST


def _cols(v):
    v = np.asarray(v, np.float32)
    return np.ascontiguousarray(v.reshape(-1, 128).T)


def _rep(v):
    v = np.asarray(v, np.float32).reshape(1, -1)
    return np.ascontiguousarray(np.broadcast_to(v, (128, v.shape[1])))


def prep_shared(inp):
    s = {}
    s.update(_consts())
    for k in ("ada_w", "w_in", "w_out", "exp_w_gate", "exp_w_up", "exp_w_down", "hy_f_w1", "hy_f_w2", "hy_f_w3"):
        s[k] = np.ascontiguousarray(inp[k], dtype=np.float32)
    s["w_router"] = np.ascontiguousarray(inp["w_router"], dtype=np.float32)
    s["ada_bT"] = np.stack([_cols(inp["ada_b"][l]) for l in range(DEPTH)])
    s["n1gT"] = np.stack([_cols(inp["norm1_g"][l]) for l in range(DEPTH)])
    s["n2gT"] = np.stack([_cols(inp["norm2_g"][l]) for l in range(DEPTH)])
    s["hsw_bc"] = np.stack([np.stack([_rep(inp["hy_short_w"][l, k]) for k in range(3)], axis=1) for l in range(DEPTH)])
    s["hsb_bc"] = np.stack([_rep(inp["hy_short_b"][l]) for l in range(DEPTH)])
    s["hyb_bc"] = np.stack([_rep(inp["hy_bias"][l].reshape(-1)) for l in range(DEPTH)])
    s["hy_b1c"] = np.ascontiguousarray(np.asarray(inp["hy_f_b1"], np.float32)[:, :, None])
    s["hy_b2c"] = np.ascontiguousarray(np.asarray(inp["hy_f_b2"], np.float32)[:, :, None])
    s["hy_frc"] = np.ascontiguousarray(np.asarray(inp["hy_f_freq"], np.float32)[:, :, None])
    s["gm_lng"] = np.stack([_rep(inp["gm_ln_g"][l]) for l in range(DEPTH)])
    s["gm_lnb"] = np.stack([_rep(inp["gm_ln_b"][l]) for l in range(DEPTH)])
    s["gm_wsT"] = np.ascontiguousarray(np.asarray(inp["gm_ws"], np.float32).transpose(0, 3, 1, 2))
    gb = np.asarray(inp["gm_bs"], np.float32)
    s["gm_bsbc"] = np.ascontiguousarray(np.repeat(gb.transpose(0, 2, 1), 64, axis=2))
    s["cv_w"] = np.ascontiguousarray(np.asarray(inp["cv_dw_w"], np.float32).transpose(0, 2, 1).reshape(DEPTH, 2, 128, 31).transpose(0, 2, 1, 3))
    s["cv_bT"] = np.stack([_cols(inp["cv_dw_b"][l]) for l in range(DEPTH)])
    s["cv_lgT"] = np.stack([_cols(inp["cv_ln_g"][l]) for l in range(DEPTH)])
    s["cv_lbT"] = np.stack([_cols(inp["cv_ln_b"][l]) for l in range(DEPTH)])
    wuq = np.asarray(inp["w_uq"], np.float32).reshape(DEPTH, 192, 4, 96)
    wq_n = np.zeros((DEPTH, 192, 4, 128), np.float32)
    wq_n[:, :, :, 64:128] = wuq[:, :, :, 0:64]
    wq_r = np.zeros((DEPTH, 192, 4, 2, 16), np.float32)
    wq_r[:, :, :, 0, :] = wuq[:, :, :, 64:80]
    wq_r[:, :, :, 1, :] = wuq[:, :, :, 80:96]
    s["wq_n"] = wq_n.reshape(DEPTH, 192, 512)
    s["wq_r"] = wq_r.reshape(DEPTH, 192, 128)
    s["qan"] = np.ascontiguousarray(np.asarray(inp["mla_qa_norm"], np.float32)[:, :, None])
    wukv = np.asarray(inp["w_ukv"], np.float32).reshape(DEPTH, 128, 4, 128)
    wk = np.zeros((DEPTH, 128, 4, 128), np.float32)
    wk[:, :, :, 64:128] = wukv[:, :, :, 0:64]
    s["wk_n"] = wk.reshape(DEPTH, 128, 512)
    s["wv"] = np.ascontiguousarray(wukv[:, :, :, 64:128].reshape(DEPTH, 128, 256))
    s["kvn"] = np.ascontiguousarray(np.asarray(inp["mla_kva_norm"], np.float32)[:, :, None])
    s["mixg_bc"] = np.stack([_rep(inp["mix_norm_g"][l]) for l in range(DEPTH)])
    s["rb_bc"] = _rep(inp["router_bias"])
    s["fng_bc"] = _rep(inp["final_norm_g"])
    return s


def build_program(shared_shapes, debug=()):
    nc = bass.Bass("TRN2", target_bir_lowering=False)
    dbg = set(debug)
    class _Lazy(dict):
        def __missing__(self, k):
            shape, dt = shared_shapes[k]
            v = nc.dram_tensor(k, list(shape), dt, kind="ExternalInput").ap()
            self[k] = v
            return v
    I = _Lazy()
    x_in = nc.dram_tensor("x", [L, D], F32, kind="ExternalInput").ap()
    ctx_in = nc.dram_tensor("ctx", [LC, D], F32, kind="ExternalInput").ap()
    ccols = nc.dram_tensor("ccols", [128, 8, 2], F32, kind="ExternalInput").ap()
    out = nc.dram_tensor("out", [L, D], F32, kind="ExternalOutput").ap()

    def scratch(name, shape, dt):
        kind = "ExternalOutput" if name in dbg else "Internal"
        return nc.dram_tensor(name, list(shape), dt, kind=kind).ap()

    xm = scratch("xm", [NTOK, D], F32)
    xs1 = scratch("xs1", [NTOK, D], F32)
    zs = scratch("zs", [NTOK, 768], F32)
    ymix = scratch("ymix", [NTOK, D], F32)
    glu = scratch("glu", [256, NTOK], BF16)
    qT = scratch("qT", [4, 128, NTOK], BF16)
    kT = scratch("kT", [4, 128, NTOK], BF16)
    vv = scratch("vv", [NTOK, 4 * 65], BF16)
    kspec = scratch("kspec", [2 * L, 512], F32)
    kspecC = scratch("kspecC", [2 * LC, 512], F32)
    dbg_mod = scratch("dbg_mod", [128, 48, 2], F32)
    hfD = scratch("hfD", [NTOK, D], BF16)
    ypair = scratch("ypair", [32 * 512, D], F32)
    tbl = scratch("tbl", [32 * 512, 1], I32)
    dbg_sort = scratch("dbg_sort", [128, 184], F32)
    dbg_hT = scratch("dbg_hT", [128, 8, L + 2], BF16)

    with ExitStack() as top:
        P = Prog(nc, top)
        sbt = lambda st, name, shape, dt: st.enter_context(nc.sbuf_tensor("sb_%s_%d" % (name, P.buf_id()), shape, dt))
        psb = [top.enter_context(nc.psum_tensor("ps%d" % i, [128, 512], F32)) for i in range(8)]
        pB = P.bufs(8, "ps")
        ident = sbt(top, "ident", [128, 128], F32)
        identb = sbt(top, "identb", [128, 128], BF16)
        onesf = sbt(top, "onesf", [128, 128], F32)
        blk64 = sbt(top, "blk64", [128, 128], F32)
        modT = sbt(top, "modT", [128, 48, 2], F32)
        gain1 = sbt(top, "gain1", [128, 8, 2], F32)
        gain2 = sbt(top, "gain2", [128, 8, 2], F32)
        gbc = sbt(top, "gbc", [128, 4, 1024], F32)
        epsT = sbt(top, "epsT", [128, 1], F32)
        junk2 = sbt(top, "junk2", [128, 1024], BF16)
        bJ2 = P.buf("junk2")
        bC = P.buf("consts")
        bOut = P.buf("out_dram")
        bXs1 = P.buf("xs1_dram")
        P.op("pool", lambda e: e.memset(epsT[:], EPS), writes=[bC])
        bMod = P.buf("mod")
        bGbc = P.buf("gbc")
        P.dma("sp", ident[:], I["ident"], writes=[bC])
        P.dma("sp", onesf[:], I["onesf"], writes=[bC])
        P.dma("sp", blk64[:], I["blk64"], writes=[bC])
        P.op("dve", lambda e: e.tensor_copy(out=identb[:], in_=ident[:]), reads=[bC], writes=[bC])

        def xin_ap(l, t0, n):
            if l == 0:
                if t0 < L:
                    return x_in[t0:t0 + n, :]
                return ctx_in[t0 - L:t0 - L + n, :]
            return xs1[t0:t0 + n, :]

        BLOCKS = [(b * 512, 4, 0) for b in range(8)] + [(L, 2, 1)]

        for l in range(DEPTH):
            last = (l == DEPTH - 1)
            with ExitStack() as st:
                aw = [sbt(st, "aw%d" % i, [128, 6144], F32) for i in range(2)]
                bAw = P.bufs(2, "aw")
                scs = sbt(st, "scs", [128, 8, 2], F32)
                abT = sbt(st, "abT", [128, 48], F32)
                n1g = sbt(st, "n1g", [128, 8], F32)
                n2g = sbt(st, "n2g", [128, 8], F32)
                tmp = sbt(st, "tmpA", [128, 8, 2], F32)
                diag = [sbt(st, "diag%d" % i, [128, 128], F32) for i in range(2)]
                bDiag = P.bufs(2, "diag")
                bS = P.buf("scs")
                P.dma("sp", scs[:], ccols, writes=[bS])
                P.dma("sp", abT[:], I["ada_bT"][l], writes=[bS])
                P.dma("sp", n1g[:], I["n1gT"][l], writes=[bS])
                P.dma("sp", n2g[:], I["n2gT"][l], writes=[bS])
                P.op("act", lambda e: e.activation(out=scs[:], in_=scs[:], func=AF.Silu), reads=[bS], writes=[bS])
                for kc in range(8):
                    sl = kc % 2
                    P.dma("sp" if kc % 2 == 0 else "pool", aw[sl][:], I["ada_w"][l, kc * 128:(kc + 1) * 128, :], writes=[bAw[sl]])
                    for n in range(48):
                        P.op("pe", lambda e: e.matmul(psb[0][:, 2 * n:2 * n + 2], lhsT=aw[sl][:, n * 128:(n + 1) * 128],
                                                      rhs=scs[:, kc, :], start=(kc == 0 and n == 0), stop=(kc == 7)),
                             reads=[bAw[sl], bS], writes=[pB[0]], inc=(n == 47))
                P.op("dve", lambda e: e.tensor_tensor(out=modT[:], in0=psb[0][:, 0:96].rearrange("p (a b) -> p a b", b=2),
                                                       in1=abT[:].unsqueeze(2).to_broadcast([128, 48, 2]), op=ALU.add),
                     reads=[pB[0], bS], writes=[bMod])
                for (gain, ng, off) in ((gain1, n1g, 8), (gain2, n2g, 32)):
                    P.op("dve", lambda e: e.tensor_scalar(out=tmp[:], in0=modT[:, off:off + 8, :], scalar1=1.0, scalar2=None, op0=ALU.add),
                         reads=[bMod], writes=[bS])
                    P.op("dve", lambda e: e.tensor_tensor(out=gain[:], in0=tmp[:], in1=ng[:].unsqueeze(2).to_broadcast([128, 8, 2]), op=ALU.mult),
                         reads=[bS], writes=[bMod])
                k = 0
                for gi, (off, w) in enumerate(((16, 0), (16, 1), (40, 0), (40, 1))):
                    for half in range(2):
                        bank = 1 + (k % 2)
                        for j in range(4):
                            dc = half * 4 + j
                            dsl = (k * 4 + j) % 2
                            P.op("dve", lambda e: e.tensor_scalar(out=diag[dsl][:], in0=ident[:], scalar1=modT[:, off + dc, w:w + 1], scalar2=None, op0=ALU.mult),
                                 reads=[bMod, bC], writes=[bDiag[dsl]])
                            P.op("pe", lambda e: e.matmul(psb[bank][:, j * 128:(j + 1) * 128], lhsT=onesf[:], rhs=diag[dsl][:], start=(j == 0), stop=True),
                                 reads=[bDiag[dsl], bC], writes=[pB[bank]], inc=True)
                        P.op("act", lambda e: e.activation(out=gbc[:, gi, half * 512:(half + 1) * 512], in_=psb[bank][:], func=AF.Identity),
                             reads=[pB[bank]], writes=[bGbc])
                        k += 1
                if "dbg_mod" in dbg and l == 0:
                    P.dma("sp", dbg_mod, modT[:], reads=[bMod])
                P.barrier()
            if os.environ.get("MK_STOP") == "A":
                break
            with ExitStack() as st:
                hT = [sbt(st, "hTl", [128, 8, L + 2], BF16), sbt(st, "hTc", [128, 8, LC + 2], BF16)]
                bHl = P.bufs(8, "hTl")
                bHc = P.buf("hTc")
                bPad = P.buf("hTpad")
                wb = sbt(st, "wb", [128, 8, IN_COLS - 768], BF16)
                wh = sbt(st, "wh", [128, 8, 3, 768], BF16)
                bW = P.buf("w_in_b")
                for (tt, n) in ((hT[0], L), (hT[1], LC)):
                    P.op("pool", lambda e: e.memset(tt[:, :, 0:1], 0.0), writes=[bPad])
                    P.op("pool", lambda e: e.memset(tt[:, :, n + 1:n + 2], 0.0), writes=[bPad])

                def hbufs(w, tl, n):
                    if w == 1:
                        return [bHc, bPad]
                    lo = max(tl - 1, 0) // 512
                    hi = min(tl + n, L - 1) // 512
                    return [bHl[i] for i in range(lo, hi + 1)] + [bPad]

                with ExitStack() as s1:
                    xt = [sbt(s1, "xt%d" % i, [128, 1024], F32) for i in range(2)]
                    bXt = P.bufs(2, "xt")
                    xn = [sbt(s1, "xn%d" % i, [128, 4, 1024], BF16) for i in range(2)]
                    bXn = P.bufs(2, "xn")
                    junk = sbt(s1, "junk", [128, 1024], BF16)
                    bJ = P.buf("junk")
                    ssq = [sbt(s1, "ssq%d" % i, [128, 1], F32) for i in range(2)]
                    bSs = P.bufs(2, "ssq")
                    wst = [sbt(s1, "wst%d" % i, [128, IN_COLS], F32) for i in range(2)]
                    bWst = P.bufs(2, "wst")
                    hsw = sbt(s1, "hsw", [128, 3, 768], F32)
                    bHsw = P.buf("hsw")
                    P.dma("pool", hsw[:], I["hsw_bc"][l], writes=[bHsw])
                    for dc in range(8):
                        sl = dc % 2
                        P.dma("pool", wst[sl][:], I["w_in"][l, dc * 128:(dc + 1) * 128, :], writes=[bWst[sl]])
                        P.op("pool", lambda e: e.tensor_copy(out=wb[:, dc, :], in_=wst[sl][:, 768:IN_COLS]), reads=[bWst[sl]], writes=[bW])
                        for k in range(3):
                            P.op("pool", lambda e: e.tensor_tensor(out=wh[:, dc, k, :], in0=wst[sl][:, 0:768], in1=hsw[:, k, :], op=ALU.mult),
                                 reads=[bWst[sl], bHsw], writes=[bW])
                    ti = 0
                    for bi, (t0, nt, w) in enumerate(BLOCKS):
                        if last and w == 1 and False:
                            continue
                        xs_ = bi % 2
                        for t in range(nt):
                            sl = ti % 2
                            ti += 1
                            P.dma("sp", xt[sl][:], xin_ap(l, t0 + t * 128, 128), writes=[bXt[sl]])
                            P.op("act", lambda e: e.activation(out=junk[:], in_=xt[sl][:], func=AF.Square, accum_out=ssq[sl][:]),
                                 reads=[bXt[sl]], writes=[bJ, bSs[sl]])
                            P.op("act", lambda e: e.activation(out=ssq[sl][:], in_=ssq[sl][:], func=AF.Sqrt, bias=epsT[:, 0:1], scale=1.0 / D),
                                 reads=[bSs[sl], bC], writes=[bSs[sl]])
                            P.op("dve", lambda e: e.reciprocal(out=ssq[sl][:], in_=ssq[sl][:]), reads=[bSs[sl]], writes=[bSs[sl]])
                            P.op("dve", lambda e: e.tensor_scalar(out=xn[xs_][:, t, :], in0=xt[sl][:], scalar1=ssq[sl][:, 0:1], scalar2=None, op0=ALU.mult),
                                 reads=[bXt[sl], bSs[sl]], writes=[bXn[xs_]])
                        tl0 = t0 if w == 0 else 0
                        hb = [bHl[bi]] if w == 0 else [bHc]
                        for dc in range(8):
                            bank = 6 + dc % 2
                            pT = psb[bank][:].bitcast(BF16)
                            for t in range(nt):
                                P.op("pe", lambda e: e.transpose(out=pT[:, t * 128:(t + 1) * 128], in_=xn[xs_][:, t, dc * 128:(dc + 1) * 128], identity=identb[:]),
                                     reads=[bXn[xs_], bC], writes=[pB[bank]], inc=(t == nt - 1))
                            dst = hT[w][:, dc, 1 + tl0:1 + tl0 + nt * 128]
                            if dc % 2 == 0:
                                P.op("act", lambda e: e.activation(out=dst, in_=pT[:, 0:nt * 128], func=AF.Identity,
                                                                   bias=modT[:, dc, w:w + 1], scale=gain1[:, dc, w:w + 1]),
                                     reads=[pB[bank], bMod], writes=hb)
                            else:
                                P.op("dve", lambda e: e.tensor_scalar(out=dst, in0=pT[:, 0:nt * 128], scalar1=gain1[:, dc, w:w + 1],
                                                                      scalar2=modT[:, dc, w:w + 1], op0=ALU.mult, op1=ALU.add),
                                     reads=[pB[bank], bMod], writes=hb)
                    if "dbg_hT" in dbg and l == 0:
                        P.dma("sp", dbg_hT, hT[0][:], reads=bHl + [bPad])
                    P.barrier()
                if os.environ.get("MK_STOP") == "B1":
                    break
                with ExitStack() as s2:
                    F_ = lambda name, shape, dt=F32: sbt(s2, name, shape, dt)
                    hsb = F_("hsb", [128, 768]); lng = F_("lng", [128, 256]); lnb = F_("lnb", [128, 256]); bsbc = F_("bsbc", [128, 256])
                    wsT = F_("wsT", [128, 4, 128], BF16)
                    wqn = F_("wqn", [128, 2, 512], BF16); wqr = F_("wqr", [128, 2, 128], BF16)
                    wkn = F_("wkn", [128, 512], BF16); wvb = F_("wvb", [128, 256], BF16)
                    wtmp = F_("wtmp", [128, 640]); gcol = F_("gcol", [128, 3]); wsTf = wtmp[:, 0:512].rearrange("p (g i) -> p g i", g=4)
                    bS2 = P.buf("b2consts")
                    P.dma("sp", hsb[:], I["hsb_bc"][l], writes=[bS2])
                    P.dma("sp", lng[:], I["gm_lng"][l], writes=[bS2])
                    P.dma("sp", lnb[:], I["gm_lnb"][l], writes=[bS2])
                    P.dma("sp", bsbc[:], I["gm_bsbc"][l], writes=[bS2])
                    bWt = P.buf("wtmp")
                    P.dma("sp", wsTf, I["gm_wsT"][l], writes=[bWt])
                    P.op("pool", lambda e: e.tensor_copy(out=wsT[:], in_=wsTf), reads=[bWt], writes=[bS2])
                    P.dma("sp", gcol[:, 0:1], I["qan"][l, 0:128, :], writes=[bS2])
                    P.dma("sp", gcol[0:64, 1:2], I["qan"][l, 128:192, :], writes=[bS2])
                    P.dma("sp", gcol[:, 2:3], I["kvn"][l], writes=[bS2])
                    for rc, rows in ((0, 128), (1, 64)):
                        P.dma("sp", wtmp[0:rows, 0:512], I["wq_n"][l, rc * 128:rc * 128 + rows, :], writes=[bWt])
                        P.dma("sp", wtmp[0:rows, 512:640], I["wq_r"][l, rc * 128:rc * 128 + rows, :], writes=[bWt])
                        P.op("dve", lambda e: e.tensor_scalar(out=wqn[0:rows, rc, :], in0=wtmp[0:rows, 0:512], scalar1=gcol[0:rows, rc:rc + 1], scalar2=None, op0=ALU.mult),
                             reads=[bWt, bS2], writes=[bS2])
                        P.op("dve", lambda e: e.tensor_scalar(out=wqr[0:rows, rc, :], in0=wtmp[0:rows, 512:640], scalar1=gcol[0:rows, rc:rc + 1], scalar2=None, op0=ALU.mult),
                             reads=[bWt, bS2], writes=[bS2])
                    P.dma("sp", wtmp[:, 0:512], I["wk_n"][l], writes=[bWt])
                    P.op("dve", lambda e: e.tensor_scalar(out=wkn[:], in0=wtmp[:, 0:512], scalar1=gcol[:, 2:3], scalar2=None, op0=ALU.mult), reads=[bWt, bS2], writes=[bS2])
                    P.dma("sp", wtmp[:, 0:256], I["wv"][l], writes=[bWt])
                    P.op("dve", lambda e: e.tensor_scalar(out=wvb[:], in0=wtmp[:, 0:256], scalar1=gcol[:, 2:3], scalar2=None, op0=ALU.mult), reads=[bWt, bS2], writes=[bS2])
                    zt0 = F_("zt0", [128, 768]); zt = [zt0, zt0]; bZt0 = P.buf("zt"); bZt = [bZt0, bZt0]
                    zg = [F_("zg%d" % i, [128, 512]) for i in range(2)]; bZg = P.bufs(2, "zg")
                    st1 = [F_("st1%d" % i, [128, 2]) for i in range(2)]; bSt = P.bufs(2, "st1")
                    vc = [F_("vc%d" % i, [128, 256]) for i in range(2)]; bVc = P.bufs(2, "vc")
                    vnb = [F_("vnb%d" % i, [128, 256], BF16) for i in range(2)]; bVn = P.bufs(2, "vnb")
                    ygm = [F_("ygm%d" % i, [128, 256]) for i in range(2)]; bYg = P.bufs(2, "ygm")

                    glt = [F_("glt%d" % i, [128, 512], BF16) for i in range(2)]; bGl = P.bufs(2, "glt")
                    cqb = F_("cqb", [128, 2, 512], BF16); sq0 = F_("sq0", [128, 2, 512]); bCq = P.buf("cqb")
                    sig = sq0[:, 1, :]; bSig = bCq
                    rrep = F_("rrep", [128, 512]); bRr = P.buf("rrep")
                    qt = F_("qt", [128, 4, 512], BF16); bQt = P.buf("qt")
                    kt = F_("kt", [128, 4, 512], BF16); bKt = P.buf("kt")
                    vt = F_("vt", [128, 4, 4, 65], BF16); bVt = P.buf("vt")
                    ckvb = cqb[:, 0, :]; sqk = sq0[:, 0, :]; bCk = bCq
                    rrk = rrep; bRk = bRr
                    rcol = F_("rcol", [128, 4]); bRc = P.buf("rcol")
                    rcs = [F_("ropec", [16, 512]), F_("ropes", [16, 512])]; bRope = P.buf("rope")
                    ra = F_("ra", [16, 512]); rb = F_("rb", [16, 512]); rt1 = F_("rt1", [16, 512]); rt2 = F_("rt2", [16, 512]); bR = P.buf("ropetmp")

                    P.op("pool", lambda e: e.memset(qt[:], 0.0), writes=[bQt])
                    P.op("pool", lambda e: e.memset(kt[:], 0.0), writes=[bKt])
                    P.op("pool", lambda e: e.memset(vt[:], 1.0), writes=[bVt])
                    bZs = P.buf("zs_dram"); bYm = P.buf("ymix_dram"); bGlu = P.buf("glu_dram"); bQd = P.buf("q_dram"); bKd = P.buf("k_dram"); bVd = P.buf("v_dram")

                    def rope_apply(src1, src2, dst1, dst2, nb, do_rope, rd, wr):
                        if do_rope:
                            P.op("dve", lambda e: e.tensor_tensor(out=rt1[:, :nb], in0=src1, in1=rcs[0][:, :nb], op=ALU.mult), reads=rd + [bRope], writes=[bR])
                            P.op("pool", lambda e: e.tensor_tensor(out=rt2[:, :nb], in0=src2, in1=rcs[1][:, :nb], op=ALU.mult), reads=rd + [bRope], writes=[bR])
                            P.op("dve", lambda e: e.tensor_tensor(out=dst1, in0=rt1[:, :nb], in1=rt2[:, :nb], op=ALU.subtract), reads=[bR], writes=wr)
                            P.op("dve", lambda e: e.tensor_tensor(out=rt1[:, :nb], in0=src1, in1=rcs[1][:, :nb], op=ALU.mult), reads=rd + [bRope] + wr, writes=[bR])
                            P.op("pool", lambda e: e.tensor_tensor(out=rt2[:, :nb], in0=src2, in1=rcs[0][:, :nb], op=ALU.mult), reads=rd + [bRope] + wr, writes=[bR])
                            P.op("dve", lambda e: e.tensor_tensor(out=dst2, in0=rt1[:, :nb], in1=rt2[:, :nb], op=ALU.add), reads=[bR], writes=wr)
                        else:
                            P.op("dve", lambda e: e.tensor_copy(out=dst1, in_=src1), reads=rd, writes=wr)
                            P.op("dve", lambda e: e.tensor_copy(out=dst2, in_=src2), reads=rd, writes=wr)

                    tix = 0
                    for bi, (t0, nt, w) in enumerate(BLOCKS):
                        nb = nt * 128
                        tl0 = t0 if w == 0 else 0
                        HT = hT[w]
                        full = not (last and w == 1)
                        hb = hbufs(w, tl0, nb)
                        rhs = lambda dc: HT[:, dc, 1 + tl0:1 + tl0 + nb]
                        if w == 0:
                            P.dma("sp", rcs[0][:, :nb], I["ropec"][:, t0:t0 + nb], writes=[bRope])
                            P.dma("sp", rcs[1][:, :nb], I["ropes"][:, t0:t0 + nb], writes=[bRope])
                        if full:
                            for j in range(2):
                                for (bank, c0) in ((4, 512 + j * 128), (5, 768 + j * 128)):
                                    for dc in range(8):
                                        P.op("pe", lambda e: e.matmul(psb[bank][:, :nb], lhsT=wb[:, dc, c0:c0 + 128], rhs=rhs(dc), start=(dc == 0), stop=(dc == 7)),
                                             reads=hb + [bW], writes=[pB[bank]], inc=(dc == 7))
                                P.op("act", lambda e: e.activation(out=sig[:, :nb], in_=psb[5][:, :nb], func=AF.Sigmoid), reads=[pB[5]], writes=[bSig])
                                P.op("dve", lambda e: e.tensor_tensor(out=glt[j][:, :nb], in0=psb[4][:, :nb], in1=sig[:, :nb], op=ALU.mult),
                                     reads=[pB[4], bSig], writes=[bGl[j]])
                                P.dma("pool", glu[j * 128:(j + 1) * 128, t0:t0 + nb], glt[j][:, :nb], reads=[bGl[j]], writes=[bGlu])
                            for (bank, c0, rows, rc) in ((4, 1024, 128, 0), (5, 1152, 64, 1)):
                                for dc in range(8):
                                    P.op("pe", lambda e: e.matmul(psb[bank][0:rows, :nb], lhsT=wb[:, dc, c0:c0 + rows], rhs=rhs(dc), start=(dc == 0), stop=(dc == 7)),
                                         reads=hb + [bW], writes=[pB[bank]], inc=(dc == 7))
                                P.op("act", lambda e: e.activation(out=cqb[0:rows, rc, :nb], in_=psb[bank][0:rows, :nb], func=AF.Identity), reads=[pB[bank]], writes=[bCq])
                                P.op("act", lambda e: e.activation(out=sq0[0:rows, rc, :nb], in_=psb[bank][0:rows, :nb], func=AF.Square), reads=[pB[bank]], writes=[bCq])
                            P.op("pe", lambda e: e.matmul(psb[6][:, :nb], lhsT=onesf[:, :], rhs=sq0[:, 0, :nb], start=True, stop=False), reads=[bCq, bC], writes=[pB[6]], inc=False)
                            P.op("pe", lambda e: e.matmul(psb[6][:, :nb], lhsT=onesf[0:64, :], rhs=sq0[0:64, 1, :nb], start=False, stop=True), reads=[bCq, bC], writes=[pB[6]])
                            P.op("act", lambda e: e.activation(out=rrep[:, :nb], in_=psb[6][:, :nb], func=AF.Sqrt, bias=epsT[:, 0:1], scale=1.0 / 192), reads=[pB[6], bC], writes=[bRr])
                            P.op("dve", lambda e: e.reciprocal(out=rrep[:, :nb], in_=rrep[:, :nb]), reads=[bRr], writes=[bRr])
                            for h in range(4):
                                P.op("pe", lambda e: e.matmul(psb[7][:, :nb], lhsT=wqn[:, 0, h * 128:(h + 1) * 128], rhs=cqb[:, 0, :nb], start=True, stop=False), reads=[bCq, bS2], writes=[pB[7]], inc=False)
                                P.op("pe", lambda e: e.matmul(psb[7][:, :nb], lhsT=wqn[0:64, 1, h * 128:(h + 1) * 128], rhs=cqb[0:64, 1, :nb], start=False, stop=True), reads=[bCq, bS2], writes=[pB[7]])
                                P.op("dve", lambda e: e.tensor_tensor(out=qt[64:128, h, :nb], in0=psb[7][64:128, :nb], in1=rrep[64:128, :nb], op=ALU.mult),
                                     reads=[pB[7], bRr], writes=[bQt])
                                for (bank, c0) in ((4, h * 32), (5, h * 32 + 16)):
                                    P.op("pe", lambda e: e.matmul(psb[bank][0:16, :nb], lhsT=wqr[:, 0, c0:c0 + 16], rhs=cqb[:, 0, :nb], start=True, stop=False), reads=[bCq, bS2], writes=[pB[bank]], inc=False)
                                    P.op("pe", lambda e: e.matmul(psb[bank][0:16, :nb], lhsT=wqr[0:64, 1, c0:c0 + 16], rhs=cqb[0:64, 1, :nb], start=False, stop=True), reads=[bCq, bS2], writes=[pB[bank]])
                                P.op("dve", lambda e: e.tensor_tensor(out=ra[:, :nb], in0=psb[4][0:16, :nb], in1=rrep[0:16, :nb], op=ALU.mult), reads=[pB[4], bRr], writes=[bR])
                                P.op("dve", lambda e: e.tensor_tensor(out=rb[:, :nb], in0=psb[5][0:16, :nb], in1=rrep[0:16, :nb], op=ALU.mult), reads=[pB[5], bRr], writes=[bR])
                                rope_apply(ra[:, :nb], rb[:, :nb], qt[0:16, h, :nb], qt[32:48, h, :nb], nb, w == 0, [bR], [bQt])
                            P.dma("pool", qT[:, :, t0:t0 + nb].rearrange("h p t -> p h t"), qt[:, :, :nb], reads=[bQt], writes=[bQd])
                        for dc in range(8):
                            P.op("pe", lambda e: e.matmul(psb[6][:, :nb], lhsT=wb[:, dc, 1216:1344], rhs=rhs(dc), start=(dc == 0), stop=(dc == 7)),
                                 reads=hb + [bW], writes=[pB[6]], inc=(dc == 7))
                        P.op("act", lambda e: e.activation(out=ckvb[:, :nb], in_=psb[6][:, :nb], func=AF.Identity), reads=[pB[6]], writes=[bCk])
                        P.op("act", lambda e: e.activation(out=sqk[:, :nb], in_=psb[6][:, :nb], func=AF.Square), reads=[pB[6]], writes=[bCk])
                        P.op("pe", lambda e: e.matmul(psb[7][:, :nb], lhsT=onesf[:, :], rhs=sqk[:, :nb], start=True, stop=True), reads=[bCk, bC], writes=[pB[7]])
                        P.op("act", lambda e: e.activation(out=rrk[:, :nb], in_=psb[7][:, :nb], func=AF.Sqrt, bias=epsT[:, 0:1], scale=1.0 / 128), reads=[pB[7], bC], writes=[bRk])
                        P.op("dve", lambda e: e.reciprocal(out=rrk[:, :nb], in_=rrk[:, :nb]), reads=[bRk], writes=[bRk])
                        for t in range(nt):
                            P.op("pe", lambda e: e.matmul(psb[5][:, 32 + t:33 + t], lhsT=sqk[:, t * 128:(t + 1) * 128], rhs=onesf[:, 0:1], start=(t == 0), stop=True),
                                 reads=[bCk, bC], writes=[pB[5]], inc=(t == nt - 1))
                        P.op("act", lambda e: e.activation(out=rcol[:, 0:nt], in_=psb[5][:, 32:32 + nt], func=AF.Sqrt, bias=epsT[:, 0:1], scale=1.0 / 128), reads=[pB[5], bC], writes=[bRc])
                        P.op("dve", lambda e: e.reciprocal(out=rcol[:, 0:nt], in_=rcol[:, 0:nt]), reads=[bRc], writes=[bRc])
                        for h in range(4):
                            P.op("pe", lambda e: e.matmul(psb[7][:, :nb], lhsT=wkn[:, h * 128:(h + 1) * 128], rhs=ckvb[:, :nb], start=True, stop=True), reads=[bCk, bS2], writes=[pB[7]])
                            P.op("dve", lambda e: e.tensor_tensor(out=kt[64:128, h, :nb], in0=psb[7][64:128, :nb], in1=rrk[64:128, :nb], op=ALU.mult),
                                 reads=[pB[7], bRk], writes=[bKt])
                        for (bank, c0) in ((4, 1344), (5, 1360)):
                            for dc in range(8):
                                P.op("pe", lambda e: e.matmul(psb[bank][0:16, :nb], lhsT=wb[:, dc, c0:c0 + 16], rhs=rhs(dc), start=(dc == 0), stop=(dc == 7)),
                                     reads=hb + [bW], writes=[pB[bank]], inc=(dc == 7))
                        P.op("act", lambda e: e.activation(out=ra[:, :nb], in_=psb[4][0:16, :nb], func=AF.Identity), reads=[pB[4]], writes=[bR])
                        P.op("act", lambda e: e.activation(out=rb[:, :nb], in_=psb[5][0:16, :nb], func=AF.Identity), reads=[pB[5]], writes=[bR])
                        rope_apply(ra[:, :nb], rb[:, :nb], kt[0:16, 0, :nb], kt[32:48, 0, :nb], nb, w == 0, [bR], [bKt])
                        for h in range(1, 4):
                            P.op("pool", lambda e: e.tensor_copy(out=kt[0:16, h, :nb], in_=kt[0:16, 0, :nb]), reads=[bKt], writes=[bKt])
                            P.op("pool", lambda e: e.tensor_copy(out=kt[32:48, h, :nb], in_=kt[32:48, 0, :nb]), reads=[bKt], writes=[bKt])
                        P.dma("pool", kT[:, :, t0:t0 + nb].rearrange("h p t -> p h t"), kt[:, :, :nb], reads=[bKt], writes=[bKd])
                        for t in range(nt):
                            P.op("pe", lambda e: e.matmul(psb[7][:, 0:256], lhsT=ckvb[:, t * 128:(t + 1) * 128], rhs=wvb[:, :], start=True, stop=True), reads=[bCk, bS2], writes=[pB[7]])
                            P.op("act", lambda e: e.activation(out=vt[:, t, :, 0:64], in_=psb[7][:, 0:256].rearrange("p (h d) -> p h d", h=4), func=AF.Identity, scale=rcol[:, t:t + 1]),
                                 reads=[pB[7], bRc], writes=[bVt])
                        P.dma("pool", vv[t0:t0 + nb, :].rearrange("(t p) c -> p t c", p=128), vt[:, 0:nt, :, :].rearrange("p t h d -> p t (h d)"), reads=[bVt], writes=[bVd])
                        if not full:
                            continue
                        pending_tail = []
                        for t in range(nt):
                            sl = tix % 2
                            tix += 1
                            tl = tl0 + t * 128
                            tg = t0 + t * 128
                            hbt = hbufs(w, tl, 128)
                            for half in range(2):
                                bank = half
                                n = 0
                                for k in range(3):
                                    for dc in range(8):
                                        P.op("pe", lambda e: e.matmul(psb[bank][:, 0:384], lhsT=HT[:, dc, tl + k:tl + k + 128], rhs=wh[:, dc, k, half * 384:(half + 1) * 384],
                                                                      start=(n == 0), stop=(n == 23)), reads=hbt + [bW], writes=[pB[bank]], inc=(n == 23))
                                        n += 1
                                P.op("dve", lambda e: e.tensor_tensor(out=zt[sl][:, half * 384:(half + 1) * 384], in0=psb[bank][:, 0:384], in1=hsb[:, half * 384:(half + 1) * 384], op=ALU.add),
                                     reads=[pB[bank], bS2], writes=[bZt[sl]])
                            P.dma("pool", zs[tg:tg + 128, :], zt[sl][:], reads=[bZt[sl]], writes=[bZs])
                            for dc in range(8):
                                P.op("pe", lambda e: e.matmul(psb[2][:, :], lhsT=HT[:, dc, tl + 1:tl + 129], rhs=wb[:, dc, 0:512], start=(dc == 0), stop=(dc == 7)),
                                     reads=hbt + [bW], writes=[pB[2]], inc=(dc == 7))
                            while pending_tail:
                                pending_tail.pop(0)()
                            P.op("act", lambda e: e.activation(out=zg[sl][:, 0:256], in_=psb[2][:, 0:256], func=AF.Gelu), reads=[pB[2]], writes=[bZg[sl]])
                            P.op("act", lambda e: e.activation(out=zg[sl][:, 256:512], in_=psb[2][:, 256:512], func=AF.Gelu, accum_out=st1[sl][:, 0:1]), reads=[pB[2]], writes=[bZg[sl], bSt[sl]])
                            P.op("dve", lambda e: e.tensor_scalar(out=st1[sl][:, 0:1], in0=st1[sl][:, 0:1], scalar1=-1.0 / 256, scalar2=None, op0=ALU.mult), reads=[bSt[sl]], writes=[bSt[sl]])
                            P.op("dve", lambda e: e.tensor_scalar(out=vc[sl][:], in0=zg[sl][:, 256:512], scalar1=st1[sl][:, 0:1], scalar2=None, op0=ALU.add), reads=[bZg[sl], bSt[sl]], writes=[bVc[sl]])
                            P.op("act", lambda e: e.activation(out=junk2[:, 0:256], in_=vc[sl][:], func=AF.Square, accum_out=st1[sl][:, 1:2]), reads=[bVc[sl]], writes=[bJ2, bSt[sl]])
                            P.op("act", lambda e: e.activation(out=st1[sl][:, 1:2], in_=st1[sl][:, 1:2], func=AF.Sqrt, bias=epsT[:, 0:1], scale=1.0 / 256), reads=[bSt[sl], bC], writes=[bSt[sl]])
                            P.op("dve", lambda e: e.reciprocal(out=st1[sl][:, 1:2], in_=st1[sl][:, 1:2]), reads=[bSt[sl]], writes=[bSt[sl]])
                            P.op("dve", lambda e: e.scalar_tensor_tensor(out=vc[sl][:], in0=vc[sl][:], scalar=st1[sl][:, 1:2], in1=lng[:], op0=ALU.mult, op1=ALU.mult), reads=[bVc[sl], bSt[sl], bS2], writes=[bVc[sl]])
                            P.op("pool", lambda e: e.tensor_tensor(out=vnb[sl][:], in0=vc[sl][:], in1=lnb[:], op=ALU.add), reads=[bVc[sl], bS2], writes=[bVn[sl]])
                            def gm_tail(sl=sl, tg=tg):
                                for g in range(4):
                                    P.op("pe", lambda e: e.matmul(psb[3][:, g * 64:(g + 1) * 64], lhsT=wsT[:, g, :], rhs=vnb[sl][:, g * 64:(g + 1) * 64], start=(g == 0), stop=True),
                                         reads=[bVn[sl], bS2], writes=[pB[3]], inc=(g == 3))
                                P.op("dve", lambda e: e.tensor_tensor(out=ygm[sl][:], in0=psb[3][:, 0:256], in1=bsbc[:], op=ALU.add), reads=[pB[3], bS2], writes=[bYg[sl]])
                                P.op("pool", lambda e: e.tensor_tensor(out=ygm[sl][:], in0=ygm[sl][:], in1=zg[sl][:, 0:256], op=ALU.mult), reads=[bYg[sl], bZg[sl]], writes=[bYg[sl]])
                                P.dma("pool", ymix[tg:tg + 128, 256:512], ygm[sl][:], reads=[bYg[sl]], writes=[bYm])
                            pending_tail.append(gm_tail)
                        while pending_tail:
                            pending_tail.pop(0)()
                    P.barrier()
            if os.environ.get("MK_STOP") == "B":
                break
            with ExitStack() as st:
                F_ = lambda name, shape, dt=F32: sbt(st, name, shape, dt)
                kTs = F_("kTs", [128, 4, NTOK], BF16); bK = P.buf("kTs")
                vs = F_("vs", [128, 34, 260], BF16); bV = P.buf("vs")
                P.dma("sp", kTs[:, 0:2, :], kT[0:2].rearrange("h p t -> p h t"), reads=[bKd], writes=[bK])
                P.dma("sp", kTs[:, 2:4, :], kT[2:4].rearrange("h p t -> p h t"), reads=[bKd], writes=[bK])
                for c in range(0, 34, 8):
                    n = min(8, 34 - c)
                    P.dma("pool", vs[:, c:c + n, :], vv[c * 128:(c + n) * 128, :].rearrange("(t p) c -> p t c", p=128), reads=[bVd], writes=[bV])
                qs = [F_("qs%d" % i, [128, 4, 512], BF16) for i in range(2)]; bQs = P.bufs(2, "qs")
                pt = [F_("pt%d" % i, [128, 512], BF16) for i in range(3)]; bPt = P.bufs(3, "pt")
                oat = [F_("oat%d" % i, [128, 4, 256]) for i in range(2)]; bOa = P.bufs(2, "oat")
                rden = [F_("rden%d" % i, [128, 4]) for i in range(2)]; bRd = P.bufs(2, "rden")
                SC = 1.0 / float(np.sqrt(96.0))
                ablocks = [(bi, t0, nt, w) for bi, (t0, nt, w) in enumerate(BLOCKS) if not (last and w == 1)]
                iters = []
                for ai, (bi, t0, nt, w) in enumerate(ablocks):
                    kts = list(range(34)) if w == 0 else [32, 33]
                    for h in range(4):
                        for ki, kb in enumerate(kts):
                            iters.append((ai, t0, nt, w, h, ki, kb, len(kts)))

                def load_q(ai):
                    bi, t0, nt, w = ablocks[ai]
                    nb = nt * 128
                    P.dma("sp", qs[ai % 2][:, :, :nb], qT[:, :, t0:t0 + nb].rearrange("h p t -> p h t"), reads=[bQd], writes=[bQs[ai % 2]])

                def issue_qk(i):
                    ai, t0, nt, w, h, ki, kb, nk = iters[i]
                    nb = nt * 128
                    sl = ai % 2
                    sb_ = i % 3
                    if h == 0 and ki == 0 and ai + 1 < len(ablocks):
                        load_q(ai + 1)
                    P.op("pe", lambda e: e.matmul(psb[sb_][:, :nb], lhsT=kTs[:, h, kb * 128:(kb + 1) * 128], rhs=qs[sl][:, h, :nb], start=True, stop=True),
                         reads=[bK, bQs[sl]], writes=[pB[sb_]])
                    P.op("act", lambda e: e.activation(out=pt[sb_][:, :nb], in_=psb[sb_][:, :nb], func=AF.Exp, scale=SC), reads=[pB[sb_]], writes=[bPt[sb_]])

                def issue_pv(i):
                    ai, t0, nt, w, h, ki, kb, nk = iters[i]
                    nb = nt * 128
                    sl = ai % 2
                    sb_ = i % 3
                    ob = 4 + (h % 2)
                    for q_ in range(nt):
                        P.op("pe", lambda e: e.matmul(psb[ob][:, q_ * 65:(q_ + 1) * 65], lhsT=pt[sb_][:, q_ * 128:(q_ + 1) * 128], rhs=vs[:, kb, h * 65:(h + 1) * 65],
                                                      start=(ki == 0 and q_ == 0), stop=(ki == nk - 1)),
                             reads=[bPt[sb_], bV], writes=[pB[ob]], inc=(q_ == nt - 1))
                    if ki == nk - 1:
                        rs_ = h % 2
                        P.op("dve", lambda e: e.reciprocal(out=rden[rs_][:, 0:nt], in_=psb[ob][:, 0:nt * 65].rearrange("p (q c) -> p q c", c=65)[:, :, 64]),
                             reads=[pB[ob]], writes=[bRd[rs_]])
                        for q_ in range(nt):
                            P.op("dve", lambda e: e.tensor_scalar(out=oat[sl][:, q_, h * 64:(h + 1) * 64], in0=psb[ob][:, q_ * 65:q_ * 65 + 64], scalar1=rden[rs_][:, q_:q_ + 1], scalar2=None, op0=ALU.mult),
                                 reads=[pB[ob], bRd[rs_]], writes=[bOa[sl]])
                        if h == 3:
                            P.dma("pool", ymix[t0:t0 + nb, 768:1024].rearrange("(t p) c -> p t c", p=128), oat[sl][:, 0:nt, :], reads=[bOa[sl]], writes=[bYm])

                LA = 2
                load_q(0)
                for i in range(len(iters) + LA):
                    if i < len(iters):
                        issue_qk(i)
                    if i - LA >= 0:
                        issue_pv(i - LA)
                P.barrier()
            if os.environ.get("MK_STOP") == "AT":
                break
            with ExitStack() as st:
                F_ = lambda name, shape, dt=F32: sbt(st, name, shape, dt)
                gl = [F_("gll", [128, 2, L + 30], BF16), F_("glc", [128, 2, LC + 30], BF16)]; bG = P.bufs(2, "gl")
                for (w, n, c0) in ((0, L, 0), (1, LC, L)):
                    if last and w == 1:
                        continue
                    P.op("pool", lambda e: e.memset(gl[w][:, :, 0:15], 0.0), writes=[bG[w]])
                    P.op("pool", lambda e: e.memset(gl[w][:, :, n + 15:n + 30], 0.0), writes=[bG[w]])
                    for j in range(2):
                        P.dma("sp", gl[w][:, j, 15:15 + n], glu[j * 128:(j + 1) * 128, c0:c0 + n], reads=[bGlu], writes=[bG[w]])
                cvw = F_("cvw", [128, 2, 31]); cvb = F_("cvb", [128, 2]); clg = F_("clg", [128, 2]); clb = F_("clb", [128, 2]); bCv = P.buf("cvconst")
                P.dma("sp", cvw[:], I["cv_w"][l], writes=[bCv])
                P.dma("sp", cvb[:], I["cv_bT"][l], writes=[bCv])
                P.dma("sp", clg[:], I["cv_lgT"][l], writes=[bCv])
                P.dma("sp", clb[:], I["cv_lbT"][l], writes=[bCv])
                dg = F_("dg", [128, 2, 31, 128], BF16); bDg = P.buf("dg")
                for j in range(2):
                    for k in range(31):
                        P.op("dve" if k % 2 == 0 else "pool", lambda e: e.tensor_scalar(out=dg[:, j, k, :], in0=ident[:], scalar1=cvw[:, j, k:k + 1], scalar2=None, op0=ALU.mult),
                             reads=[bCv, bC], writes=[bDg])
                gsb = [F_("gsb%d" % i, [128, 512]) for i in range(2)]; bGs = P.bufs(2, "gsb")
                cen = [F_("cen%d" % i, [128, 512]) for i in range(2)]; bCe = P.bufs(2, "cen")
                sqs = [F_("sqs%d" % i, [128, 512]) for i in range(2)]; bSq = P.bufs(2, "sqs")
                ycT = [F_("ycT%d" % i, [128, 512]) for i in range(2)]; bYc = P.bufs(2, "ycT")
                ysb = [F_("ysb%d" % i, [128, 256]) for i in range(2)]; bYs = P.bufs(2, "ysb")
                tix = 0
                for bi, (t0, nt, w) in enumerate(BLOCKS):
                    if last and w == 1:
                        continue
                    nb = nt * 128
                    tl0 = t0 if w == 0 else 0
                    for j in range(2):
                        for k in range(31):
                            P.op("pe", lambda e: e.matmul(psb[j][:, :nb], lhsT=dg[:, j, k, :], rhs=gl[w][:, j, tl0 + k:tl0 + k + nb], start=(k == 0), stop=(k == 30)),
                                 reads=[bDg, bG[w]], writes=[pB[j]], inc=(k == 30))
                        P.op("act", lambda e: e.activation(out=gsb[j][:, :nb], in_=psb[j][:, :nb], func=AF.Identity, bias=cvb[:, j:j + 1]), reads=[pB[j], bCv], writes=[bGs[j]])
                        P.op("pe", lambda e: e.matmul(psb[2 + j][:, :nb], lhsT=blk64[:], rhs=gsb[j][:, :nb], start=True, stop=True), reads=[bGs[j], bC], writes=[pB[2 + j]])
                        P.op("dve", lambda e: e.tensor_tensor(out=cen[j][:, :nb], in0=gsb[j][:, :nb], in1=psb[2 + j][:, :nb], op=ALU.subtract), reads=[bGs[j], pB[2 + j]], writes=[bCe[j]])
                        P.op("act", lambda e: e.activation(out=sqs[j][:, :nb], in_=cen[j][:, :nb], func=AF.Square), reads=[bCe[j]], writes=[bSq[j]])
                        P.op("pe", lambda e: e.matmul(psb[2 + j][:, :nb], lhsT=blk64[:], rhs=sqs[j][:, :nb], start=True, stop=True), reads=[bSq[j], bC], writes=[pB[2 + j]])
                        P.op("act", lambda e: e.activation(out=sqs[j][:, :nb], in_=psb[2 + j][:, :nb], func=AF.Sqrt, bias=epsT[:, 0:1]), reads=[pB[2 + j], bC], writes=[bSq[j]])
                        P.op("dve", lambda e: e.reciprocal(out=sqs[j][:, :nb], in_=sqs[j][:, :nb]), reads=[bSq[j]], writes=[bSq[j]])
                        P.op("dve", lambda e: e.tensor_tensor(out=cen[j][:, :nb], in0=cen[j][:, :nb], in1=sqs[j][:, :nb], op=ALU.mult), reads=[bCe[j], bSq[j]], writes=[bCe[j]])
                        P.op("act", lambda e: e.activation(out=ycT[j][:, :nb], in_=cen[j][:, :nb], func=AF.Silu, bias=clb[:, j:j + 1], scale=clg[:, j:j + 1]), reads=[bCe[j], bCv], writes=[bYc[j]])
                    for t in range(nt):
                        sl = tix % 2
                        tix += 1
                        bank = 4 + sl
                        for j in range(2):
                            P.op("pe", lambda e: e.transpose(out=psb[bank][:, j * 128:(j + 1) * 128], in_=ycT[j][:, t * 128:(t + 1) * 128], identity=ident[:]),
                                 reads=[bYc[j], bC], writes=[pB[bank]], inc=(j == 1))
                        P.op("act", lambda e: e.activation(out=ysb[sl][:], in_=psb[bank][:, 0:256], func=AF.Identity), reads=[pB[bank]], writes=[bYs[sl]])
                        tg = t0 + t * 128
                        P.dma("pool", ymix[tg:tg + 128, 512:768], ysb[sl][:], reads=[bYs[sl]], writes=[bYm])
                P.barrier()
            if os.environ.get("MK_STOP") == "CV":
                break
            def hyena(w, Lh, tag, c0, ksp):
                NT = Lh // 128
                NR = 2 * NT
                NS = max(NT // 2, 1)
                TB = min(512, Lh)
                with ExitStack() as st:
                    F_ = lambda name, shape, dt=F32: sbt(st, name, shape, dt)
                    slab = [F_("slab%d" % i, [128, 32 * 512 if Lh == L else NT * 512], BF16) for i in range(2)]; bSl = P.bufs(2, "slab"); bSlB = P.bufs(2, "slabB")

                    def load_slab(sl, view3, src3, n0):
                        h = max(n0 // 2, 1)
                        P.dma("sp", view3[:, 0:h, :], src3[:, 0:h, :], writes=[bSl[sl]])
                        if h < n0:
                            P.dma("act", view3[:, h:n0, :], src3[:, h:n0, :], writes=[bSlB[sl]])
                    invn = F_("invn", [128, 512]); bIn = P.buf("invn")
                    rsc = F_("rsc", [128, NR]); bRs = P.buf("rsc")
                    P.dma("sp", rsc[:], I["rsc" + tag], writes=[bRs])
                    bKs = P.buf("kspec_dram")
                    with ExitStack() as s1:
                        G_ = lambda name, shape, dt=F32: sbt(s1, name, shape, dt)
                        w1 = G_("w1", [33, 64]); w2 = G_("w2", [64, 64]); w3 = G_("w3", [64, 1024]); cb = G_("cb", [64, 6]); bFw = P.buf("fw")
                        P.dma("sp", w1[:], I["hy_f_w1"][l], writes=[bFw])
                        P.dma("sp", w2[:], I["hy_f_w2"][l], writes=[bFw])
                        P.dma("sp", w3[:], I["hy_f_w3"][l], writes=[bFw])
                        P.dma("sp", cb[:, 0:1], I["hy_b1c"][l], writes=[bFw])
                        P.dma("sp", cb[:, 1:2], I["hy_b2c"][l], writes=[bFw])
                        P.dma("sp", cb[:, 2:3], I["hy_frc"][l], writes=[bFw])
                        P.op("dve", lambda e: e.tensor_tensor(out=cb[:, 3:4], in0=cb[:, 0:1], in1=cb[:, 2:3], op=ALU.mult), reads=[bFw], writes=[bFw])
                        P.op("dve", lambda e: e.tensor_tensor(out=cb[:, 4:5], in0=cb[:, 1:2], in1=cb[:, 2:3], op=ALU.mult), reads=[bFw], writes=[bFw])
                        zT = G_("zT", [33, TB]); bZ = P.buf("zT")
                        arg = G_("arg", [64, TB]); msk_ = G_("mskf", [64, TB]); h1 = G_("h1", [64, TB]); bAr = P.buf("arg"); bH1 = P.buf("h1")
                        h2T = G_("h2T", [64, Lh]); bH2 = P.buf("h2T")
                        rre = G_("rre", [128, NT, 512], BF16); rim = G_("rim", [128, NT, 512], BF16); bRe = P.buf("rre")
                        dect = [G_("dect%d" % i, [128, 256]) for i in range(2)]; bDe = P.bufs(2, "dect")
                        kk = G_("kk", [128, 1024]); ak = G_("ak", [128, 1024]); bKk = P.buf("kk"); bAk = P.buf("ak")
                        spo = [G_("spo%d" % i, [128, 512]) for i in range(2)]; bSp = P.bufs(2, "spo")

                        def sin_layer(ps_ap, bcol, dst, nbk, rd, wr):
                            P.op("act", lambda e: e.activation(out=arg[:, :nbk], in_=ps_ap, func=AF.Identity, bias=cb[:, bcol:bcol + 1], scale=cb[:, 2:3]), reads=rd + [bFw], writes=[bAr])
                            for _ in range(2):
                                P.op("dve", lambda e: e.tensor_single_scalar(out=msk_[:, :nbk], in_=arg[:, :nbk], scalar=PI, op=ALU.is_gt), reads=[bAr], writes=[bAr])
                                P.op("dve", lambda e: e.scalar_tensor_tensor(out=arg[:, :nbk], in0=msk_[:, :nbk], scalar=-2.0 * PI, in1=arg[:, :nbk], op0=ALU.mult, op1=ALU.add), reads=[bAr], writes=[bAr])
                                P.op("dve", lambda e: e.tensor_single_scalar(out=msk_[:, :nbk], in_=arg[:, :nbk], scalar=-PI, op=ALU.is_lt), reads=[bAr], writes=[bAr])
                                P.op("dve", lambda e: e.scalar_tensor_tensor(out=arg[:, :nbk], in0=msk_[:, :nbk], scalar=2.0 * PI, in1=arg[:, :nbk], op0=ALU.mult, op1=ALU.add), reads=[bAr], writes=[bAr])
                            P.op("act", lambda e: e.activation(out=dst, in_=arg[:, :nbk], func=AF.Sin), reads=[bAr], writes=wr)

                        for b0 in range(0, Lh, TB):
                            P.dma("sp", zT[:, :], I["hyz" + tag][:, b0:b0 + TB], writes=[bZ])
                            P.op("pe", lambda e: e.matmul(psb[0][0:64, :TB], lhsT=w1[:, :], rhs=zT[:, :], start=True, stop=True), reads=[bZ, bFw], writes=[pB[0]])
                            sin_layer(psb[0][0:64, :TB], 3, h1[:, :TB], TB, [pB[0]], [bH1])
                            P.op("pe", lambda e: e.matmul(psb[1][0:64, :TB], lhsT=w2[:, :], rhs=h1[:, :TB], start=True, stop=True), reads=[bH1, bFw], writes=[pB[1]])
                            sin_layer(psb[1][0:64, :TB], 4, h2T[:, b0:b0 + TB], TB, [pB[1]], [bH2])
                        for ti in range(NT):
                            sl = ti % 2
                            P.dma("sp", dect[sl][:], I["hydec" + tag][:, ti, :], writes=[bDe[sl]])
                            for o in range(2):
                                bank = 2 + o
                                P.op("pe", lambda e: e.matmul(psb[bank][:, :], lhsT=h2T[:, ti * 128:(ti + 1) * 128], rhs=w3[:, o * 512:(o + 1) * 512], start=True, stop=True), reads=[bH2, bFw], writes=[pB[bank]])
                                P.op("dve", lambda e: e.tensor_tensor(out=kk[:, o * 512:(o + 1) * 512].rearrange("p (a c) -> p a c", c=256), in0=psb[bank][:, :].rearrange("p (a c) -> p a c", c=256),
                                                                       in1=dect[sl][:].unsqueeze(1).to_broadcast([128, 2, 256]), op=ALU.mult), reads=[pB[bank], bDe[sl]], writes=[bKk])
                            if ti == 0:
                                for o in range(2):
                                    P.op("dve", lambda e: e.memset(kk[0:1, o * 512 + 256:o * 512 + 512], 0.0), reads=[bKk], writes=[bKk])
                            P.op("dve", lambda e: e.scalar_tensor_tensor(out=ak[:], in0=kk[:], scalar=-1.0, in1=kk[:], op0=ALU.mult, op1=ALU.max), reads=[bKk], writes=[bAk])
                            for o in range(2):
                                P.op("pe", lambda e: e.matmul(psb[6 + o][:, :], lhsT=onesf[:], rhs=ak[:, o * 512:(o + 1) * 512], start=(ti == 0), stop=(ti == NT - 1)), reads=[bAk, bC], writes=[pB[6 + o]])
                            kv_ = kk[:].rearrange("p (o d c) -> p o d c", o=2, d=2)
                            P.op("pool", lambda e: e.tensor_tensor(out=rre[:, ti, :].rearrange("p (o c) -> p o c", o=2), in0=kv_[:, :, 0, :], in1=kv_[:, :, 1, :], op=ALU.add), reads=[bKk], writes=[bRe])
                            P.op("pool", lambda e: e.tensor_tensor(out=rim[:, ti, :].rearrange("p (o c) -> p o c", o=2), in0=kv_[:, :, 0, :], in1=kv_[:, :, 1, :], op=ALU.subtract), reads=[bKk], writes=[bRe])
                        for o in range(2):
                            P.op("act", lambda e: e.activation(out=invn[:, o * 256:(o + 1) * 256], in_=psb[6 + o][:, 0:256], func=AF.Identity), reads=[pB[6 + o]], writes=[bIn])
                            P.op("dve", lambda e: e.tensor_tensor(out=invn[:, o * 256:(o + 1) * 256], in0=psb[6 + o][:, 256:512], in1=invn[:, o * 256:(o + 1) * 256], op=ALU.add), reads=[pB[6 + o], bIn], writes=[bIn])
                        P.op("dve", lambda e: e.reciprocal(out=invn[:], in_=invn[:]), reads=[bIn], writes=[bIn])
                        k = 0
                        for s_ in range(NS):
                            sl = s_ % 2
                            sv = slab[sl][:, 0:NT * 512].rearrange("p (t c) -> p t c", c=512)
                            load_slab(sl, sv, I["dftf" + tag][s_], NT)
                            rcs_ = [2 * s_, 2 * s_ + 1, NT + 2 * s_, NT + 2 * s_ + 1] if NT >= 2 else None
                            for j, rc in enumerate(rcs_):
                                src = rim if j >= 2 else rre
                                bank = k % 2
                                for tc in range(NT):
                                    P.op("pe", lambda e: e.matmul(psb[bank][:, :], lhsT=sv[:, tc, j * 128:(j + 1) * 128], rhs=src[:, tc, :], start=(tc == 0), stop=(tc == NT - 1)),
                                         reads=[bSl[sl], bSlB[sl], bRe], writes=[pB[bank]], inc=(tc == NT - 1))
                                osl = k % 2
                                P.op("dve", lambda e: e.scalar_tensor_tensor(out=spo[osl][:], in0=psb[bank][:, :], scalar=rsc[:, rc:rc + 1], in1=invn[:], op0=ALU.mult, op1=ALU.mult),
                                     reads=[pB[bank], bRs, bIn], writes=[bSp[osl]])
                                if rc == NT:
                                    for tc in range(NT):
                                        P.op("pe", lambda e: e.matmul(psb[2][0:1, :], lhsT=sv[:, tc, j * 128:j * 128 + 1], rhs=rre[:, tc, :], start=(tc == 0), stop=(tc == NT - 1)),
                                             reads=[bSl[sl], bSlB[sl], bRe], writes=[pB[2]], inc=(tc == NT - 1))
                                    P.op("dve", lambda e: e.scalar_tensor_tensor(out=spo[osl][0:1, :], in0=psb[2][0:1, :], scalar=rsc[0:1, rc:rc + 1], in1=invn[0:1, :], op0=ALU.mult, op1=ALU.mult),
                                         reads=[pB[2], bRs, bIn, bSp[osl]], writes=[bSp[osl]])
                                P.dma("pool", ksp[rc * 128:(rc + 1) * 128, :], spo[osl][:], reads=[bSp[osl]], writes=[bKs])
                                k += 1
                        P.barrier()
                    with ExitStack() as s2:
                        G_ = lambda name, shape, dt=F32: sbt(s2, name, shape, dt)
                        ub = G_("ub", [128, NT, 256], BF16); bUb = P.buf("ub")
                        Yf = G_("Yf", [128, NR, 256], BF16); bYf = P.buf("Yf")
                        y1f = G_("y1f", [128, NT, 256]); bY1 = P.buf("y1f")
                        hyb = G_("hyb", [128, 512]); bHb = P.buf("hyb")
                        P.dma("sp", hyb[:], I["hyb_bc"][l], writes=[bHb])
                        vst = [G_("vst%d" % i, [128, 8, 256]) for i in range(2)]; bVs = P.bufs(2, "vst")
                        for c in range(0, NT, 8):
                            n = min(8, NT - c)
                            sl = (c // 8) % 2
                            P.dma("sp", vst[sl][:, 0:n, :], zs[c0 + c * 128:c0 + (c + n) * 128, 512:768].rearrange("(t p) c -> p t c", p=128), reads=[bZs], writes=[bVs[sl]])
                            P.op("pool", lambda e: e.tensor_copy(out=ub[:, c:c + n, :], in_=vst[sl][:, 0:n, :]), reads=[bVs[sl]], writes=[bUb])
                        kt_ = [G_("ktab%d" % i, [128, 2, 256]) for i in range(2)]; bKt_ = P.bufs(2, "ktab")
                        tm = [G_("tm%d" % i, [128, 256]) for i in range(4)]; bTm = P.bufs(4, "tm")
                        zt_ = [G_("hzt%d" % i, [128, 768]) for i in range(2)]; bZt_ = P.bufs(2, "hzt")
                        yo = [G_("yo%d" % i, [128, 256]) for i in range(2)]; bYo = P.bufs(2, "yo")
                        kq = 0
                        for o in range(2):
                            for s_ in range(NS):
                                sl = kq % 2
                                kq += 1
                                sv = slab[sl][:, 0:NT * 512].rearrange("p (t c) -> p t c", c=512)
                                load_slab(sl, sv, I["dftf" + tag][s_], NT)
                                for j in range(4):
                                    bank = (s_ % 2) * 2 + (j % 2)
                                    cs = (j // 2) * 256
                                    for tc in range(NT):
                                        P.op("pe", lambda e: e.matmul(psb[bank][:, cs:cs + 256], lhsT=sv[:, tc, j * 128:(j + 1) * 128], rhs=ub[:, tc, :], start=(tc == 0), stop=(tc == NT - 1)),
                                             reads=[bSl[sl], bSlB[sl], bUb], writes=[pB[bank]], inc=(tc == NT - 1))
                                for jj in range(2):
                                    fc = 2 * s_ + jj
                                    bank = (s_ % 2) * 2 + jj
                                    ks_ = fc % 2
                                    P.dma("sp", kt_[ks_][:, 0, :], ksp[fc * 128:(fc + 1) * 128, o * 256:(o + 1) * 256], reads=[bKs], writes=[bKt_[ks_]])
                                    P.dma("sp", kt_[ks_][:, 1, :], ksp[(NT + fc) * 128:(NT + fc + 1) * 128, o * 256:(o + 1) * 256], reads=[bKs], writes=[bKt_[ks_]])
                                    Ure = psb[bank][:, 0:256]
                                    Uim = psb[bank][:, 256:512]
                                    Kre = kt_[ks_][:, 0, :]
                                    Kim = kt_[ks_][:, 1, :]
                                    P.op("dve", lambda e: e.tensor_tensor(out=tm[0][:], in0=Ure, in1=Kre, op=ALU.mult), reads=[pB[bank], bKt_[ks_]], writes=[bTm[0]])
                                    P.op("dve", lambda e: e.tensor_tensor(out=tm[1][:], in0=Uim, in1=Kim, op=ALU.mult), reads=[pB[bank], bKt_[ks_]], writes=[bTm[1]])
                                    P.op("pool", lambda e: e.tensor_tensor(out=Yf[:, fc, :], in0=tm[0][:], in1=tm[1][:], op=ALU.subtract), reads=[bTm[0], bTm[1]], writes=[bYf])
                                    P.op("dve", lambda e: e.tensor_tensor(out=tm[2][:], in0=Ure, in1=Kim, op=ALU.mult), reads=[pB[bank], bKt_[ks_]], writes=[bTm[2]])
                                    P.op("dve", lambda e: e.tensor_tensor(out=tm[3][:], in0=Uim, in1=Kre, op=ALU.mult), reads=[pB[bank], bKt_[ks_]], writes=[bTm[3]])
                                    P.op("pool", lambda e: e.tensor_tensor(out=Yf[:, NT + fc, :], in0=tm[2][:], in1=tm[3][:], op=ALU.add), reads=[bTm[2], bTm[3]], writes=[bYf])
                                    if fc == 0:
                                        P.op("dve", lambda e: e.tensor_tensor(out=Yf[0:1, 0, :], in0=Ure[0:1, :], in1=Kre[0:1, :], op=ALU.mult), reads=[pB[bank], bKt_[ks_], bYf], writes=[bYf])
                                        P.op("dve", lambda e: e.tensor_tensor(out=Yf[0:1, NT, :], in0=Uim[0:1, :], in1=Kim[0:1, :], op=ALU.mult), reads=[pB[bank], bKt_[ks_], bYf], writes=[bYf])
                            for s_ in range(NS):
                                sl = kq % 2
                                kq += 1
                                iv = slab[sl][:, 0:NR * 256].rearrange("p (r c) -> p r c", c=256)
                                load_slab(sl, iv, I["dfti" + tag][s_], NR)
                                for tt in range(2 if NT >= 2 else 1):
                                    ti = 2 * s_ + tt
                                    bank = 4 + (ti % 2)
                                    for rc in range(NR):
                                        P.op("pe", lambda e: e.matmul(psb[bank][:, 0:256], lhsT=iv[:, rc, tt * 128:(tt + 1) * 128], rhs=Yf[:, rc, :], start=(rc == 0), stop=(rc == NR - 1)),
                                             reads=[bSl[sl], bSlB[sl], bYf], writes=[pB[bank]], inc=(rc == NR - 1))
                                    zsl = ti % 2
                                    tg = c0 + ti * 128
                                    P.dma("sp", zt_[zsl][:], zs[tg:tg + 128, :], reads=[bZs], writes=[bZt_[zsl]])
                                    u32 = zt_[zsl][:, 512:768] if o == 0 else y1f[:, ti, :]
                                    gate = zt_[zsl][:, o * 256:(o + 1) * 256]
                                    P.op("dve", lambda e: e.tensor_tensor(out=yo[zsl][:], in0=u32, in1=hyb[:, o * 256:(o + 1) * 256], op=ALU.mult), reads=[bZt_[zsl], bY1, bHb], writes=[bYo[zsl]])
                                    P.op("dve", lambda e: e.tensor_tensor(out=yo[zsl][:], in0=psb[bank][:, 0:256], in1=yo[zsl][:], op=ALU.add), reads=[pB[bank], bYo[zsl]], writes=[bYo[zsl]])
                                    if o == 0:
                                        P.op("pool", lambda e: e.tensor_tensor(out=y1f[:, ti, :], in0=yo[zsl][:], in1=gate, op=ALU.mult), reads=[bYo[zsl], bZt_[zsl]], writes=[bY1])
                                        P.op("pool", lambda e: e.tensor_copy(out=ub[:, ti, :], in_=y1f[:, ti, :]), reads=[bY1], writes=[bUb])
                                    else:
                                        P.op("pool", lambda e: e.tensor_tensor(out=yo[zsl][:], in0=yo[zsl][:], in1=gate, op=ALU.mult), reads=[bYo[zsl], bZt_[zsl]], writes=[bYo[zsl]])
                                        P.dma("pool", ymix[tg:tg + 128, 0:256], yo[zsl][:], reads=[bYo[zsl]], writes=[bYm])
                        P.barrier()
                P.barrier()

            hyena(0, L, "L", 0, kspec)
            if not last:
                hyena(1, LC, "C", L, kspecC)
            if os.environ.get("MK_STOP") == "HY":
                break

            with ExitStack() as st:
                F_ = lambda name, shape, dt=F32: sbt(st, name, shape, dt)
                wo = F_("wo", [128, 8, 1024], BF16); bWo = P.buf("wo")
                wst = [F_("wost%d" % i, [128, 1024]) for i in range(2)]; bWs = P.bufs(2, "wost")
                mixg = F_("mixg", [128, 1024]); bMg = P.buf("mixg")
                P.dma("sp", mixg[:], I["mixg_bc"][l], writes=[bMg])
                for kc in range(8):
                    sl = kc % 2
                    P.dma("sp", wst[sl][:], I["w_out"][l, kc * 128:(kc + 1) * 128, :], writes=[bWs[sl]])
                    P.op("pool", lambda e: e.tensor_copy(out=wo[:, kc, :], in_=wst[sl][:]), reads=[bWs[sl]], writes=[bWo])
                yt = [F_("yt%d" % i, [128, 1024]) for i in range(3)]; bYt = P.bufs(3, "yt")
                xt = [F_("oxt%d" % i, [128, 1024]) for i in range(3)]; bXt = P.bufs(3, "oxt")
                yn = [F_("yn%d" % i, [128, 1024], BF16) for i in range(3)]; bYn = P.bufs(3, "yn")
                ynT = [F_("ynT%d" % i, [128, 8, 128], BF16) for i in range(3)]; bYT = P.bufs(3, "ynT")
                xo = [F_("xo%d" % i, [128, 1024]) for i in range(3)]; bXo = P.bufs(3, "xo")
                s4 = [F_("s4%d" % i, [128, 4]) for i in range(3)]; bS4 = P.bufs(3, "s4")
                bXm = P.buf("xm_dram")
                ntile = 32 if last else 34
                for ti in range(ntile):
                    sl = ti % 3
                    w = 0 if ti < 32 else 1
                    tg = ti * 128
                    P.dma("sp", yt[sl][:], ymix[tg:tg + 128, :], reads=[bYm], writes=[bYt[sl]])
                    P.dma("sp", xt[sl][:], xin_ap(l, tg, 128), writes=[bXt[sl]])
                    for g in range(4):
                        P.op("act", lambda e: e.activation(out=junk2[:, 0:256], in_=yt[sl][:, g * 256:(g + 1) * 256], func=AF.Square, accum_out=s4[sl][:, g:g + 1]),
                             reads=[bYt[sl]], writes=[bJ2, bS4[sl]])
                    P.op("act", lambda e: e.activation(out=s4[sl][:], in_=s4[sl][:], func=AF.Sqrt, bias=epsT[:, 0:1], scale=1.0 / 256), reads=[bS4[sl], bC], writes=[bS4[sl]])
                    P.op("dve", lambda e: e.reciprocal(out=s4[sl][:], in_=s4[sl][:]), reads=[bS4[sl]], writes=[bS4[sl]])
                    for g in range(4):
                        P.op("dve", lambda e: e.scalar_tensor_tensor(out=yn[sl][:, g * 256:(g + 1) * 256], in0=yt[sl][:, g * 256:(g + 1) * 256], scalar=s4[sl][:, g:g + 1],
                                                                     in1=mixg[:, g * 256:(g + 1) * 256], op0=ALU.mult, op1=ALU.mult),
                             reads=[bYt[sl], bS4[sl], bMg], writes=[bYn[sl]])
                    for half in range(2):
                        bank = 6 + half
                        pT = psb[bank][:].bitcast(BF16)
                        for j in range(4):
                            kc = half * 4 + j
                            P.op("pe", lambda e: e.transpose(out=pT[:, j * 128:(j + 1) * 128], in_=yn[sl][:, kc * 128:(kc + 1) * 128], identity=identb[:]),
                                 reads=[bYn[sl], bC], writes=[pB[bank]], inc=(j == 3))
                        P.op("act", lambda e: e.activation(out=ynT[sl][:, half * 4:(half + 1) * 4, :], in_=pT[:, 0:512].rearrange("p (a b) -> p a b", a=4), func=AF.Identity),
                             reads=[pB[bank]], writes=[bYT[sl]])
                    for half in range(2):
                        for kc in range(8):
                            P.op("pe", lambda e: e.matmul(psb[half][:, :], lhsT=ynT[sl][:, kc, :], rhs=wo[:, kc, half * 512:(half + 1) * 512], start=(kc == 0), stop=(kc == 7)),
                                 reads=[bYT[sl], bWo], writes=[pB[half]], inc=(kc == 7))
                        P.op("dve", lambda e: e.tensor_tensor(out=xo[sl][:, half * 512:(half + 1) * 512], in0=psb[half][:, :], in1=gbc[:, w, half * 512:(half + 1) * 512], op=ALU.mult),
                             reads=[pB[half], bGbc], writes=[bXo[sl]])
                        P.op("pool", lambda e: e.tensor_tensor(out=xo[sl][:, half * 512:(half + 1) * 512], in0=xo[sl][:, half * 512:(half + 1) * 512], in1=xt[sl][:, half * 512:(half + 1) * 512], op=ALU.add),
                             reads=[bXo[sl], bXt[sl]], writes=[bXo[sl]])
                    P.dma("pool", xm[tg:tg + 128, :], xo[sl][:], reads=[bXo[sl]], writes=[bXm])
                P.barrier()
            if os.environ.get("MK_STOP") == "O":
                break
            with ExitStack() as st:
                F_ = lambda name, shape, dt=F32: sbt(st, name, shape, dt)
                NTL = 31 if last else 32
                NSL = 32 * 512
                BIG = 32768.0
                ntile = 32 if last else 34
                sel_all = F_("sel_all", [128, 34, 16]); g_all = F_("g_all", [128, 34, 16]); bSel = P.buf("sel_all"); bGa = P.buf("g_all")
                slotA_i = F_("slotA_i", [128, 34], I32); slotB_i = F_("slotB_i", [128, 34], I32); gA = F_("gA", [128, 34]); gB = F_("gB", [128, 34])
                widx_i = F_("widx_i", [128, 32], I32); bSlots = P.buf("slots")
                tokidx = F_("tokidx", [128, 34], I32); bTok = P.buf("tokidx")
                P.dma("sp", tokidx[:], I["tokidx"], writes=[bTok])
                P.op("pool", lambda e: e.memset(sel_all[:], 0.0), writes=[bSel])
                P.op("pool", lambda e: e.memset(g_all[:], 0.0), writes=[bGa])
                bTbl = P.buf("tbl_dram"); bHfD = P.buf("hfD_dram"); bYp = P.buf("ypair_dram")
                P.dma("sp", tbl, I["tblfill"], writes=[bTbl])
                with ExitStack() as s1:
                    G_ = lambda name, shape, dt=F32: sbt(s1, name, shape, dt)
                    wr = G_("wr", [128, 8, 16]); rbb = G_("rbb", [128, 16]); bMc = P.buf("mconst")
                    P.dma("sp", wr[:], I["w_router"].rearrange("(c p) e -> p c e", p=128), writes=[bMc])
                    P.dma("sp", rbb[:], I["rb_bc"], writes=[bMc])
                    bc2 = G_("bc2", [128, 4, 1024]); bBc2 = P.buf("bc2")
                    diag = [G_("mdiag%d" % i, [128, 128]) for i in range(2)]; bDiag = P.bufs(2, "mdiag")
                    k = 0
                    for gi, (srct, off, w) in enumerate(((modT, 24, 0), (modT, 24, 1), (gain2, 0, 0), (gain2, 0, 1))):
                        for half in range(2):
                            bank = 1 + (k % 2)
                            for j in range(4):
                                dc = half * 4 + j
                                dsl = (k * 4 + j) % 2
                                P.op("dve", lambda e: e.tensor_scalar(out=diag[dsl][:], in0=ident[:], scalar1=srct[:, off + dc, w:w + 1], scalar2=None, op0=ALU.mult),
                                     reads=[bMod, bC], writes=[bDiag[dsl]])
                                P.op("pe", lambda e: e.matmul(psb[bank][:, j * 128:(j + 1) * 128], lhsT=onesf[:], rhs=diag[dsl][:], start=(j == 0), stop=True),
                                     reads=[bDiag[dsl], bC], writes=[pB[bank]], inc=True)
                            P.op("act", lambda e: e.activation(out=bc2[:, gi, half * 512:(half + 1) * 512], in_=psb[bank][:], func=AF.Identity),
                                 reads=[pB[bank]], writes=[bBc2])
                            k += 1
                    mx = [G_("mx%d" % i, [128, 1024]) for i in range(4)]; bMx = P.bufs(4, "mx")
                    xn4 = G_("xn4", [128, 4, 1024]); bXn = P.bufs(4, "xn4")
                    hf32 = [G_("hf32_%d" % i, [128, 512]) for i in range(2)]; bH32 = P.bufs(2, "hf32")
                    htm = [G_("htm%d" % i, [128, 1024]) for i in range(4)]; bHtm = P.bufs(4, "htm")
                    hfb = [G_("hfb%d" % i, [128, 1024], BF16) for i in range(4)]; bHfb = P.bufs(4, "hfb")
                    ms = [G_("ms%d" % i, [128, 1]) for i in range(4)]; bMs = P.bufs(4, "ms")
                    sg = G_("sg", [128, 4, 16]); sl_ = G_("selv", [128, 4, 16]); p6 = G_("p6", [128, 16, 6]); gs = G_("gs", [128, 16]); gmx = G_("gmx", [128, 4])
                    isb = G_("isb", [128, 16]); thr = G_("thr", [128, 16]); msk = G_("msk", [128, 4, 16]); den = G_("den", [128, 4]); bRt = P.buf("router")
                    mti = 0
                    for bidx, (t0, nt, w) in enumerate(BLOCKS):
                        if last and w == 1:
                            continue
                        nb = nt * 128
                        T0 = t0 // 128
                        for t in range(nt):
                            sl = mti % 4
                            mti += 1
                            P.dma("sp", mx[sl][:], xm[t0 + t * 128:t0 + (t + 1) * 128, :], reads=[bXm], writes=[bMx[sl]])
                            P.op("act", lambda e: e.activation(out=junk2[:], in_=mx[sl][:], func=AF.Square, accum_out=ms[sl][:]), reads=[bMx[sl]], writes=[bJ2, bMs[sl]])
                            P.op("act", lambda e: e.activation(out=ms[sl][:], in_=ms[sl][:], func=AF.Sqrt, bias=epsT[:, 0:1], scale=1.0 / D), reads=[bMs[sl], bC], writes=[bMs[sl]])
                            P.op("dve", lambda e: e.reciprocal(out=ms[sl][:], in_=ms[sl][:]), reads=[bMs[sl]], writes=[bMs[sl]])
                            P.op("dve", lambda e: e.tensor_scalar(out=xn4[:, t, :], in0=mx[sl][:], scalar1=ms[sl][:, 0:1], scalar2=None, op0=ALU.mult), reads=[bMx[sl], bMs[sl]], writes=[bXn[t]])
                            P.op("dve", lambda e: e.tensor_tensor(out=htm[sl][:], in0=xn4[:, t, :], in1=bc2[:, 2 + w, :], op=ALU.mult), reads=[bXn[t], bBc2], writes=[bHtm[sl]])
                            P.op("pool", lambda e: e.tensor_tensor(out=hfb[sl][:], in0=htm[sl][:], in1=bc2[:, w, :], op=ALU.add), reads=[bHtm[sl], bBc2], writes=[bHfb[sl]])
                            P.dma("pool", hfD[t0 + t * 128:t0 + (t + 1) * 128, :], hfb[sl][:], reads=[bHfb[sl]], writes=[bHfD])
                        def m1_T(dc):
                            bank = 6 + dc % 2
                            for t in range(nt):
                                P.op("pe", lambda e: e.transpose(out=psb[bank][:, t * 128:(t + 1) * 128], in_=xn4[:, t, dc * 128:(dc + 1) * 128], identity=ident[:]),
                                     reads=[bXn[t], bC], writes=[pB[bank]], inc=(t == nt - 1))
                            P.op("act", lambda e: e.activation(out=hf32[dc % 2][:, :nb], in_=psb[bank][:, :nb], func=AF.Identity, bias=modT[:, 24 + dc, w:w + 1], scale=gain2[:, dc, w:w + 1]),
                                 reads=[pB[bank], bMod], writes=[bH32[dc % 2]])

                        def m1_R(dc):
                            for t in range(nt):
                                P.op("pe", lambda e: e.matmul(psb[5][:, t * 16:(t + 1) * 16], lhsT=hf32[dc % 2][:, t * 128:(t + 1) * 128], rhs=wr[:, dc, :], start=(dc == 0 and t == 0), stop=(dc == 7)),
                                     reads=[bH32[dc % 2], bMc], writes=[pB[5]], inc=(t == nt - 1))

                        m1_T(0)
                        for dc in range(8):
                            if dc + 1 < 8:
                                m1_T(dc + 1)
                            m1_R(dc)
                        G = nt * 4
                        P.op("act", lambda e: e.activation(out=sg[:, 0:nt, :], in_=psb[5][:, 0:nt * 16].rearrange("p (t e) -> p t e", e=16), func=AF.Sigmoid), reads=[pB[5]], writes=[bRt])
                        P.op("dve", lambda e: e.tensor_tensor(out=sl_[:, 0:nt, :], in0=sg[:, 0:nt, :], in1=rbb[:].unsqueeze(1).to_broadcast([128, nt, 16]), op=ALU.add), reads=[bRt, bMc], writes=[bRt])
                        sv = sl_[:, 0:nt, :].rearrange("p t (g k) -> p (t g) k", k=4)
                        for (opx, dst) in ((ALU.add, gs), (ALU.min, thr)):
                            P.op("dve", lambda e: e.tensor_tensor(out=p6[:, 0:G, 0:2], in0=sv[:, :, 0:2], in1=sv[:, :, 2:4], op=opx), reads=[bRt], writes=[bRt])
                            P.op("dve", lambda e: e.tensor_tensor(out=p6[:, 0:G, 2:5], in0=sv[:, :, 0:3], in1=sv[:, :, 1:4], op=opx), reads=[bRt], writes=[bRt])
                            P.op("dve", lambda e: e.tensor_tensor(out=p6[:, 0:G, 5:6], in0=sv[:, :, 0:1], in1=sv[:, :, 3:4], op=opx), reads=[bRt], writes=[bRt])
                            P.op("dve", lambda e: e.tensor_reduce(out=dst[:, 0:G], in_=p6[:, 0:G, :], axis=AX.X, op=ALU.max), reads=[bRt], writes=[bRt])
                        P.op("dve", lambda e: e.tensor_reduce(out=gmx[:, 0:nt], in_=gs[:, 0:G].rearrange("p (t g) -> p t g", g=4), axis=AX.X, op=ALU.max), reads=[bRt], writes=[bRt])
                        P.op("dve", lambda e: e.tensor_tensor(out=isb[:, 0:G].rearrange("p (t g) -> p t g", g=4), in0=gs[:, 0:G].rearrange("p (t g) -> p t g", g=4),
                                                               in1=gmx[:, 0:nt].unsqueeze(2).to_broadcast([128, nt, 4]), op=ALU.is_ge), reads=[bRt], writes=[bRt])
                        mv = msk[:, 0:nt, :].rearrange("p t (g k) -> p (t g) k", k=4)
                        P.op("dve", lambda e: e.tensor_tensor(out=mv, in0=sv, in1=thr[:, 0:G].unsqueeze(2).to_broadcast([128, G, 4]), op=ALU.is_ge), reads=[bRt], writes=[bRt])
                        P.op("dve", lambda e: e.tensor_tensor(out=mv, in0=mv, in1=isb[:, 0:G].unsqueeze(2).to_broadcast([128, G, 4]), op=ALU.mult), reads=[bRt], writes=[bRt])
                        P.op("dve", lambda e: e.tensor_copy(out=sel_all[:, T0:T0 + nt, :], in_=msk[:, 0:nt, :]), reads=[bRt], writes=[bSel])
                        P.op("dve", lambda e: e.tensor_tensor(out=msk[:, 0:nt, :], in0=msk[:, 0:nt, :], in1=sg[:, 0:nt, :], op=ALU.mult), reads=[bRt], writes=[bRt])
                        P.op("dve", lambda e: e.tensor_reduce(out=den[:, 0:nt], in_=msk[:, 0:nt, :], axis=AX.X, op=ALU.add), reads=[bRt], writes=[bRt])
                        P.op("dve", lambda e: e.reciprocal(out=den[:, 0:nt], in_=den[:, 0:nt]), reads=[bRt], writes=[bRt])
                        P.op("dve", lambda e: e.tensor_tensor(out=g_all[:, T0:T0 + nt, :], in0=msk[:, 0:nt, :], in1=den[:, 0:nt].unsqueeze(2).to_broadcast([128, nt, 16]), op=ALU.mult), reads=[bRt], writes=[bGa])
                    P.barrier()
                with ExitStack() as s2:
                    G_ = lambda name, shape, dt=F32: sbt(s2, name, shape, dt)
                    tri = G_("tri", [128, 128]); k9 = G_("k9", [128, 16, 9]); k32 = G_("k32", [128, 32, 16]); pidx = G_("pidx", [128, 1]); bK = P.buf("sortconst")
                    P.dma("sp", tri[:], I["tri"], writes=[bK])
                    P.dma("sp", k9[:], I["k9"], writes=[bK])
                    P.dma("sp", k32[:], I["k32"], writes=[bK])
                    P.dma("sp", pidx[:], I["pidx"], writes=[bK])
                    cs = G_("cs", [128, 35, 16]); bCs = P.buf("cs")
                    rk = G_("rk", [128, 34, 16]); slot = G_("slot", [128, 34, 16]); s3 = G_("s3", [128, 34, 16]); bSm = P.buf("sortmath")
                    cnt = G_("cnt", [128, 16]); c9 = G_("c9", [128, 16, 9]); nte = G_("nte", [128, 16]); padc = G_("padc", [128, 16]); offend = G_("offend", [128, 16]); offm = G_("offm", [128, 16])
                    c32 = G_("c32", [128, 32, 16]); ek = G_("ek", [128, 32]); sA = G_("sA", [128, 34]); sB = G_("sB", [128, 34]); gsum = G_("gsum", [128, 34])
                    P.op("dve", lambda e: e.memset(cs[:, 0, :], 0.0), writes=[bCs])
                    for t in range(1, 35):
                        P.op("dve", lambda e: e.tensor_tensor(out=cs[:, t, :], in0=cs[:, t - 1, :], in1=sel_all[:, t - 1, :], op=ALU.add), reads=[bSel, bCs], writes=[bCs])
                    fl = lambda ap: ap.rearrange("p t e -> p (t e)")
                    P.op("pe", lambda e: e.matmul(psb[0][:, 0:512], lhsT=tri[:], rhs=fl(sel_all[:, 0:32, :]), start=True, stop=False), reads=[bK, bSel], writes=[pB[0]], inc=False)
                    P.op("pe", lambda e: e.matmul(psb[0][:, 0:512], lhsT=onesf[:], rhs=fl(cs[:, 0:32, :]), start=False, stop=True), reads=[bC, bCs], writes=[pB[0]])
                    P.op("pe", lambda e: e.matmul(psb[1][:, 0:32], lhsT=tri[:], rhs=fl(sel_all[:, 32:34, :]), start=True, stop=False), reads=[bK, bSel], writes=[pB[1]], inc=False)
                    P.op("pe", lambda e: e.matmul(psb[1][:, 0:32], lhsT=onesf[:], rhs=fl(cs[:, 32:34, :]), start=False, stop=True), reads=[bC, bCs], writes=[pB[1]])
                    P.op("pe", lambda e: e.matmul(psb[2][:, 0:16], lhsT=onesf[:], rhs=cs[:, 34, :], start=True, stop=True), reads=[bC, bCs], writes=[pB[2]])
                    P.op("act", lambda e: e.activation(out=fl(rk[:, 0:32, :]), in_=psb[0][:, 0:512], func=AF.Identity), reads=[pB[0]], writes=[bSm])
                    P.op("act", lambda e: e.activation(out=fl(rk[:, 32:34, :]), in_=psb[1][:, 0:32], func=AF.Identity), reads=[pB[1]], writes=[bSm])
                    P.op("act", lambda e: e.activation(out=cnt[:], in_=psb[2][:, 0:16], func=AF.Identity), reads=[pB[2]], writes=[bSm])
                    D_ = lambda fn, rd=(): P.op("dve", fn, reads=[bSm, bK, bSel, bGa] + list(rd), writes=[bSm])
                    D_(lambda e: e.tensor_tensor(out=c9[:], in0=cnt[:].unsqueeze(2).to_broadcast([128, 16, 9]), in1=k9[:], op=ALU.is_gt))
                    D_(lambda e: e.tensor_reduce(out=nte[:], in_=c9[:], axis=AX.X, op=ALU.add))
                    D_(lambda e: e.tensor_scalar(out=padc[:], in0=nte[:], scalar1=512.0, scalar2=None, op0=ALU.mult))
                    D_(lambda e: e.tensor_copy(out=offend[:, 0:1], in_=padc[:, 0:1]))
                    for ex in range(1, 16):
                        D_(lambda e: e.tensor_tensor(out=offend[:, ex:ex + 1], in0=offend[:, ex - 1:ex], in1=padc[:, ex:ex + 1], op=ALU.add))
                    D_(lambda e: e.tensor_tensor(out=offm[:], in0=offend[:], in1=padc[:], op=ALU.subtract))
                    D_(lambda e: e.tensor_tensor(out=s3[:], in0=rk[:], in1=offm[:].unsqueeze(1).to_broadcast([128, 34, 16]), op=ALU.add))
                    D_(lambda e: e.tensor_tensor(out=s3[:], in0=s3[:], in1=sel_all[:], op=ALU.mult))
                    D_(lambda e: e.tensor_reduce(out=sB[:], in_=s3[:], axis=AX.X, op=ALU.max))
                    D_(lambda e: e.tensor_scalar(out=slot[:], in0=sel_all[:], scalar1=-BIG, scalar2=BIG, op0=ALU.mult, op1=ALU.add))
                    D_(lambda e: e.tensor_tensor(out=slot[:], in0=slot[:], in1=s3[:], op=ALU.add))
                    D_(lambda e: e.tensor_reduce(out=sA[:], in_=slot[:], axis=AX.X, op=ALU.min))
                    D_(lambda e: e.tensor_tensor(out=slot[:], in0=slot[:], in1=sA[:].unsqueeze(2).to_broadcast([128, 34, 16]), op=ALU.is_equal))
                    D_(lambda e: e.tensor_tensor(out=slot[:], in0=slot[:], in1=g_all[:], op=ALU.mult))
                    P.op("dve", lambda e: e.tensor_reduce(out=gA[:], in_=slot[:], axis=AX.X, op=ALU.add), reads=[bSm], writes=[bSlots])
                    D_(lambda e: e.tensor_reduce(out=gsum[:], in_=g_all[:], axis=AX.X, op=ALU.add))
                    P.op("dve", lambda e: e.tensor_tensor(out=gB[:], in0=gsum[:], in1=gA[:], op=ALU.subtract), reads=[bSm, bSlots], writes=[bSlots])
                    P.op("dve", lambda e: e.tensor_copy(out=slotA_i[:], in_=sA[:]), reads=[bSm], writes=[bSlots])
                    P.op("dve", lambda e: e.tensor_copy(out=slotB_i[:], in_=sB[:]), reads=[bSm], writes=[bSlots])
                    D_(lambda e: e.tensor_tensor(out=c32[:], in0=k32[:], in1=offend[:].unsqueeze(1).to_broadcast([128, 32, 16]), op=ALU.is_ge))
                    D_(lambda e: e.tensor_reduce(out=ek[:], in_=c32[:], axis=AX.X, op=ALU.add))
                    D_(lambda e: e.tensor_scalar(out=ek[:], in0=ek[:], scalar1=15.0, scalar2=None, op0=ALU.min))
                    D_(lambda e: e.tensor_scalar(out=ek[:], in0=ek[:], scalar1=128.0, scalar2=pidx[:, 0:1], op0=ALU.mult, op1=ALU.add))
                    D_(lambda e: e.tensor_scalar(out=ek[:], in0=ek[:], scalar1=float(l * 2048), scalar2=None, op0=ALU.add))
                    P.op("dve", lambda e: e.tensor_copy(out=widx_i[:], in_=ek[:]), reads=[bSm], writes=[bSlots])
                    for t in range(ntile):
                        for si in (slotA_i, slotB_i):
                            P.idma(tbl[:, :], tokidx[:, t:t + 1], bass.IndirectOffsetOnAxis(ap=si[:, t:t + 1], axis=0), None, NSL - 1, reads=[bSlots, bTok, bTbl], writes=[])
                    if "dbg_sort" in dbg:
                        P.dma("sp", dbg_sort[:, 0:34], sA[:], reads=[bSm])
                        P.dma("sp", dbg_sort[:, 34:68], sB[:], reads=[bSm])
                        P.dma("sp", dbg_sort[:, 68:102], gA[:], reads=[bSlots])
                        P.dma("sp", dbg_sort[:, 102:136], gB[:], reads=[bSlots])
                        P.dma("sp", dbg_sort[:, 136:168], ek[:], reads=[bSm])
                        P.dma("sp", dbg_sort[:, 168:184], cnt[:], reads=[bSm])
                    P.barrier()
                with ExitStack() as s3_:
                    G_ = lambda name, shape, dt=F32: sbt(s3_, name, shape, dt)
                    stg = [G_("stg%d" % i, [128, 4096]) for i in range(3)]; bStg = P.bufs(3, "stg")
                    wgb = [G_("wgb%d" % i, [128, 8, 4, 128], BF16) for i in range(2)]; bWg = P.bufs(2, "wgb"); bWgD = P.bufs(2, "wgbD"); bWgP = P.bufs(2, "wgbP")
                    wub = [G_("wub%d" % i, [128, 8, 4, 128], BF16) for i in range(2)]; bWu = P.bufs(2, "wub"); bWuP = P.bufs(2, "wubP")
                    wdb = [G_("wdb%d" % i, [128, 4, 1024], BF16) for i in range(2)]; bWdA = P.bufs(2, "wdbA"); bWdB = P.bufs(2, "wdbB")
                    idxk = [G_("idxk%d" % i, [128, 4], I32) for i in range(2)]; bIdx = P.bufs(2, "idxk")
                    xg = [G_("xg%d" % i, [128, 4, 1024], BF16) for i in range(2)]; bXg = [P.bufs(4, "xg%d_" % i) for i in range(2)]
                    hT_ = [G_("hTm%d" % i, [128, 8, 512], BF16) for i in range(2)]; bHT = [P.bufs(4, "hTm%d_" % i) for i in range(2)]
                    aT = [G_("aT%d" % i, [128, 4, 512], BF16) for i in range(2)]; bAT = P.bufs(2, "aT")
                    sgu = [G_("sgu%d" % i, [128, 512]) for i in range(2)]; bSg = P.bufs(2, "sgu")
                    yp = [G_("yp%d" % i, [128, 1024]) for i in range(2)]; bYpA = P.bufs(2, "ypA"); bYpB = P.bufs(2, "ypB")
                    for i in range(2):
                        P.op("pool", lambda e: e.memset(xg[i][:], 0.0), writes=bXg[i])
                    wviews = (I["exp_w_gate"].rearrange("l e (p j) f -> (l e p) (j f)", j=8),
                              I["exp_w_up"].rearrange("l e (p j) f -> (l e p) (j f)", j=8),
                              I["exp_w_down"].rearrange("l e (p j) d -> (l e p) (j d)", j=4))

                    def prefetch(k):
                        ks = k % 2
                        P.dma("sp", idxk[ks][:], tbl[k * 512:(k + 1) * 512, :].rearrange("(p j) o -> p (j o)", p=128), reads=[bTbl], writes=[bIdx[ks]])
                        for j in range(4):
                            P.idma(xg[ks][:, j, :], hfD[:, :], None, bass.IndirectOffsetOnAxis(ap=idxk[ks][:, j:j + 1], axis=0), NTOK - 1, reads=[bIdx[ks], bHfD], writes=[bXg[ks][j]])
                        for m in range(3):
                            P.idma(stg[m][:], wviews[m], None, bass.IndirectOffsetOnAxis(ap=widx_i[:, k:k + 1], axis=0), 4095, reads=[bSlots], writes=[bStg[m]])

                    def casts(k):
                        ks = k % 2
                        gv = stg[0][:].rearrange("p (jd m jf) -> p jd m jf", jd=8, jf=4)
                        uv = stg[1][:].rearrange("p (jd m jf) -> p jd m jf", jd=8, jf=4)
                        dv = stg[2][:].rearrange("p (jf d) -> p jf d", jf=4)
                        for jf in (0, 1):
                            P.op("act", lambda e: e.activation(out=wgb[ks][:, :, jf, :], in_=gv[:, :, :, jf], func=AF.Identity), reads=[bStg[0]], writes=[bWg[ks]])
                        P.op("dve", lambda e: e.tensor_copy(out=wgb[ks][:, :, 2, :], in_=gv[:, :, :, 2]), reads=[bStg[0]], writes=[bWgD[ks]])
                        P.op("pool", lambda e: e.tensor_copy(out=wgb[ks][:, :, 3, :], in_=gv[:, :, :, 3]), reads=[bStg[0]], writes=[bWgP[ks]])
                        for jf in (0, 1, 2):
                            P.op("dve", lambda e: e.tensor_copy(out=wub[ks][:, :, jf, :], in_=uv[:, :, :, jf]), reads=[bStg[1]], writes=[bWu[ks]])
                        P.op("pool", lambda e: e.tensor_copy(out=wub[ks][:, :, 3, :], in_=uv[:, :, :, 3]), reads=[bStg[1]], writes=[bWuP[ks]])
                        P.op("act", lambda e: e.activation(out=wdb[ks][:, 0:2, :], in_=dv[:, 0:2, :], func=AF.Identity), reads=[bStg[2]], writes=[bWdA[ks]])
                        P.op("dve", lambda e: e.tensor_copy(out=wdb[ks][:, 2:4, :], in_=dv[:, 2:4, :]), reads=[bStg[2]], writes=[bWdB[ks]])

                    prefetch(0)
                    tcount = 0
                    for k in range(NTL):
                        ks = k % 2
                        for s in range(4):
                            bank = 4 + (tcount % 2)
                            pT = psb[bank][:].bitcast(BF16)
                            xv = xg[ks][:, s, :].rearrange("p (m j) -> p j m", j=8)
                            for jd in range(8):
                                P.op("pe", lambda e: e.transpose(out=pT[:, jd * 128:(jd + 1) * 128], in_=xv[:, jd, :], identity=identb[:]),
                                     reads=[bXg[ks][s], bC], writes=[pB[bank]], inc=(jd == 7))
                            dst = hT_[ks][:, :, s * 128:(s + 1) * 128]
                            src = pT[:, 0:1024].rearrange("p (j m) -> p j m", j=8)
                            if tcount % 2 == 0:
                                P.op("act", lambda e: e.activation(out=dst, in_=src, func=AF.Identity), reads=[pB[bank]], writes=[bHT[ks][s]])
                            else:
                                P.op("dve", lambda e: e.tensor_copy(out=dst, in_=src), reads=[pB[bank]], writes=[bHT[ks][s]])
                            tcount += 1
                        casts(k)
                        if k + 1 < NTL:
                            prefetch(k + 1)
                        for jf in range(4):
                            pg = (jf % 2) * 2
                            for (bank, wt, bw) in ((pg, wgb, [bWg[ks], bWgD[ks], bWgP[ks]]), (pg + 1, wub, [bWu[ks], bWuP[ks]])):
                                for jd in range(8):
                                    P.op("pe", lambda e: e.matmul(psb[bank][:, :], lhsT=wt[ks][:, jd, jf, :], rhs=hT_[ks][:, jd, :], start=(jd == 0), stop=(jd == 7)),
                                         reads=bw + bHT[ks], writes=[pB[bank]], inc=(jd == 7))
                            ssl = jf % 2
                            P.op("act", lambda e: e.activation(out=sgu[ssl][:], in_=psb[pg][:, :], func=AF.Silu), reads=[pB[pg]], writes=[bSg[ssl]])
                            P.op("dve", lambda e: e.tensor_tensor(out=aT[ks][:, jf, :], in0=psb[pg + 1][:, :], in1=sgu[ssl][:], op=ALU.mult), reads=[pB[pg + 1], bSg[ssl]], writes=[bAT[ks]])
                        for s in range(4):
                            ys = s % 2
                            b0 = 6 if s % 2 == 0 else 0
                            for dh in range(2):
                                bank = b0 + dh
                                for jf in range(4):
                                    P.op("pe", lambda e: e.matmul(psb[bank][:, :], lhsT=aT[ks][:, jf, s * 128:(s + 1) * 128], rhs=wdb[ks][:, jf, dh * 512:(dh + 1) * 512], start=(jf == 0), stop=(jf == 3)),
                                         reads=[bAT[ks], bWdA[ks], bWdB[ks]], writes=[pB[bank]], inc=(jf == 3))
                            P.op("act", lambda e: e.activation(out=yp[ys][:, 0:512], in_=psb[b0][:, :], func=AF.Identity), reads=[pB[b0]], writes=[bYpA[ys]])
                            P.op("dve", lambda e: e.tensor_copy(out=yp[ys][:, 512:1024], in_=psb[b0 + 1][:, :]), reads=[pB[b0 + 1]], writes=[bYpB[ys]])
                            P.dma("sp", ypair[k * 512:(k + 1) * 512, :].rearrange("(p j) d -> p j d", j=4)[:, s, :], yp[ys][:], reads=[bYpA[ys], bYpB[ys]], writes=[bYp])
                    P.barrier()
                with ExitStack() as s4_:
                    G_ = lambda name, shape, dt=F32: sbt(s4_, name, shape, dt)
                    ya = [G_("ya%d" % i, [128, 1024]) for i in range(3)]; bYa = P.bufs(3, "ya")
                    yb = [G_("yb%d" % i, [128, 1024]) for i in range(3)]; bYb = P.bufs(3, "yb")
                    mx = [G_("emx%d" % i, [128, 1024]) for i in range(3)]; bMx = P.bufs(3, "emx")
                    ms = [G_("ems%d" % i, [128, 1]) for i in range(3)]; bMs = P.bufs(3, "ems")
                    fng = G_("fng", [128, 1024]); bFn = P.buf("fng")
                    if last:
                        P.dma("sp", fng[:], I["fng_bc"], writes=[bFn])
                    for t in range(ntile):
                        sl = t % 3
                        w = 0 if t < 32 else 1
                        tg = t * 128
                        P.idma(ya[sl][:], ypair[:, :], None, bass.IndirectOffsetOnAxis(ap=slotA_i[:, t:t + 1], axis=0), NSL - 1, reads=[bSlots, bYp], writes=[bYa[sl]])
                        P.idma(yb[sl][:], ypair[:, :], None, bass.IndirectOffsetOnAxis(ap=slotB_i[:, t:t + 1], axis=0), NSL - 1, reads=[bSlots, bYp], writes=[bYb[sl]])
                        P.dma("sp", mx[sl][:], xm[tg:tg + 128, :], reads=[bXm], writes=[bMx[sl]])
                        P.op("dve", lambda e: e.tensor_scalar(out=ya[sl][:], in0=ya[sl][:], scalar1=gA[:, t:t + 1], scalar2=None, op0=ALU.mult), reads=[bYa[sl], bSlots], writes=[bYa[sl]])
                        P.op("dve", lambda e: e.scalar_tensor_tensor(out=ya[sl][:], in0=yb[sl][:], scalar=gB[:, t:t + 1], in1=ya[sl][:], op0=ALU.mult, op1=ALU.add), reads=[bYa[sl], bYb[sl], bSlots], writes=[bYa[sl]])
                        P.op("dve", lambda e: e.tensor_tensor(out=ya[sl][:], in0=ya[sl][:], in1=gbc[:, 2 + w, :], op=ALU.mult), reads=[bYa[sl], bGbc], writes=[bYa[sl]])
                        P.op("dve", lambda e: e.tensor_tensor(out=ya[sl][:], in0=ya[sl][:], in1=mx[sl][:], op=ALU.add), reads=[bYa[sl], bMx[sl]], writes=[bYa[sl]])
                        if last:
                            P.op("act", lambda e: e.activation(out=junk2[:], in_=ya[sl][:], func=AF.Square, accum_out=ms[sl][:]), reads=[bYa[sl]], writes=[bJ2, bMs[sl]])
                            P.op("act", lambda e: e.activation(out=ms[sl][:], in_=ms[sl][:], func=AF.Sqrt, bias=epsT[:, 0:1], scale=1.0 / D), reads=[bMs[sl], bC], writes=[bMs[sl]])
                            P.op("dve", lambda e: e.reciprocal(out=ms[sl][:], in_=ms[sl][:]), reads=[bMs[sl]], writes=[bMs[sl]])
                            P.op("dve", lambda e: e.scalar_tensor_tensor(out=ya[sl][:], in0=ya[sl][:], scalar=ms[sl][:, 0:1], in1=fng[:], op0=ALU.mult, op1=ALU.mult), reads=[bYa[sl], bMs[sl], bFn], writes=[bYa[sl]])
                            P.dma("act", out[tg:tg + 128, :], ya[sl][:], reads=[bYa[sl]], writes=[bOut])
                        else:
                            P.dma("act", xs1[tg:tg + 128, :], ya[sl][:], reads=[bYa[sl]], writes=[bXs1])
                    P.barrier()
                P.barrier()
            if os.environ.get("MK_STOP") == "M":
                break
        P.barrier()
    return nc, list(I.keys())


_PROG = {}


def _dt_of(a):
    if a.dtype == np.int32:
        return I32
    return BF16 if a.dtype == ml_dtypes.bfloat16 else F32


def kernel(**inputs):
    inp = {k: np.asarray(v) for k, v in inputs.items()}
    shared = prep_shared(inp)
    debug = tuple(os.environ.get("MK_DEBUG", "").split(",")) if os.environ.get("MK_DEBUG") else ()
    key = (debug, os.environ.get("MK_STOP"))
    if key not in _PROG:
        shapes = {k: (v.shape, _dt_of(v)) for k, v in shared.items()}
        _PROG[key] = build_program(shapes, debug)
    nc, used = _PROG[key]
    x = np.ascontiguousarray(inp["x"], dtype=np.float32)
    ctx = np.ascontiguousarray(inp["ctx"], dtype=np.float32)
    c = np.asarray(inp["c"], np.float32)
    cc = _cols(inp["c_ctx"])
    in_maps = []
    for b in range(8):
        m = {k: shared[k] for k in used}
        m["x"] = x[b]
        m["ctx"] = ctx[b]
        m["ccols"] = np.ascontiguousarray(np.stack([_cols(c[b]), cc], axis=-1))
        in_maps.append(m)
    res = run_bass_kernel_spmd(nc, in_maps, core_ids=list(range(8)))
    if debug:
        kernel.last = res.results
    return np.stack([np.asarray(r["out"], dtype=np.float32) for r in res.results], axis=0)
```

```python
import os
import numpy as np
import ml_dtypes
from contextlib import ExitStack
import concourse.bass as bass
import concourse.mybir as mybir
from concourse.bass_utils import run_bass_kernel_spmd

F32 = mybir.dt.float32
BF16 = mybir.dt.bfloat16
I32 = mybir.dt.int32
AF = mybir.ActivationFunctionType
ALU = mybir.AluOpType
AX = mybir.AxisListType

D = 1024
L = 4096
LC = 256
NTOK = L + LC
DEPTH = 2
EPS = 1e-6
HY_OFF, GM_OFF, CV_OFF, MQ_OFF, MKV_OFF, KR_OFF, IN_COLS = 0, 768, 1280, 1792, 1984, 2112, 2144
NE = 16
FE = 512
PI = float(np.pi)


class Buf:
    __slots__ = ("name", "w", "r")

    def __init__(self, name):
        self.name = name
        self.w = None
        self.r = []


class Eng:
    def __init__(self, name, eng, sem):
        self.name = name
        self.eng = eng
        self.sem = sem
        self.count = 0
        self.waited = {}


class Prog:
    def __init__(self, nc, stack, n_dma_sems=32):
        self.nc = nc
        self.engs = {}
        for name, e in (("pe", nc.tensor), ("act", nc.scalar), ("dve", nc.vector),
                        ("pool", nc.gpsimd), ("sp", nc.sync)):
            sem = stack.enter_context(nc.semaphore("s_" + name))
            self.engs[name] = Eng(name, e, sem)
        self.dma_sems = []
        for i in range(n_dma_sems):
            sem = stack.enter_context(nc.semaphore("s_dma%d" % i))
            self.dma_sems.append([sem, 0, "dma%d" % i])
        self.dma_rr = 0
        self.nbuf = 0

    def buf_id(self):
        self.nbuf += 1
        return self.nbuf

    def buf(self, name=None):
        self.nbuf += 1
        return Buf(name or "b%d" % self.nbuf)

    def bufs(self, n, name="b"):
        return [self.buf("%s%d" % (name, i)) for i in range(n)]

    def _wait(self, E, tok):
        if tok is None:
            return
        key, sem, val = tok
        if key == E.name and (E.name == "pe" or val > E.count):
            return
        if E.waited.get(key, 0) >= val:
            return
        E.eng.wait_ge(sem, val)
        E.waited[key] = val

    def _deps(self, E, reads, writes):
        for b in reads:
            self._wait(E, b.w)
        for b in writes:
            self._wait(E, b.w)
            for t in b.r:
                self._wait(E, t)

    def _commit(self, tok, reads, writes):
        for b in reads:
            b.r.append(tok)
            if len(b.r) > 16:
                latest = {}
                for t in b.r:
                    if t[0] not in latest or latest[t[0]][2] < t[2]:
                        latest[t[0]] = t
                b.r = list(latest.values())
        for b in writes:
            b.w = tok
            b.r = []

    def op(self, en, fn, reads=(), writes=(), inc=True):
        E = self.engs[en]
        self._deps(E, reads, writes)
        ins = fn(E.eng)
        if inc:
            E.count += 1
            ins.then_inc(E.sem, 1)
            tok = (E.name, E.sem, E.count)
        else:
            tok = (E.name, E.sem, E.count + 1)
        self._commit(tok, reads, writes)
        return tok

    def dma(self, en, out, in_, reads=(), writes=(), **kw):
        E = self.engs[en]
        slot = self.dma_sems[self.dma_rr]
        self.dma_rr = (self.dma_rr + 1) % len(self.dma_sems)
        sem, cnt, key = slot
        if cnt > 0:
            self._wait(E, (key, sem, cnt))
        self._deps(E, reads, writes)
        ins = E.eng.dma_start(out=out, in_=in_, **kw)
        cnt += 16
        slot[1] = cnt
        ins.then_inc(sem, 16)
        tok = (key, sem, cnt)
        self._commit(tok, reads, writes)
        return tok

    def idma(self, out, in_, out_off, in_off, bounds, reads=(), writes=(), breg=None):
        E = self.engs["pool"]
        slot = self.dma_sems[self.dma_rr]
        self.dma_rr = (self.dma_rr + 1) % len(self.dma_sems)
        sem, cnt, key = slot
        if cnt > 0:
            self._wait(E, (key, sem, cnt))
        self._deps(E, reads, writes)
        if breg is not None:
            ins = E.eng.indirect_dma_start(out=out, out_offset=out_off, in_=in_, in_offset=in_off, bounds_check=breg, oob_is_err=False)
        else:
            ins = E.eng.indirect_dma_start(out=out, out_offset=out_off, in_=in_, in_offset=in_off)
        cnt += 16
        slot[1] = cnt
        ins.then_inc(sem, 16)
        tok = (key, sem, cnt)
        self._commit(tok, reads, writes)
        return tok

    def barrier(self):
        toks = [(E.name, E.sem, E.count) for E in self.engs.values() if E.count > 0]
        toks += [(k, s, c) for s, c, k in self.dma_sems if c > 0]
        for E in self.engs.values():
            for t in toks:
                self._wait(E, t)


_CONST = {}


def _bf(a):
    return np.ascontiguousarray(a.astype(ml_dtypes.bfloat16))


def _dft_slabs(Lh):
    N = 2 * Lh
    NT = Lh // 128
    t = np.arange(Lh, dtype=np.int64)
    F = np.empty((2 * Lh, Lh), np.float32)
    for r0 in range(0, Lh, 256):
        r = np.arange(r0, r0 + 256, dtype=np.int64)[:, None]
        ang = ((r * t[None, :]) % N).astype(np.float64) * (2 * np.pi / N)
        F[r0:r0 + 256] = np.cos(ang)
        F[Lh + r0:Lh + r0 + 256] = -np.sin(ang)
    F[Lh] = np.where(t % 2 == 0, 1.0, -1.0)
    Fb = F.astype(ml_dtypes.bfloat16)
    del F
    NS = max(NT // 2, 1)
    fwd = np.empty((NS, 128, NT, 512), ml_dtypes.bfloat16)
    for s in range(NS):
        rcs = [2 * s, 2 * s + 1, NT + 2 * s, NT + 2 * s + 1]
        for j, rc in enumerate(rcs):
            blk = Fb[rc * 128:(rc + 1) * 128, :]
            fwd[s, :, :, j * 128:(j + 1) * 128] = blk.T.reshape(NT, 128, 128).transpose(1, 0, 2)
    inv = np.empty((NS, 128, 2 * NT, 256), ml_dtypes.bfloat16)
    for s in range(NS):
        blk = Fb[:, s * 256:(s + 1) * 256]
        inv[s] = blk.reshape(2 * NT, 128, 256).transpose(1, 0, 2)
    rs = np.full((2 * Lh,), 2.0 / N, np.float32)
    rs[0] = 1.0 / N
    rs[Lh] = 1.0 / N
    rowscale = np.ascontiguousarray(rs.reshape(2 * NT, 128).T)
    return fwd, inv, rowscale


def _hy_tables(Lh):
    t = np.linspace(0.0, 1.0, Lh, dtype=np.float32)[:, None]
    w = (2.0 * np.pi * np.arange(Lh, dtype=np.float32)[:, None] / Lh).astype(np.float32)
    f = np.linspace(1e-4, 15, 16, dtype=np.float32)[None, :]
    z = np.concatenate([t, np.cos(f * w), -np.sin(f * w)], axis=-1).astype(np.float32)
    deltas = np.abs(np.linspace(np.log(1e-2) / 1.5, np.log(1e-2) / 0.3, 256, dtype=np.float32))
    decay = np.exp(-t * deltas[None, :]).astype(np.float32)
    NT = Lh // 128
    zT = np.ascontiguousarray(z.T)
    dec = np.ascontiguousarray(decay.reshape(NT, 128, 256).transpose(1, 0, 2))
    return zT, dec


def _consts():
    if _CONST:
        return _CONST
    c = {}
    c["ident"] = np.eye(128, dtype=np.float32)
    c["onesf"] = np.ones((128, 128), np.float32)
    bo = np.zeros((128, 128), np.float32)
    bo[:64, :64] = 1.0 / 64
    bo[64:, 64:] = 1.0 / 64
    c["blk64"] = bo
    row = np.repeat(np.arange(L // 64), 64).astype(np.float32)
    col = np.tile(np.arange(64), L // 64).astype(np.float32)
    inv = (10000.0 ** (-np.arange(8, dtype=np.float32) / 8)).astype(np.float32)
    ang = np.concatenate([row[:, None] * inv, col[:, None] * inv], axis=-1).astype(np.float32)
    c["ropec"] = np.ascontiguousarray(np.cos(ang).T.astype(np.float32))
    c["ropes"] = np.ascontiguousarray(np.sin(ang).T.astype(np.float32))
    sel = np.zeros((16, 16, 128), np.float32)
    for e in range(16):
        sel[e, e, :] = 1.0
    c["sel"] = sel
    c["tri"] = np.triu(np.ones((128, 128), np.float32), k=1)
    c["k9"] = np.ascontiguousarray(np.broadcast_to((512.0 * np.arange(9, dtype=np.float32))[None, None, :], (128, 16, 9)))
    c["k32"] = np.ascontiguousarray(np.broadcast_to((512.0 * np.arange(32, dtype=np.float32))[None, :, None], (128, 32, 16)))
    c["pidx"] = np.arange(128, dtype=np.float32).reshape(128, 1)
    c["tokidx"] = np.ascontiguousarray((np.arange(34, dtype=np.int32)[None, :] * 128 + np.arange(128, dtype=np.int32)[:, None]).astype(np.int32))
    c["tblfill"] = np.zeros((32 * 512, 1), np.int32)
    for Lh, tag in ((L, "L"), (LC, "C")):
        fwd, invs, rsc = _dft_slabs(Lh)
        c["dftf" + tag] = fwd
        c["dfti" + tag] = invs
        c["rsc" + tag] = rsc
        zT, dec = _hy_tables(Lh)
        c["hyz" + tag] = zT
        c["hydec" + tag] = dec
    _CONST.update(c)
    return _CONST


def _cols(v):
    v = np.asarray(v, np.float32)
    return np.ascontiguousarray(v.reshape(-1, 128).T)


def _rep(v):
    v = np.asarray(v, np.float32).reshape(1, -1)
    return np.ascontiguousarray(np.broadcast_to(v, (128, v.shape[1])))


def prep_shared(inp):
    s = {}
    s.update(_consts())
    for k in ("ada_w", "w_in", "w_out", "exp_w_gate", "exp_w_up", "exp_w_down", "hy_f_w1", "hy_f_w2", "hy_f_w3"):
        s[k] = np.ascontiguousarray(inp[k], dtype=np.float32)
    s["w_router"] = np.ascontiguousarray(inp["w_router"], dtype=np.float32)
    s["ada_bT"] = np.stack([_cols(inp["ada_b"][l]) for l in range(DEPTH)])
    s["n1gT"] = np.stack([_cols(inp["norm1_g"][l]) for l in range(DEPTH)])
    s["n2gT"] = np.stack([_cols(inp["norm2_g"][l]) for l in range(DEPTH)])
    s["hsw_bc"] = np.stack([np.stack([_rep(inp["hy_short_w"][l, k]) for k in range(3)], axis=1) for l in range(DEPTH)])
    s["hsb_bc"] = np.stack([_rep(inp["hy_short_b"][l]) for l in range(DEPTH)])
    s["hyb_bc"] = np.stack([_rep(inp["hy_bias"][l].reshape(-1)) for l in range(DEPTH)])
    s["hy_b1c"] = np.ascontiguousarray(np.asarray(inp["hy_f_b1"], np.float32)[:, :, None])
    s["hy_b2c"] = np.ascontiguousarray(np.asarray(inp["hy_f_b2"], np.float32)[:, :, None])
    s["hy_frc"] = np.ascontiguousarray(np.asarray(inp["hy_f_freq"], np.float32)[:, :, None])
    s["gm_lng"] = np.stack([_rep(inp["gm_ln_g"][l]) for l in range(DEPTH)])
    s["gm_lnb"] = np.stack([_rep(inp["gm_ln_b"][l]) for l in range(DEPTH)])
    s["gm_wsT"] = np.ascontiguousarray(np.asarray(inp["gm_ws"], np.float32).transpose(0, 3, 1, 2))
    gb = np.asarray(inp["gm_bs"], np.float32)
    s["gm_bsbc"] = np.ascontiguousarray(np.repeat(gb.transpose(0, 2, 1), 64, axis=2))
    s["cv_w"] = np.ascontiguousarray(np.asarray(inp["cv_dw_w"], np.float32).transpose(0, 2, 1).reshape(DEPTH, 2, 128, 31).transpose(0, 2, 1, 3))
    s["cv_bT"] = np.stack([_cols(inp["cv_dw_b"][l]) for l in range(DEPTH)])
    s["cv_lgT"] = np.stack([_cols(inp["cv_ln_g"][l]) for l in range(DEPTH)])
    s["cv_lbT"] = np.stack([_cols(inp["cv_ln_b"][l]) for l in range(DEPTH)])
    wuq = np.asarray(inp["w_uq"], np.float32).reshape(DEPTH, 192, 4, 96)
    wq_n = np.zeros((DEPTH, 192, 4, 128), np.float32)
    wq_n[:, :, :, 64:128] = wuq[:, :, :, 0:64]
    wq_r = np.zeros((DEPTH, 192, 4, 2, 16), np.float32)
    wq_r[:, :, :, 0, :] = wuq[:, :, :, 64:80]
    wq_r[:, :, :, 1, :] = wuq[:, :, :, 80:96]
    s["wq_n"] = wq_n.reshape(DEPTH, 192, 512)
    s["wq_r"] = wq_r.reshape(DEPTH, 192, 128)
    s["qan"] = np.ascontiguousarray(np.asarray(inp["mla_qa_norm"], np.float32)[:, :, None])
    wukv = np.asarray(inp["w_ukv"], np.float32).reshape(DEPTH, 128, 4, 128)
    wk = np.zeros((DEPTH, 128, 4, 128), np.float32)
    wk[:, :, :, 64:128] = wukv[:, :, :, 0:64]
    s["wk_n"] = wk.reshape(DEPTH, 128, 512)
    s["wv"] = np.ascontiguousarray(wukv[:, :, :, 64:128].reshape(DEPTH, 128, 256))
    s["kvn"] = np.ascontiguousarray(np.asarray(inp["mla_kva_norm"], np.float32)[:, :, None])
    s["mixg_bc"] = np.stack([_rep(inp["mix_norm_g"][l]) for l in range(DEPTH)])
    s["rb_bc"] = _rep(inp["router_bias"])
    s["fng_bc"] = _rep(inp["final_norm_g"])
    return s


def build_program(shared_shapes, debug=()):
    nc = bass.Bass("TRN2", target_bir_lowering=False)
    dbg = set(debug)
    class _Lazy(dict):
        def __missing__(self, k):
            shape, dt = shared_shapes[k]
            v = nc.dram_tensor(k, list(shape), dt, kind="ExternalInput").ap()
            self[k] = v
            return v
    I = _Lazy()
    x_in = nc.dram_tensor("x", [L, D], F32, kind="ExternalInput").ap()
    ctx_in = nc.dram_tensor("ctx", [LC, D], F32, kind="ExternalInput").ap()
    ccols = nc.dram_tensor("ccols", [128, 8, 2], F32, kind="ExternalInput").ap()
    out = nc.dram_tensor("out", [L, D], F32, kind="ExternalOutput").ap()

    def scratch(name, shape, dt):
        kind = "ExternalOutput" if name in dbg else "Internal"
        return nc.dram_tensor(name, list(shape), dt, kind=kind).ap()

    xm = scratch("xm", [NTOK, D], F32)
    xs1 = scratch("xs1", [NTOK, D], F32)
    zs = scratch("zs", [NTOK, 768], F32)
    ymix = scratch("ymix", [NTOK, D], F32)
    glu = scratch("glu", [256, NTOK], BF16)
    qT = scratch("qT", [4, 128, NTOK], BF16)
    kT = scratch("kT", [4, 128, NTOK], BF16)
    vv = scratch("vv", [NTOK, 4 * 65], BF16)
    kspec = scratch("kspec", [2 * L, 512], F32)
    kspecC = scratch("kspecC", [2 * LC, 512], F32)
    dbg_mod = scratch("dbg_mod", [128, 48, 2], F32)
    hfD = scratch("hfD", [NTOK, D], BF16)
    ypair = scratch("ypair", [32 * 512, D], F32)
    tbl = scratch("tbl", [32 * 512, 1], I32)
    dbg_sort = scratch("dbg_sort", [128, 184], F32)
    dbg_hT = scratch("dbg_hT", [128, 8, L + 2], BF16)

    with ExitStack() as top:
        P = Prog(nc, top)
        sbt = lambda st, name, shape, dt: st.enter_context(nc.sbuf_tensor("sb_%s_%d" % (name, P.buf_id()), shape, dt))
        psb = [top.enter_context(nc.psum_tensor("ps%d" % i, [128, 512], F32)) for i in range(8)]
        pB = P.bufs(8, "ps")
        ident = sbt(top, "ident", [128, 128], F32)
        identb = sbt(top, "identb", [128, 128], BF16)
        onesf = sbt(top, "onesf", [128, 128], F32)
        blk64 = sbt(top, "blk64", [128, 128], F32)
        modT = sbt(top, "modT", [128, 48, 2], F32)
        gain1 = sbt(top, "gain1", [128, 8, 2], F32)
        gain2 = sbt(top, "gain2", [128, 8, 2], F32)
        gbc = sbt(top, "gbc", [128, 4, 1024], F32)
        epsT = sbt(top, "epsT", [128, 1], F32)
        junk2 = sbt(top, "junk2", [128, 1024], BF16)
        bJ2 = P.buf("junk2")
        bC = P.buf("consts")
        bOut = P.buf("out_dram")
        bXs1 = P.buf("xs1_dram")
        P.op("pool", lambda e: e.memset(epsT[:], EPS), writes=[bC])
        bMod = P.buf("mod")
        bGbc = P.buf("gbc")
        P.dma("sp", ident[:], I["ident"], writes=[bC])
        P.dma("sp", onesf[:], I["onesf"], writes=[bC])
        P.dma("sp", blk64[:], I["blk64"], writes=[bC])
        P.op("dve", lambda e: e.tensor_copy(out=identb[:], in_=ident[:]), reads=[bC], writes=[bC])

        def xin_ap(l, t0, n):
            if l == 0:
                if t0 < L:
                    return x_in[t0:t0 + n, :]
                return ctx_in[t0 - L:t0 - L + n, :]
            return xs1[t0:t0 + n, :]

        BLOCKS = [(b * 512, 4, 0) for b in range(8)] + [(L, 2, 1)]

        for l in range(DEPTH):
            last = (l == DEPTH - 1)
            with ExitStack() as st:
                aw = [sbt(st, "aw%d" % i, [128, 6144], F32) for i in range(2)]
                bAw = P.bufs(2, "aw")
                scs = sbt(st, "scs", [128, 8, 2], F32)
                abT = sbt(st, "abT", [128, 48], F32)
                n1g = sbt(st, "n1g", [128, 8], F32)
                n2g = sbt(st, "n2g", [128, 8], F32)
                tmp = sbt(st, "tmpA", [128, 8, 2], F32)
                diag = [sbt(st, "diag%d" % i, [128, 128], F32) for i in range(2)]
                bDiag = P.bufs(2, "diag")
                bS = P.buf("scs")
                P.dma("sp", scs[:], ccols, writes=[bS])
                P.dma("sp", abT[:], I["ada_bT"][l], writes=[bS])
                P.dma("sp", n1g[:], I["n1gT"][l], writes=[bS])
                P.dma("sp", n2g[:], I["n2gT"][l], writes=[bS])
                P.op("act", lambda e: e.activation(out=scs[:], in_=scs[:], func=AF.Silu), reads=[bS], writes=[bS])
                for kc in range(8):
                    sl = kc % 2
                    P.dma("sp" if kc % 2 == 0 else "pool", aw[sl][:], I["ada_w"][l, kc * 128:(kc + 1) * 128, :], writes=[bAw[sl]])
                    for n in range(48):
                        P.op("pe", lambda e: e.matmul(psb[0][:, 2 * n:2 * n + 2], lhsT=aw[sl][:, n * 128:(n + 1) * 128],
                                                      rhs=scs[:, kc, :], start=(kc == 0 and n == 0), stop=(kc == 7)),
                             reads=[bAw[sl], bS], writes=[pB[0]], inc=(n == 47))
                P.op("dve", lambda e: e.tensor_tensor(out=modT[:], in0=psb[0][:, 0:96].rearrange("p (a b) -> p a b", b=2),
                                                       in1=abT[:].unsqueeze(2).to_broadcast([128, 48, 2]), op=ALU.add),
                     reads=[pB[0], bS], writes=[bMod])
                for (gain, ng, off) in ((gain1, n1g, 8), (gain2, n2g, 32)):
                    P.op("dve", lambda e: e.tensor_scalar(out=tmp[:], in0=modT[:, off:off + 8, :], scalar1=1.0, scalar2=None, op0=ALU.add),
                         reads=[bMod], writes=[bS])
                    P.op("dve", lambda e: e.tensor_tensor(out=gain[:], in0=tmp[:], in1=ng[:].unsqueeze(2).to_broadcast([128, 8, 2]), op=ALU.mult),
                         reads=[bS], writes=[bMod])
                k = 0
                for gi, (off, w) in enumerate(((16, 0), (16, 1), (40, 0), (40, 1))):
                    for half in range(2):
                        bank = 1 + (k % 2)
                        for j in range(4):
                            dc = half * 4 + j
                            dsl = (k * 4 + j) % 2
                            P.op("dve", lambda e: e.tensor_scalar(out=diag[dsl][:], in0=ident[:], scalar1=modT[:, off + dc, w:w + 1], scalar2=None, op0=ALU.mult),
                                 reads=[bMod, bC], writes=[bDiag[dsl]])
                            P.op("pe", lambda e: e.matmul(psb[bank][:, j * 128:(j + 1) * 128], lhsT=onesf[:], rhs=diag[dsl][:], start=(j == 0), stop=True),
                                 reads=[bDiag[dsl], bC], writes=[pB[bank]], inc=True)
                        P.op("act", lambda e: e.activation(out=gbc[:, gi, half * 512:(half + 1) * 512], in_=psb[bank][:], func=AF.Identity),
                             reads=[pB[bank]], writes=[bGbc])
                        k += 1
                if "dbg_mod" in dbg and l == 0:
                    P.dma("sp", dbg_mod, modT[:], reads=[bMod])
                P.barrier()
            if os.environ.get("MK_STOP") == "A":
                break
            with ExitStack() as st:
                hT = [sbt(st, "hTl", [128, 8, L + 2], BF16), sbt(st, "hTc", [128, 8, LC + 2], BF16)]
                bHl = P.bufs(8, "hTl")
                bHc = P.buf("hTc")
                bPad = P.buf("hTpad")
                wb = sbt(st, "wb", [128, 8, IN_COLS - 768], BF16)
                wh = sbt(st, "wh", [128, 8, 3, 768], BF16)
                bW = P.buf("w_in_b")
                for (tt, n) in ((hT[0], L), (hT[1], LC)):
                    P.op("pool", lambda e: e.memset(tt[:, :, 0:1], 0.0), writes=[bPad])
                    P.op("pool", lambda e: e.memset(tt[:, :, n + 1:n + 2], 0.0), writes=[bPad])

                def hbufs(w, tl, n):
                    if w == 1:
                        return [bHc, bPad]
                    lo = max(tl - 1, 0) // 512
                    hi = min(tl + n, L - 1) // 512
                    return [bHl[i] for i in range(lo, hi + 1)] + [bPad]

                with ExitStack() as s1:
                    xt = [sbt(s1, "xt%d" % i, [128, 1024], F32) for i in range(2)]
                    bXt = P.bufs(2, "xt")
                    xn = [sbt(s1, "xn%d" % i, [128, 4, 1024], BF16) for i in range(2)]
                    bXn = P.bufs(2, "xn")
                    junk = sbt(s1, "junk", [128, 1024], BF16)
                    bJ = P.buf("junk")
                    ssq = [sbt(s1, "ssq%d" % i, [128, 1], F32) for i in range(2)]
                    bSs = P.bufs(2, "ssq")
                    wst = [sbt(s1, "wst%d" % i, [128, IN_COLS], F32) for i in range(2)]
                    bWst = P.bufs(2, "wst")
                    hsw = sbt(s1, "hsw", [128, 3, 768], F32)
                    bHsw = P.buf("hsw")
                    P.dma("pool", hsw[:], I["hsw_bc"][l], writes=[bHsw])
                    for dc in range(8):
                        sl = dc % 2
                        P.dma("pool", wst[sl][:], I["w_in"][l, dc * 128:(dc + 1) * 128, :], writes=[bWst[sl]])
                        P.op("pool", lambda e: e.tensor_copy(out=wb[:, dc, :], in_=wst[sl][:, 768:IN_COLS]), reads=[bWst[sl]], writes=[bW])
                        for k in range(3):
                            P.op("pool", lambda e: e.tensor_tensor(out=wh[:, dc, k, :], in0=wst[sl][:, 0:768], in1=hsw[:, k, :], op=ALU.mult),
                                 reads=[bWst[sl], bHsw], writes=[bW])
                    ti = 0
                    for bi, (t0, nt, w) in enumerate(BLOCKS):
                        if last and w == 1 and False:
                            continue
                        xs_ = bi % 2
                        for t in range(nt):
                            sl = ti % 2
                            ti += 1
                            P.dma("sp", xt[sl][:], xin_ap(l, t0 + t * 128, 128), writes=[bXt[sl]])
                            P.op("act", lambda e: e.activation(out=junk[:], in_=xt[sl][:], func=AF.Square, accum_out=ssq[sl][:]),
                                 reads=[bXt[sl]], writes=[bJ, bSs[sl]])
                            P.op("act", lambda e: e.activation(out=ssq[sl][:], in_=ssq[sl][:], func=AF.Sqrt, bias=epsT[:, 0:1], scale=1.0 / D),
                                 reads=[bSs[sl], bC], writes=[bSs[sl]])
                            P.op("dve", lambda e: e.reciprocal(out=ssq[sl][:], in_=ssq[sl][:]), reads=[bSs[sl]], writes=[bSs[sl]])
                            P.op("dve", lambda e: e.tensor_scalar(out=xn[xs_][:, t, :], in0=xt[sl][:], scalar1=ssq[sl][:, 0:1], scalar2=None, op0=ALU.mult),
                                 reads=[bXt[sl], bSs[sl]], writes=[bXn[xs_]])
                        tl0 = t0 if w == 0 else 0
                        hb = [bHl[bi]] if w == 0 else [bHc]
                        for dc in range(8):
                            bank = 6 + dc % 2
                            pT = psb[bank][:].bitcast(BF16)
                            for t in range(nt):
                                P.op("pe", lambda e: e.transpose(out=pT[:, t * 128:(t + 1) * 128], in_=xn[xs_][:, t, dc * 128:(dc + 1) * 128], identity=identb[:]),
                                     reads=[bXn[xs_], bC], writes=[pB[bank]], inc=(t == nt - 1))
                            dst = hT[w][:, dc, 1 + tl0:1 + tl0 + nt * 128]
                            if dc % 2 == 0:
                                P.op("act", lambda e: e.activation(out=dst, in_=pT[:, 0:nt * 128], func=AF.Identity,
                                                                   bias=modT[:, dc, w:w + 1], scale=gain1[:, dc, w:w + 1]),
                                     reads=[pB[bank], bMod], writes=hb)
                            else:
                                P.op("dve", lambda e: e.tensor_scalar(out=dst, in0=pT[:, 0:nt * 128], scalar1=gain1[:, dc, w:w + 1],
                                                                      scalar2=modT[:, dc, w:w + 1], op0=ALU.mult, op1=ALU.add),
                                     reads=[pB[bank], bMod], writes=hb)
                    if "dbg_hT" in dbg and l == 0:
                        P.dma("sp", dbg_hT, hT[0][:], reads=bHl + [bPad])
                    P.barrier()
                if os.environ.get("MK_STOP") == "B1":
                    break
                with ExitStack() as s2:
                    F_ = lambda name, shape, dt=F32: sbt(s2, name, shape, dt)
                    hsb = F_("hsb", [128, 768]); lng = F_("lng", [128, 256]); lnb = F_("lnb", [128, 256]); bsbc = F_("bsbc", [128, 256])
                    wsT = F_("wsT", [128, 4, 128], BF16)
                    wqn = F_("wqn", [128, 2, 512], BF16); wqr = F_("wqr", [128, 2, 128], BF16)
                    wkn = F_("wkn", [128, 512], BF16); wvb = F_("wvb", [128, 256], BF16)
                    wtmp = F_("wtmp", [128, 640]); gcol = F_("gcol", [128, 3]); wsTf = wtmp[:, 0:512].rearrange("p (g i) -> p g i", g=4)
                    bS2 = P.buf("b2consts")
                    P.dma("sp", hsb[:], I["hsb_bc"][l], writes=[bS2])
                    P.dma("sp", lng[:], I["gm_lng"][l], writes=[bS2])
                    P.dma("sp", lnb[:], I["gm_lnb"][l], writes=[bS2])
                    P.dma("sp", bsbc[:], I["gm_bsbc"][l], writes=[bS2])
                    bWt = P.buf("wtmp")
                    P.dma("sp", wsTf, I["gm_wsT"][l], writes=[bWt])
                    P.op("pool", lambda e: e.tensor_copy(out=wsT[:], in_=wsTf), reads=[bWt], writes=[bS2])
                    P.dma("sp", gcol[:, 0:1], I["qan"][l, 0:128, :], writes=[bS2])
                    P.dma("sp", gcol[0:64, 1:2], I["qan"][l, 128:192, :], writes=[bS2])
                    P.dma("sp", gcol[:, 2:3], I["kvn"][l], writes=[bS2])
                    for rc, rows in ((0, 128), (1, 64)):
                        P.dma("sp", wtmp[0:rows, 0:512], I["wq_n"][l, rc * 128:rc * 128 + rows, :], writes=[bWt])
                        P.dma("sp", wtmp[0:rows, 512:640], I["wq_r"][l, rc * 128:rc * 128 + rows, :], writes=[bWt])
                        P.op("dve", lambda e: e.tensor_scalar(out=wqn[0:rows, rc, :], in0=wtmp[0:rows, 0:512], scalar1=gcol[0:rows, rc:rc + 1], scalar2=None, op0=ALU.mult),
                             reads=[bWt, bS2], writes=[bS2])
                        P.op("dve", lambda e: e.tensor_scalar(out=wqr[0:rows, rc, :], in0=wtmp[0:rows, 512:640], scalar1=gcol[0:rows, rc:rc + 1], scalar2=None, op0=ALU.mult),
                             reads=[bWt, bS2], writes=[bS2])
                    P.dma("sp", wtmp[:, 0:512], I["wk_n"][l], writes=[bWt])
                    P.op("dve", lambda e: e.tensor_scalar(out=wkn[:], in0=wtmp[:, 0:512], scalar1=gcol[:, 2:3], scalar2=None, op0=ALU.mult), reads=[bWt, bS2], writes=[bS2])
                    P.dma("sp", wtmp[:, 0:256], I["wv"][l], writes=[bWt])
                    P.op("dve", lambda e: e.tensor_scalar(out=wvb[:], in0=wtmp[:, 0:256], scalar1=gcol[:, 2:3], scalar2=None, op0=ALU.mult), reads=[bWt, bS2], writes=[bS2])
                    zt = [F_("zt%d" % i, [128, 768]) for i in range(2)]; bZt = P.bufs(2, "zt")
                    zg = [F_("zg%d" % i, [128, 512]) for i in range(2)]; bZg = P.bufs(2, "zg")
                    st1 = [F_("st1%d" % i, [128, 2]) for i in range(2)]; bSt = P.bufs(2, "st1")
                    vc = [F_("vc%d" % i, [128, 256]) for i in range(2)]; bVc = P.bufs(2, "vc")
                    vnb = [F_("vnb%d" % i, [128, 256], BF16) for i in range(2)]; bVn = P.bufs(2, "vnb")
                    ygm = [F_("ygm%d" % i, [128, 256]) for i in range(2)]; bYg = P.bufs(2, "ygm")

                    glt = [F_("glt%d" % i, [128, 512], BF16) for i in range(2)]; bGl = P.bufs(2, "glt")
                    cqb = F_("cqb", [128, 2, 512], BF16); sq0 = F_("sq0", [128, 2, 512]); bCq = P.buf("cqb")
                    sig = sq0[:, 1, :]; bSig = bCq
                    rrep = F_("rrep", [128, 512]); bRr = P.buf("rrep")
                    qt = F_("qt", [128, 4, 512], BF16); bQt = P.buf("qt")
                    kt = F_("kt", [128, 4, 512], BF16); bKt = P.buf("kt")
                    vt = F_("vt", [128, 4, 4, 65], BF16); bVt = P.buf("vt")
                    ckvb = cqb[:, 0, :]; sqk = sq0[:, 0, :]; bCk = bCq
                    rrk = rrep; bRk = bRr
                    rcol = F_("rcol", [128, 4]); bRc = P.buf("rcol")
                    rcs = [F_("ropec", [16, 512]), F_("ropes", [16, 512])]; bRope = P.buf("rope")
                    ra = F_("ra", [16, 512]); rb = F_("rb", [16, 512]); rt1 = F_("rt1", [16, 512]); rt2 = F_("rt2", [16, 512]); bR = P.buf("ropetmp")

                    P.op("pool", lambda e: e.memset(qt[:], 0.0), writes=[bQt])
                    P.op("pool", lambda e: e.memset(kt[:], 0.0), writes=[bKt])
                    P.op("pool", lambda e: e.memset(vt[:], 1.0), writes=[bVt])
                    bZs = P.buf("zs_dram"); bYm = P.buf("ymix_dram"); bGlu = P.buf("glu_dram"); bQd = P.buf("q_dram"); bKd = P.buf("k_dram"); bVd = P.buf("v_dram")

                    def rope_apply(src1, src2, dst1, dst2, nb, do_rope, rd, wr):
                        if do_rope:
                            P.op("dve", lambda e: e.tensor_tensor(out=rt1[:, :nb], in0=src1, in1=rcs[0][:, :nb], op=ALU.mult), reads=rd + [bRope], writes=[bR])
                            P.op("pool", lambda e: e.tensor_tensor(out=rt2[:, :nb], in0=src2, in1=rcs[1][:, :nb], op=ALU.mult), reads=rd + [bRope], writes=[bR])
                            P.op("dve", lambda e: e.tensor_tensor(out=dst1, in0=rt1[:, :nb], in1=rt2[:, :nb], op=ALU.subtract), reads=[bR], writes=wr)
                            P.op("dve", lambda e: e.tensor_tensor(out=rt1[:, :nb], in0=src1, in1=rcs[1][:, :nb], op=ALU.mult), reads=rd + [bRope] + wr, writes=[bR])
                            P.op("pool", lambda e: e.tensor_tensor(out=rt2[:, :nb], in0=src2, in1=rcs[0][:, :nb], op=ALU.mult), reads=rd + [bRope] + wr, writes=[bR])
                            P.op("dve", lambda e: e.tensor_tensor(out=dst2, in0=rt1[:, :nb], in1=rt2[:, :nb], op=ALU.add), reads=[bR], writes=wr)
                        else:
                            P.op("dve", lambda e: e.tensor_copy(out=dst1, in_=src1), reads=rd, writes=wr)
                            P.op("dve", lambda e: e.tensor_copy(out=dst2, in_=src2), reads=rd, writes=wr)

                    tix = 0
                    for bi, (t0, nt, w) in enumerate(BLOCKS):
                        nb = nt * 128
                        tl0 = t0 if w == 0 else 0
                        HT = hT[w]
                        full = not (last and w == 1)
                        hb = hbufs(w, tl0, nb)
                        rhs = lambda dc: HT[:, dc, 1 + tl0:1 + tl0 + nb]
                        if w == 0:
                            P.dma("sp", rcs[0][:, :nb], I["ropec"][:, t0:t0 + nb], writes=[bRope])
                            P.dma("sp", rcs[1][:, :nb], I["ropes"][:, t0:t0 + nb], writes=[bRope])
                        if full:
                            for j in range(2):
                                for (bank, c0) in ((4, 512 + j * 128), (5, 768 + j * 128)):
                                    for dc in range(8):
                                        P.op("pe", lambda e: e.matmul(psb[bank][:, :nb], lhsT=wb[:, dc, c0:c0 + 128], rhs=rhs(dc), start=(dc == 0), stop=(dc == 7)),
                                             reads=hb + [bW], writes=[pB[bank]], inc=(dc == 7))
                                P.op("act", lambda e: e.activation(out=sig[:, :nb], in_=psb[5][:, :nb], func=AF.Sigmoid), reads=[pB[5]], writes=[bSig])
                                P.op("dve", lambda e: e.tensor_tensor(out=glt[j][:, :nb], in0=psb[4][:, :nb], in1=sig[:, :nb], op=ALU.mult),
                                     reads=[pB[4], bSig], writes=[bGl[j]])
                                P.dma("pool", glu[j * 128:(j + 1) * 128, t0:t0 + nb], glt[j][:, :nb], reads=[bGl[j]], writes=[bGlu])
                            for (bank, c0, rows, rc) in ((4, 1024, 128, 0), (5, 1152, 64, 1)):
                                for dc in range(8):
                                    P.op("pe", lambda e: e.matmul(psb[bank][0:rows, :nb], lhsT=wb[:, dc, c0:c0 + rows], rhs=rhs(dc), start=(dc == 0), stop=(dc == 7)),
                                         reads=hb + [bW], writes=[pB[bank]], inc=(dc == 7))
                                P.op("act", lambda e: e.activation(out=cqb[0:rows, rc, :nb], in_=psb[bank][0:rows, :nb], func=AF.Identity), reads=[pB[bank]], writes=[bCq])
                                P.op("act", lambda e: e.activation(out=sq0[0:rows, rc, :nb], in_=psb[bank][0:rows, :nb], func=AF.Square), reads=[pB[bank]], writes=[bCq])
                            P.op("pe", lambda e: e.matmul(psb[6][:, :nb], lhsT=onesf[:, :], rhs=sq0[:, 0, :nb], start=True, stop=False), reads=[bCq, bC], writes=[pB[6]], inc=False)
                            P.op("pe", lambda e: e.matmul(psb[6][:, :nb], lhsT=onesf[0:64, :], rhs=sq0[0:64, 1, :nb], start=False, stop=True), reads=[bCq, bC], writes=[pB[6]])
                            P.op("act", lambda e: e.activation(out=rrep[:, :nb], in_=psb[6][:, :nb], func=AF.Sqrt, bias=epsT[:, 0:1], scale=1.0 / 192), reads=[pB[6], bC], writes=[bRr])
                            P.op("dve", lambda e: e.reciprocal(out=rrep[:, :nb], in_=rrep[:, :nb]), reads=[bRr], writes=[bRr])
                            for h in range(4):
                                P.op("pe", lambda e: e.matmul(psb[7][:, :nb], lhsT=wqn[:, 0, h * 128:(h + 1) * 128], rhs=cqb[:, 0, :nb], start=True, stop=False), reads=[bCq, bS2], writes=[pB[7]], inc=False)
                                P.op("pe", lambda e: e.matmul(psb[7][:, :nb], lhsT=wqn[0:64, 1, h * 128:(h + 1) * 128], rhs=cqb[0:64, 1, :nb], start=False, stop=True), reads=[bCq, bS2], writes=[pB[7]])
                                P.op("dve", lambda e: e.tensor_tensor(out=qt[64:128, h, :nb], in0=psb[7][64:128, :nb], in1=rrep[64:128, :nb], op=ALU.mult),
                                     reads=[pB[7], bRr], writes=[bQt])
                                for (bank, c0) in ((4, h * 32), (5, h * 32 + 16)):
                                    P.op("pe", lambda e: e.matmul(psb[bank][0:16, :nb], lhsT=wqr[:, 0, c0:c0 + 16], rhs=cqb[:, 0, :nb], start=True, stop=False), reads=[bCq, bS2], writes=[pB[bank]], inc=False)
                                    P.op("pe", lambda e: e.matmul(psb[bank][0:16, :nb], lhsT=wqr[0:64, 1, c0:c0 + 16], rhs=cqb[0:64, 1, :nb], start=False, stop=True), reads=[bCq, bS2], writes=[pB[bank]])
                                P.op("dve", lambda e: e.tensor_tensor(out=ra[:, :nb], in0=psb[4][0:16, :nb], in1=rrep[0:16, :nb], op=ALU.mult), reads=[pB[4], bRr], writes=[bR])
                                P.op("dve", lambda e: e.tensor_tensor(out=rb[:, :nb], in0=psb[5][0:16, :nb], in1=rrep[0:16, :nb], op=ALU.mult), reads=[pB[5], bRr], writes=[bR])
                                rope_apply(ra[:, :nb], rb[:, :nb], qt[0:16, h, :nb], qt[32:48, h, :nb], nb, w == 0, [bR], [bQt])
                            P.dma("pool", qT[:, :, t0:t0 + nb].rearrange("h p t -> p h t"), qt[:, :, :nb], reads=[bQt], writes=[bQd])
                        for dc in range(8):
                            P.op("pe", lambda e: e.matmul(psb[6][:, :nb], lhsT=wb[:, dc, 1216:1344], rhs=rhs(dc), start=(dc == 0), stop=(dc == 7)),
                                 reads=hb + [bW], writes=[pB[6]], inc=(dc == 7))
                        P.op("act", lambda e: e.activation(out=ckvb[:, :nb], in_=psb[6][:, :nb], func=AF.Identity), reads=[pB[6]], writes=[bCk])
                        P.op("act", lambda e: e.activation(out=sqk[:, :nb], in_=psb[6][:, :nb], func=AF.Square), reads=[pB[6]], writes=[bCk])
                        P.op("pe", lambda e: e.matmul(psb[7][:, :nb], lhsT=onesf[:, :], rhs=sqk[:, :nb], start=True, stop=True), reads=[bCk, bC], writes=[pB[7]])
                        P.op("act", lambda e: e.activation(out=rrk[:, :nb], in_=psb[7][:, :nb], func=AF.Sqrt, bias=epsT[:, 0:1], scale=1.0 / 128), reads=[pB[7], bC], writes=[bRk])
                        P.op("dve", lambda e: e.reciprocal(out=rrk[:, :nb], in_=rrk[:, :nb]), reads=[bRk], writes=[bRk])
                        for t in range(nt):
                            P.op("pe", lambda e: e.matmul(psb[5][:, 32 + t:33 + t], lhsT=sqk[:, t * 128:(t + 1) * 128], rhs=onesf[:, 0:1], start=(t == 0), stop=True),
                                 reads=[bCk, bC], writes=[pB[5]], inc=(t == nt - 1))
                        P.op("act", lambda e: e.activation(out=rcol[:, 0:nt], in_=psb[5][:, 32:32 + nt], func=AF.Sqrt, bias=epsT[:, 0:1], scale=1.0 / 128), reads=[pB[5], bC], writes=[bRc])
                        P.op("dve", lambda e: e.reciprocal(out=rcol[:, 0:nt], in_=rcol[:, 0:nt]), reads=[bRc], writes=[bRc])
                        for h in range(4):
                            P.op("pe", lambda e: e.matmul(psb[7][:, :nb], lhsT=wkn[:, h * 128:(h + 1) * 128], rhs=ckvb[:, :nb], start=True, stop=True), reads=[bCk, bS2], writes=[pB[7]])
                            P.op("dve", lambda e: e.tensor_tensor(out=kt[64:128, h, :nb], in0=psb[7][64:128, :nb], in1=rrk[64:128, :nb], op=ALU.mult),
                                 reads=[pB[7], bRk], writes=[bKt])
                        for (bank, c0) in ((4, 1344), (5, 1360)):
                            for dc in range(8):
                                P.op("pe", lambda e: e.matmul(psb[bank][0:16, :nb], lhsT=wb[:, dc, c0:c0 + 16], rhs=rhs(dc), start=(dc == 0), stop=(dc == 7)),
                                     reads=hb + [bW], writes=[pB[bank]], inc=(dc == 7))
                        P.op("act", lambda e: e.activation(out=ra[:, :nb], in_=psb[4][0:16, :nb], func=AF.Identity), reads=[pB[4]], writes=[bR])
                        P.op("act", lambda e: e.activation(out=rb[:, :nb], in_=psb[5][0:16, :nb], func=AF.Identity), reads=[pB[5]], writes=[bR])
                        rope_apply(ra[:, :nb], rb[:, :nb], kt[0:16, 0, :nb], kt[32:48, 0, :nb], nb, w == 0, [bR], [bKt])
                        for h in range(1, 4):
                            P.op("pool", lambda e: e.tensor_copy(out=kt[0:16, h, :nb], in_=kt[0:16, 0, :nb]), reads=[bKt], writes=[bKt])
                            P.op("pool", lambda e: e.tensor_copy(out=kt[32:48, h, :nb], in_=kt[32:48, 0, :nb]), reads=[bKt], writes=[bKt])
                        P.dma("pool", kT[:, :, t0:t0 + nb].rearrange("h p t -> p h t"), kt[:, :, :nb], reads=[bKt], writes=[bKd])
                        for t in range(nt):
                            P.op("pe", lambda e: e.matmul(psb[7][:, 0:256], lhsT=ckvb[:, t * 128:(t + 1) * 128], rhs=wvb[:, :], start=True, stop=True), reads=[bCk, bS2], writes=[pB[7]])
                            P.op("act", lambda e: e.activation(out=vt[:, t, :, 0:64], in_=psb[7][:, 0:256].rearrange("p (h d) -> p h d", h=4), func=AF.Identity, scale=rcol[:, t:t + 1]),
                                 reads=[pB[7], bRc], writes=[bVt])
                        P.dma("pool", vv[t0:t0 + nb, :].rearrange("(t p) c -> p t c", p=128), vt[:, 0:nt, :, :].rearrange("p t h d -> p t (h d)"), reads=[bVt], writes=[bVd])
                        if not full:
                            continue
                        pending_tail = []
                        for t in range(nt):
                            sl = tix % 2
                            tix += 1
                            tl = tl0 + t * 128
                            tg = t0 + t * 128
                            hbt = hbufs(w, tl, 128)
                            for half in range(2):
                                bank = half
                                n = 0
                                for k in range(3):
                                    for dc in range(8):
                                        P.op("pe", lambda e: e.matmul(psb[bank][:, 0:384], lhsT=HT[:, dc, tl + k:tl + k + 128], rhs=wh[:, dc, k, half * 384:(half + 1) * 384],
                                                                      start=(n == 0), stop=(n == 23)), reads=hbt + [bW], writes=[pB[bank]], inc=(n == 23))
                                        n += 1
                                P.op("dve", lambda e: e.tensor_tensor(out=zt[sl][:, half * 384:(half + 1) * 384], in0=psb[bank][:, 0:384], in1=hsb[:, half * 384:(half + 1) * 384], op=ALU.add),
                                     reads=[pB[bank], bS2], writes=[bZt[sl]])
                            P.dma("pool", zs[tg:tg + 128, :], zt[sl][:], reads=[bZt[sl]], writes=[bZs])
                            for dc in range(8):
                                P.op("pe", lambda e: e.matmul(psb[2][:, :], lhsT=HT[:, dc, tl + 1:tl + 129], rhs=wb[:, dc, 0:512], start=(dc == 0), stop=(dc == 7)),
                                     reads=hbt + [bW], writes=[pB[2]], inc=(dc == 7))
                            while pending_tail:
                                pending_tail.pop(0)()
                            P.op("act", lambda e: e.activation(out=zg[sl][:, 0:256], in_=psb[2][:, 0:256], func=AF.Gelu), reads=[pB[2]], writes=[bZg[sl]])
                            P.op("act", lambda e: e.activation(out=zg[sl][:, 256:512], in_=psb[2][:, 256:512], func=AF.Gelu, accum_out=st1[sl][:, 0:1]), reads=[pB[2]], writes=[bZg[sl], bSt[sl]])
                            P.op("dve", lambda e: e.tensor_scalar(out=st1[sl][:, 0:1], in0=st1[sl][:, 0:1], scalar1=-1.0 / 256, scalar2=None, op0=ALU.mult), reads=[bSt[sl]], writes=[bSt[sl]])
                            P.op("dve", lambda e: e.tensor_scalar(out=vc[sl][:], in0=zg[sl][:, 256:512], scalar1=st1[sl][:, 0:1], scalar2=None, op0=ALU.add), reads=[bZg[sl], bSt[sl]], writes=[bVc[sl]])
                            P.op("act", lambda e: e.activation(out=junk2[:, 0:256], in_=vc[sl][:], func=AF.Square, accum_out=st1[sl][:, 1:2]), reads=[bVc[sl]], writes=[bJ2, bSt[sl]])
                            P.op("act", lambda e: e.activation(out=st1[sl][:, 1:2], in_=st1[sl][:, 1:2], func=AF.Sqrt, bias=epsT[:, 0:1], scale=1.0 / 256), reads=[bSt[sl], bC], writes=[bSt[sl]])
                            P.op("dve", lambda e: e.reciprocal(out=st1[sl][:, 1:2], in_=st1[sl][:, 1:2]), reads=[bSt[sl]], writes=[bSt[sl]])
                            P.op("dve", lambda e: e.scalar_tensor_tensor(out=vc[sl][:], in0=vc[sl][:], scalar=st1[sl][:, 1:2], in1=lng[:], op0=ALU.mult, op1=ALU.mult), reads=[bVc[sl], bSt[sl], bS2], writes=[bVc[sl]])
                            P.op("pool", lambda e: e.tensor_tensor(out=vnb[sl][:], in0=vc[sl][:], in1=lnb[:], op=ALU.add), reads=[bVc[sl], bS2], writes=[bVn[sl]])
                            def gm_tail(sl=sl, tg=tg):
                                for g in range(4):
                                    P.op("pe", lambda e: e.matmul(psb[3][:, g * 64:(g + 1) * 64], lhsT=wsT[:, g, :], rhs=vnb[sl][:, g * 64:(g + 1) * 64], start=(g == 0), stop=True),
                                         reads=[bVn[sl], bS2], writes=[pB[3]], inc=(g == 3))
                                P.op("dve", lambda e: e.tensor_tensor(out=ygm[sl][:], in0=psb[3][:, 0:256], in1=bsbc[:], op=ALU.add), reads=[pB[3], bS2], writes=[bYg[sl]])
                                P.op("pool", lambda e: e.tensor_tensor(out=ygm[sl][:], in0=ygm[sl][:], in1=zg[sl][:, 0:256], op=ALU.mult), reads=[bYg[sl], bZg[sl]], writes=[bYg[sl]])
                                P.dma("pool", ymix[tg:tg + 128, 256:512], ygm[sl][:], reads=[bYg[sl]], writes=[bYm])
                            pending_tail.append(gm_tail)
                        while pending_tail:
                            pending_tail.pop(0)()
                    P.barrier()
            if os.environ.get("MK_STOP") == "B":
                break
            with ExitStack() as st:
                F_ = lambda name, shape, dt=F32: sbt(st, name, shape, dt)
                kTs = F_("kTs", [128, 4, NTOK], BF16); bK = P.buf("kTs")
                vs = F_("vs", [128, 34, 260], BF16); bV = P.buf("vs")
                P.dma("sp", kTs[:, 0:2, :], kT[0:2].rearrange("h p t -> p h t"), reads=[bKd], writes=[bK])
                P.dma("sp", kTs[:, 2:4, :], kT[2:4].rearrange("h p t -> p h t"), reads=[bKd], writes=[bK])
                for c in range(0, 34, 8):
                    n = min(8, 34 - c)
                    P.dma("pool", vs[:, c:c + n, :], vv[c * 128:(c + n) * 128, :].rearrange("(t p) c -> p t c", p=128), reads=[bVd], writes=[bV])
                qs = [F_("qs%d" % i, [128, 4, 512], BF16) for i in range(2)]; bQs = P.bufs(2, "qs")
                pt = [F_("pt%d" % i, [128, 512], BF16) for i in range(3)]; bPt = P.bufs(3, "pt")
                oat = [F_("oat%d" % i, [128, 4, 256]) for i in range(2)]; bOa = P.bufs(2, "oat")
                rden = [F_("rden%d" % i, [128, 4]) for i in range(2)]; bRd = P.bufs(2, "rden")
                SC = 1.0 / float(np.sqrt(96.0))
                ablocks = [(bi, t0, nt, w) for bi, (t0, nt, w) in enumerate(BLOCKS) if not (last and w == 1)]
                iters = []
                for ai, (bi, t0, nt, w) in enumerate(ablocks):
                    kts = list(range(34)) if w == 0 else [32, 33]
                    for h in range(4):
                        for ki, kb in enumerate(kts):
                            iters.append((ai, t0, nt, w, h, ki, kb, len(kts)))

                def load_q(ai):
                    bi, t0, nt, w = ablocks[ai]
                    nb = nt * 128
                    P.dma("sp", qs[ai % 2][:, :, :nb], qT[:, :, t0:t0 + nb].rearrange("h p t -> p h t"), reads=[bQd], writes=[bQs[ai % 2]])

                def issue_qk(i):
                    ai, t0, nt, w, h, ki, kb, nk = iters[i]
                    nb = nt * 128
                    sl = ai % 2
                    sb_ = i % 3
                    if h == 0 and ki == 0 and ai + 1 < len(ablocks):
                        load_q(ai + 1)
                    P.op("pe", lambda e: e.matmul(psb[sb_][:, :nb], lhsT=kTs[:, h, kb * 128:(kb + 1) * 128], rhs=qs[sl][:, h, :nb], start=True, stop=True),
                         reads=[bK, bQs[sl]], writes=[pB[sb_]])
                    P.op("act", lambda e: e.activation(out=pt[sb_][:, :nb], in_=psb[sb_][:, :nb], func=AF.Exp, scale=SC), reads=[pB[sb_]], writes=[bPt[sb_]])

                def issue_pv(i):
                    ai, t0, nt, w, h, ki, kb, nk = iters[i]
                    nb = nt * 128
                    sl = ai % 2
                    sb_ = i % 3
                    ob = 4 + (h % 2)
                    for q_ in range(nt):
                        P.op("pe", lambda e: e.matmul(psb[ob][:, q_ * 65:(q_ + 1) * 65], lhsT=pt[sb_][:, q_ * 128:(q_ + 1) * 128], rhs=vs[:, kb, h * 65:(h + 1) * 65],
                                                      start=(ki == 0 and q_ == 0), stop=(ki == nk - 1)),
                             reads=[bPt[sb_], bV], writes=[pB[ob]], inc=(q_ == nt - 1))
                    if ki == nk - 1:
                        rs_ = h % 2
                        P.op("dve", lambda e: e.reciprocal(out=rden[rs_][:, 0:nt], in_=psb[ob][:, 0:nt * 65].rearrange("p (q c) -> p q c", c=65)[:, :, 64]),
                             reads=[pB[ob]], writes=[bRd[rs_]])
                        for q_ in range(nt):
                            P.op("dve", lambda e: e.tensor_scalar(out=oat[sl][:, q_, h * 64:(h + 1) * 64], in0=psb[ob][:, q_ * 65:q_ * 65 + 64], scalar1=rden[rs_][:, q_:q_ + 1], scalar2=None, op0=ALU.mult),
                                 reads=[pB[ob], bRd[rs_]], writes=[bOa[sl]])
                        if h == 3:
                            P.dma("pool", ymix[t0:t0 + nb, 768:1024].rearrange("(t p) c -> p t c", p=128), oat[sl][:, 0:nt, :], reads=[bOa[sl]], writes=[bYm])

                LA = 2
                load_q(0)
                for i in range(len(iters) + LA):
                    if i < len(iters):
                        issue_qk(i)
                    if i - LA >= 0:
                        issue_pv(i - LA)
                P.barrier()
            if os.environ.get("MK_STOP") == "AT":
                break
            with ExitStack() as st:
                F_ = lambda name, shape, dt=F32: sbt(st, name, shape, dt)
                gl = [F_("gll", [128, 2, L + 30], BF16), F_("glc", [128, 2, LC + 30], BF16)]; bG = P.bufs(2, "gl")
                for (w, n, c0) in ((0, L, 0), (1, LC, L)):
                    if last and w == 1:
                        continue
                    P.op("pool", lambda e: e.memset(gl[w][:, :, 0:15], 0.0), writes=[bG[w]])
                    P.op("pool", lambda e: e.memset(gl[w][:, :, n + 15:n + 30], 0.0), writes=[bG[w]])
                    for j in range(2):
                        P.dma("sp", gl[w][:, j, 15:15 + n], glu[j * 128:(j + 1) * 128, c0:c0 + n], reads=[bGlu], writes=[bG[w]])
                cvw = F_("cvw", [128, 2, 31]); cvb = F_("cvb", [128, 2]); clg = F_("clg", [128, 2]); clb = F_("clb", [128, 2]); bCv = P.buf("cvconst")
                P.dma("sp", cvw[:], I["cv_w"][l], writes=[bCv])
                P.dma("sp", cvb[:], I["cv_bT"][l], writes=[bCv])
                P.dma("sp", clg[:], I["cv_lgT"][l], writes=[bCv])
                P.dma("sp", clb[:], I["cv_lbT"][l], writes=[bCv])
                dg = F_("dg", [128, 2, 31, 128], BF16); bDg = P.buf("dg")
                for j in range(2):
                    for k in range(31):
                        P.op("dve" if k % 2 == 0 else "pool", lambda e: e.tensor_scalar(out=dg[:, j, k, :], in0=ident[:], scalar1=cvw[:, j, k:k + 1], scalar2=None, op0=ALU.mult),
                             reads=[bCv, bC], writes=[bDg])
                gsb = [F_("gsb%d" % i, [128, 512]) for i in range(2)]; bGs = P.bufs(2, "gsb")
                cen = [F_("cen%d" % i, [128, 512]) for i in range(2)]; bCe = P.bufs(2, "cen")
                sqs = [F_("sqs%d" % i, [128, 512]) for i in range(2)]; bSq = P.bufs(2, "sqs")
                ycT = [F_("ycT%d" % i, [128, 512]) for i in range(2)]; bYc = P.bufs(2, "ycT")
                ysb = [F_("ysb%d" % i, [128, 256]) for i in range(2)]; bYs = P.bufs(2, "ysb")
                tix = 0
                for bi, (t0, nt, w) in enumerate(BLOCKS):
                    if last and w == 1:
                        continue
                    nb = nt * 128
                    tl0 = t0 if w == 0 else 0
                    for j in range(2):
                        for k in range(31):
                            P.op("pe", lambda e: e.matmul(psb[j][:, :nb], lhsT=dg[:, j, k, :], rhs=gl[w][:, j, tl0 + k:tl0 + k + nb], start=(k == 0), stop=(k == 30)),
                                 reads=[bDg, bG[w]], writes=[pB[j]], inc=(k == 30))
                        P.op("act", lambda e: e.activation(out=gsb[j][:, :nb], in_=psb[j][:, :nb], func=AF.Identity, bias=cvb[:, j:j + 1]), reads=[pB[j], bCv], writes=[bGs[j]])
                        P.op("pe", lambda e: e.matmul(psb[2 + j][:, :nb], lhsT=blk64[:], rhs=gsb[j][:, :nb], start=True, stop=True), reads=[bGs[j], bC], writes=[pB[2 + j]])
                        P.op("dve", lambda e: e.tensor_tensor(out=cen[j][:, :nb], in0=gsb[j][:, :nb], in1=psb[2 + j][:, :nb], op=ALU.subtract), reads=[bGs[j], pB[2 + j]], writes=[bCe[j]])
                        P.op("act", lambda e: e.activation(out=sqs[j][:, :nb], in_=cen[j][:, :nb], func=AF.Square), reads=[bCe[j]], writes=[bSq[j]])
                        P.op("pe", lambda e: e.matmul(psb[2 + j][:, :nb], lhsT=blk64[:], rhs=sqs[j][:, :nb], start=True, stop=True), reads=[bSq[j], bC], writes=[pB[2 + j]])
                        P.op("act", lambda e: e.activation(out=sqs[j][:, :nb], in_=psb[2 + j][:, :nb], func=AF.Sqrt, bias=epsT[:, 0:1]), reads=[pB[2 + j], bC], writes=[bSq[j]])
                        P.op("dve", lambda e: e.reciprocal(out=sqs[j][:, :nb], in_=sqs[j][:, :nb]), reads=[bSq[j]], writes=[bSq[j]])
                        P.op("dve", lambda e: e.tensor_tensor(out=cen[j][:, :nb], in0=cen[j][:, :nb], in1=sqs[j][:, :nb], op=ALU.mult), reads=[bCe[j], bSq[j]], writes=[bCe[j]])
                        P.op("act", lambda e: e.activation(out=ycT[j][:, :nb], in_=cen[j][:, :nb], func=AF.Silu, bias=clb[:, j:j + 1], scale=clg[:, j:j + 1]), reads=[bCe[j], bCv], writes=[bYc[j]])
                    for t in range(nt):
                        sl = tix % 2
                        tix += 1
                        bank = 4 + sl
                        for j in range(2):
                            P.op("pe", lambda e: e.transpose(out=psb[bank][:, j * 128:(j + 1) * 128], in_=ycT[j][:, t * 128:(t + 1) * 128], identity=ident[:]),
                                 reads=[bYc[j], bC], writes=[pB[bank]], inc=(j == 1))
                        P.op("act", lambda e: e.activation(out=ysb[sl][:], in_=psb[bank][:, 0:256], func=AF.Identity), reads=[pB[bank]], writes=[bYs[sl]])
                        tg = t0 + t * 128
                        P.dma("pool", ymix[tg:tg + 128, 512:768], ysb[sl][:], reads=[bYs[sl]], writes=[bYm])
                P.barrier()
            if os.environ.get("MK_STOP") == "CV":
                break
            def hyena(w, Lh, tag, c0, ksp):
                NT = Lh // 128
                NR = 2 * NT
                NS = max(NT // 2, 1)
                TB = min(512, Lh)
                with ExitStack() as st:
                    F_ = lambda name, shape, dt=F32: sbt(st, name, shape, dt)
                    slab = [F_("slab%d" % i, [128, 32 * 512 if Lh == L else NT * 512], BF16) for i in range(2)]; bSl = P.bufs(2, "slab"); bSlB = P.bufs(2, "slabB")

                    def load_slab(sl, view3, src3, n0):
                        h = max(n0 // 2, 1)
                        P.dma("sp", view3[:, 0:h, :], src3[:, 0:h, :], writes=[bSl[sl]])
                        if h < n0:
                            P.dma("act", view3[:, h:n0, :], src3[:, h:n0, :], writes=[bSlB[sl]])
                    invn = F_("invn", [128, 512]); bIn = P.buf("invn")
                    rsc = F_("rsc", [128, NR]); bRs = P.buf("rsc")
                    P.dma("sp", rsc[:], I["rsc" + tag], writes=[bRs])
                    bKs = P.buf("kspec_dram")
                    with ExitStack() as s1:
                        G_ = lambda name, shape, dt=F32: sbt(s1, name, shape, dt)
                        w1 = G_("w1", [33, 64]); w2 = G_("w2", [64, 64]); w3 = G_("w3", [64, 1024]); cb = G_("cb", [64, 6]); bFw = P.buf("fw")
                        P.dma("sp", w1[:], I["hy_f_w1"][l], writes=[bFw])
                        P.dma("sp", w2[:], I["hy_f_w2"][l], writes=[bFw])
                        P.dma("sp", w3[:], I["hy_f_w3"][l], writes=[bFw])
                        P.dma("sp", cb[:, 0:1], I["hy_b1c"][l], writes=[bFw])
                        P.dma("sp", cb[:, 1:2], I["hy_b2c"][l], writes=[bFw])
                        P.dma("sp", cb[:, 2:3], I["hy_frc"][l], writes=[bFw])
                        P.op("dve", lambda e: e.tensor_tensor(out=cb[:, 3:4], in0=cb[:, 0:1], in1=cb[:, 2:3], op=ALU.mult), reads=[bFw], writes=[bFw])
                        P.op("dve", lambda e: e.tensor_tensor(out=cb[:, 4:5], in0=cb[:, 1:2], in1=cb[:, 2:3], op=ALU.mult), reads=[bFw], writes=[bFw])
                        zT = G_("zT", [33, TB]); bZ = P.buf("zT")
                        arg = G_("arg", [64, TB]); msk_ = G_("mskf", [64, TB]); h1 = G_("h1", [64, TB]); bAr = P.buf("arg"); bH1 = P.buf("h1")
                        h2T = G_("h2T", [64, Lh]); bH2 = P.buf("h2T")
                        rre = G_("rre", [128, NT, 512], BF16); rim = G_("rim", [128, NT, 512], BF16); bRe = P.buf("rre")
                        dect = [G_("dect%d" % i, [128, 256]) for i in range(2)]; bDe = P.bufs(2, "dect")
                        kk = G_("kk", [128, 1024]); ak = G_("ak", [128, 1024]); bKk = P.buf("kk"); bAk = P.buf("ak")
                        spo = [G_("spo%d" % i, [128, 512]) for i in range(2)]; bSp = P.bufs(2, "spo")

                        def sin_layer(ps_ap, bcol, dst, nbk, rd, wr):
                            P.op("act", lambda e: e.activation(out=arg[:, :nbk], in_=ps_ap, func=AF.Identity, bias=cb[:, bcol:bcol + 1], scale=cb[:, 2:3]), reads=rd + [bFw], writes=[bAr])
                            for _ in range(2):
                                P.op("dve", lambda e: e.tensor_single_scalar(out=msk_[:, :nbk], in_=arg[:, :nbk], scalar=PI, op=ALU.is_gt), reads=[bAr], writes=[bAr])
                                P.op("dve", lambda e: e.scalar_tensor_tensor(out=arg[:, :nbk], in0=msk_[:, :nbk], scalar=-2.0 * PI, in1=arg[:, :nbk], op0=ALU.mult, op1=ALU.add), reads=[bAr], writes=[bAr])
                                P.op("dve", lambda e: e.tensor_single_scalar(out=msk_[:, :nbk], in_=arg[:, :nbk], scalar=-PI, op=ALU.is_lt), reads=[bAr], writes=[bAr])
                                P.op("dve", lambda e: e.scalar_tensor_tensor(out=arg[:, :nbk], in0=msk_[:, :nbk], scalar=2.0 * PI, in1=arg[:, :nbk], op0=ALU.mult, op1=ALU.add), reads=[bAr], writes=[bAr])
                            P.op("act", lambda e: e.activation(out=dst, in_=arg[:, :nbk], func=AF.Sin), reads=[bAr], writes=wr)

                        for b0 in range(0, Lh, TB):
                            P.dma("sp", zT[:, :], I["hyz" + tag][:, b0:b0 + TB], writes=[bZ])
                            P.op("pe", lambda e: e.matmul(psb[0][0:64, :TB], lhsT=w1[:, :], rhs=zT[:, :], start=True, stop=True), reads=[bZ, bFw], writes=[pB[0]])
                            sin_layer(psb[0][0:64, :TB], 3, h1[:, :TB], TB, [pB[0]], [bH1])
                            P.op("pe", lambda e: e.matmul(psb[1][0:64, :TB], lhsT=w2[:, :], rhs=h1[:, :TB], start=True, stop=True), reads=[bH1, bFw], writes=[pB[1]])
                            sin_layer(psb[1][0:64, :TB], 4, h2T[:, b0:b0 + TB], TB, [pB[1]], [bH2])
                        for ti in range(NT):
                            sl = ti % 2
                            P.dma("sp", dect[sl][:], I["hydec" + tag][:, ti, :], writes=[bDe[sl]])
                            for o in range(2):
                                bank = 2 + o
                                P.op("pe", lambda e: e.matmul(psb[bank][:, :], lhsT=h2T[:, ti * 128:(ti + 1) * 128], rhs=w3[:, o * 512:(o + 1) * 512], start=True, stop=True), reads=[bH2, bFw], writes=[pB[bank]])
                                P.op("dve", lambda e: e.tensor_tensor(out=kk[:, o * 512:(o + 1) * 512].rearrange("p (a c) -> p a c", c=256), in0=psb[bank][:, :].rearrange("p (a c) -> p a c", c=256),
                                                                       in1=dect[sl][:].unsqueeze(1).to_broadcast([128, 2, 256]), op=ALU.mult), reads=[pB[bank], bDe[sl]], writes=[bKk])
                            if ti == 0:
                                for o in range(2):
                                    P.op("dve", lambda e: e.memset(kk[0:1, o * 512 + 256:o * 512 + 512], 0.0), reads=[bKk], writes=[bKk])
                            P.op("dve", lambda e: e.scalar_tensor_tensor(out=ak[:], in0=kk[:], scalar=-1.0, in1=kk[:], op0=ALU.mult, op1=ALU.max), reads=[bKk], writes=[bAk])
                            for o in range(2):
                                P.op("pe", lambda e: e.matmul(psb[6 + o][:, :], lhsT=onesf[:], rhs=ak[:, o * 512:(o + 1) * 512], start=(ti == 0), stop=(ti == NT - 1)), reads=[bAk, bC], writes=[pB[6 + o]])
                            kv_ = kk[:].rearrange("p (o d c) -> p o d c", o=2, d=2)
                            P.op("pool", lambda e: e.tensor_tensor(out=rre[:, ti, :].rearrange("p (o c) -> p o c", o=2), in0=kv_[:, :, 0, :], in1=kv_[:, :, 1, :], op=ALU.add), reads=[bKk], writes=[bRe])
                            P.op("pool", lambda e: e.tensor_tensor(out=rim[:, ti, :].rearrange("p (o c) -> p o c", o=2), in0=kv_[:, :, 0, :], in1=kv_[:, :, 1, :], op=ALU.subtract), reads=[bKk], writes=[bRe])
                        for o in range(2):
                            P.op("act", lambda e: e.activation(out=invn[:, o * 256:(o + 1) * 256], in_=psb[6 + o][:, 0:256], func=AF.Identity), reads=[pB[6 + o]], writes=[bIn])
                            P.op("dve", lambda e: e.tensor_tensor(out=invn[:, o * 256:(o + 1) * 256], in0=psb[6 + o][:, 256:512], in1=invn[:, o * 256:(o + 1) * 256], op=ALU.add), reads=[pB[6 + o], bIn], writes=[bIn])
                        P.op("dve", lambda e: e.reciprocal(out=invn[:], in_=invn[:]), reads=[bIn], writes=[bIn])
                        k = 0
                        for s_ in range(NS):
                            sl = s_ % 2
                            sv = slab[sl][:, 0:NT * 512].rearrange("p (t c) -> p t c", c=512)
                            load_slab(sl, sv, I["dftf" + tag][s_], NT)
                            rcs_ = [2 * s_, 2 * s_ + 1, NT + 2 * s_, NT + 2 * s_ + 1] if NT >= 2 else None
                            for j, rc in enumerate(rcs_):
                                src = rim if j >= 2 else rre
                                bank = k % 2
                                for tc in range(NT):
                                    P.op("pe", lambda e: e.matmul(psb[bank][:, :], lhsT=sv[:, tc, j * 128:(j + 1) * 128], rhs=src[:, tc, :], start=(tc == 0), stop=(tc == NT - 1)),
                                         reads=[bSl[sl], bSlB[sl], bRe], writes=[pB[bank]], inc=(tc == NT - 1))
                                osl = k % 2
                                P.op("dve", lambda e: e.scalar_tensor_tensor(out=spo[osl][:], in0=psb[bank][:, :], scalar=rsc[:, rc:rc + 1], in1=invn[:], op0=ALU.mult, op1=ALU.mult),
                                     reads=[pB[bank], bRs, bIn], writes=[bSp[osl]])
                                if rc == NT:
                                    for tc in range(NT):
                                        P.op("pe", lambda e: e.matmul(psb[2][0:1, :], lhsT=sv[:, tc, j * 128:j * 128 + 1], rhs=rre[:, tc, :], start=(tc == 0), stop=(tc == NT - 1)),
                                             reads=[bSl[sl], bSlB[sl], bRe], writes=[pB[2]], inc=(tc == NT - 1))
                                    P.op("dve", lambda e: e.scalar_tensor_tensor(out=spo[osl][0:1, :], in0=psb[2][0:1, :], scalar=rsc[0:1, rc:rc + 1], in1=invn[0:1, :], op0=ALU.mult, op1=ALU.mult),
                                         reads=[pB[2], bRs, bIn, bSp[osl]], writes=[bSp[osl]])
                                P.dma("pool", ksp[rc * 128:(rc + 1) * 128, :], spo[osl][:], reads=[bSp[osl]], writes=[bKs])
                                k += 1
                        P.barrier()
                    with ExitStack() as s2:
                        G_ = lambda name, shape, dt=F32: sbt(s2, name, shape, dt)
                        ub = G_("ub", [128, NT, 256], BF16); bUb = P.buf("ub")
                        Yf = G_("Yf", [128, NR, 256], BF16); bYf = P.buf("Yf")
                        y1f = G_("y1f", [128, NT, 256]); bY1 = P.buf("y1f")
                        hyb = G_("hyb", [128, 512]); bHb = P.buf("hyb")
                        P.dma("sp", hyb[:], I["hyb_bc"][l], writes=[bHb])
                        vst = [G_("vst%d" % i, [128, 8, 256]) for i in range(2)]; bVs = P.bufs(2, "vst")
                        for c in range(0, NT, 8):
                            n = min(8, NT - c)
                            sl = (c // 8) % 2
                            P.dma("sp", vst[sl][:, 0:n, :], zs[c0 + c * 128:c0 + (c + n) * 128, 512:768].rearrange("(t p) c -> p t c", p=128), reads=[bZs], writes=[bVs[sl]])
                            P.op("pool", lambda e: e.tensor_copy(out=ub[:, c:c + n, :], in_=vst[sl][:, 0:n, :]), reads=[bVs[sl]], writes=[bUb])
                        kt_ = [G_("ktab%d" % i, [128, 2, 256]) for i in range(2)]; bKt_ = P.bufs(2, "ktab")
                        tm = [G_("tm%d" % i, [128, 256]) for i in range(4)]; bTm = P.bufs(4, "tm")
                        zt_ = [G_("hzt%d" % i, [128, 768]) for i in range(2)]; bZt_ = P.bufs(2, "hzt")
                        yo = [G_("yo%d" % i, [128, 256]) for i in range(2)]; bYo = P.bufs(2, "yo")
                        kq = 0
                        for o in range(2):
                            for s_ in range(NS):
                                sl = kq % 2
                                kq += 1
                                sv = slab[sl][:, 0:NT * 512].rearrange("p (t c) -> p t c", c=512)
                                load_slab(sl, sv, I["dftf" + tag][s_], NT)
                                for j in range(4):
                                    bank = (s_ % 2) * 2 + (j % 2)
                                    cs = (j // 2) * 256
                                    for tc in range(NT):
                                        P.op("pe", lambda e: e.matmul(psb[bank][:, cs:cs + 256], lhsT=sv[:, tc, j * 128:(j + 1) * 128], rhs=ub[:, tc, :], start=(tc == 0), stop=(tc == NT - 1)),
                                             reads=[bSl[sl], bSlB[sl], bUb], writes=[pB[bank]], inc=(tc == NT - 1))
                                for jj in range(2):
                                    fc = 2 * s_ + jj
                                    bank = (s_ % 2) * 2 + jj
                                    ks_ = fc % 2
                                    P.dma("sp", kt_[ks_][:, 0, :], ksp[fc * 128:(fc + 1) * 128, o * 256:(o + 1) * 256], reads=[bKs], writes=[bKt_[ks_]])
                                    P.dma("sp", kt_[ks_][:, 1, :], ksp[(NT + fc) * 128:(NT + fc + 1) * 128, o * 256:(o + 1) * 256], reads=[bKs], writes=[bKt_[ks_]])
                                    Ure = psb[bank][:, 0:256]
                                    Uim = psb[bank][:, 256:512]
                                    Kre = kt_[ks_][:, 0, :]
                                    Kim = kt_[ks_][:, 1, :]
                                    P.op("dve", lambda e: e.tensor_tensor(out=tm[0][:], in0=Ure, in1=Kre, op=ALU.mult), reads=[pB[bank], bKt_[ks_]], writes=[bTm[0]])
                                    P.op("dve", lambda e: e.tensor_tensor(out=tm[1][:], in0=Uim, in1=Kim, op=ALU.mult), reads=[pB[bank], bKt_[ks_]], writes=[bTm[1]])
                                    P.op("pool", lambda e: e.tensor_tensor(out=Yf[:, fc, :], in0=tm[0][:], in1=tm[1][:], op=ALU.subtract), reads=[bTm[0], bTm[1]], writes=[bYf])
                                    P.op("dve", lambda e: e.tensor_tensor(out=tm[2][:], in0=Ure, in1=Kim, op=ALU.mult), reads=[pB[bank], bKt_[ks_]], writes=[bTm[2]])
                                    P.op("dve", lambda e: e.tensor_tensor(out=tm[3][:], in0=Uim, in1=Kre, op=ALU.mult), reads=[pB[bank], bKt_[ks_]], writes=[bTm[3]])
                                    P.op("pool", lambda e: e.tensor_tensor(out=Yf[:, NT + fc, :], in0=tm[2][:], in1=tm[3][:], op=ALU.add), reads=[bTm[2], bTm[3]], writes=[bYf])
                                    if fc == 0:
                                        P.op("dve", lambda e: e.tensor_tensor(out=Yf[0:1, 0, :], in0=Ure[0:1, :], in1=Kre[0:1, :], op=ALU.mult), reads=[pB[bank], bKt_[ks_], bYf], writes=[bYf])
                                        P.op("dve", lambda e: e.tensor_tensor(out=Yf[0:1, NT, :], in0=Uim[0:1, :], in1=Kim[0:1, :], op=ALU.mult), reads=[pB[bank], bKt_[ks_], bYf], writes=[bYf])
                            for s_ in range(NS):
                                sl = kq % 2
                                kq += 1
                                iv = slab[sl][:, 0:NR * 256].rearrange("p (r c) -> p r c", c=256)
                                load_slab(sl, iv, I["dfti" + tag][s_], NR)
                                for tt in range(2 if NT >= 2 else 1):
                                    ti = 2 * s_ + tt
                                    bank = 4 + (ti % 2)
                                    for rc in range(NR):
                                        P.op("pe", lambda e: e.matmul(psb[bank][:, 0:256], lhsT=iv[:, rc, tt * 128:(tt + 1) * 128], rhs=Yf[:, rc, :], start=(rc == 0), stop=(rc == NR - 1)),
                                             reads=[bSl[sl], bSlB[sl], bYf], writes=[pB[bank]], inc=(rc == NR - 1))
                                    zsl = ti % 2
                                    tg = c0 + ti * 128
                                    P.dma("sp", zt_[zsl][:], zs[tg:tg + 128, :], reads=[bZs], writes=[bZt_[zsl]])
                                    u32 = zt_[zsl][:, 512:768] if o == 0 else y1f[:, ti, :]
                                    gate = zt_[zsl][:, o * 256:(o + 1) * 256]
                                    P.op("dve", lambda e: e.tensor_tensor(out=yo[zsl][:], in0=u32, in1=hyb[:, o * 256:(o + 1) * 256], op=ALU.mult), reads=[bZt_[zsl], bY1, bHb], writes=[bYo[zsl]])
                                    P.op("dve", lambda e: e.tensor_tensor(out=yo[zsl][:], in0=psb[bank][:, 0:256], in1=yo[zsl][:], op=ALU.add), reads=[pB[bank], bYo[zsl]], writes=[bYo[zsl]])
                                    if o == 0:
                                        P.op("pool", lambda e: e.tensor_tensor(out=y1f[:, ti, :], in0=yo[zsl][:], in1=gate, op=ALU.mult), reads=[bYo[zsl], bZt_[zsl]], writes=[bY1])
                                        P.op("pool", lambda e: e.tensor_copy(out=ub[:, ti, :], in_=y1f[:, ti, :]), reads=[bY1], writes=[bUb])
                                    else:
                                        P.op("pool", lambda e: e.tensor_tensor(out=yo[zsl][:], in0=yo[zsl][:], in1=gate, op=ALU.mult), reads=[bYo[zsl], bZt_[zsl]], writes=[bYo[zsl]])
                                        P.dma("pool", ymix[tg:tg + 128, 0:256], yo[zsl][:], reads=[bYo[zsl]], writes=[bYm])
                        P.barrier()
                P.barrier()

            hyena(0, L, "L", 0, kspec)
            if not last:
                hyena(1, LC, "C", L, kspecC)
            if os.environ.get("MK_STOP") == "HY":
                break

            with ExitStack() as st:
                F_ = lambda name, shape, dt=F32: sbt(st, name, shape, dt)
                wo = F_("wo", [128, 8, 1024], BF16); bWo = P.buf("wo")
                wst = [F_("wost%d" % i, [128, 1024]) for i in range(2)]; bWs = P.bufs(2, "wost")
                mixg = F_("mixg", [128, 1024]); bMg = P.buf("mixg")
                P.dma("sp", mixg[:], I["mixg_bc"][l], writes=[bMg])
                for kc in range(8):
                    sl = kc % 2
                    P.dma("sp", wst[sl][:], I["w_out"][l, kc * 128:(kc + 1) * 128, :], writes=[bWs[sl]])
                    P.op("pool", lambda e: e.tensor_copy(out=wo[:, kc, :], in_=wst[sl][:]), reads=[bWs[sl]], writes=[bWo])
                yt = [F_("yt%d" % i, [128, 1024]) for i in range(3)]; bYt = P.bufs(3, "yt")
                xt = [F_("oxt%d" % i, [128, 1024]) for i in range(3)]; bXt = P.bufs(3, "oxt")
                yn = [F_("yn%d" % i, [128, 1024], BF16) for i in range(3)]; bYn = P.bufs(3, "yn")
                ynT = [F_("ynT%d" % i, [128, 8, 128], BF16) for i in range(3)]; bYT = P.bufs(3, "ynT")
                xo = [F_("xo%d" % i, [128, 1024]) for i in range(3)]; bXo = P.bufs(3, "xo")
                s4 = [F_("s4%d" % i, [128, 4]) for i in range(3)]; bS4 = P.bufs(3, "s4")
                bXm = P.buf("xm_dram")
                ntile = 32 if last else 34
                for ti in range(ntile):
                    sl = ti % 3
                    w = 0 if ti < 32 else 1
                    tg = ti * 128
                    P.dma("sp", yt[sl][:], ymix[tg:tg + 128, :], reads=[bYm], writes=[bYt[sl]])
                    P.dma("sp", xt[sl][:], xin_ap(l, tg, 128), writes=[bXt[sl]])
                    for g in range(4):
                        P.op("act", lambda e: e.activation(out=junk2[:, 0:256], in_=yt[sl][:, g * 256:(g + 1) * 256], func=AF.Square, accum_out=s4[sl][:, g:g + 1]),
                             reads=[bYt[sl]], writes=[bJ2, bS4[sl]])
                    P.op("act", lambda e: e.activation(out=s4[sl][:], in_=s4[sl][:], func=AF.Sqrt, bias=epsT[:, 0:1], scale=1.0 / 256), reads=[bS4[sl], bC], writes=[bS4[sl]])
                    P.op("dve", lambda e: e.reciprocal(out=s4[sl][:], in_=s4[sl][:]), reads=[bS4[sl]], writes=[bS4[sl]])
                    for g in range(4):
                        P.op("dve", lambda e: e.scalar_tensor_tensor(out=yn[sl][:, g * 256:(g + 1) * 256], in0=yt[sl][:, g * 256:(g + 1) * 256], scalar=s4[sl][:, g:g + 1],
                                                                     in1=mixg[:, g * 256:(g + 1) * 256], op0=ALU.mult, op1=ALU.mult),
                             reads=[bYt[sl], bS4[sl], bMg], writes=[bYn[sl]])
                    for half in range(2):
                        bank = 6 + half
                        pT = psb[bank][:].bitcast(BF16)
                        for j in range(4):
                            kc = half * 4 + j
                            P.op("pe", lambda e: e.transpose(out=pT[:, j * 128:(j + 1) * 128], in_=yn[sl][:, kc * 128:(kc + 1) * 128], identity=identb[:]),
                                 reads=[bYn[sl], bC], writes=[pB[bank]], inc=(j == 3))
                        P.op("act", lambda e: e.activation(out=ynT[sl][:, half * 4:(half + 1) * 4, :], in_=pT[:, 0:512].rearrange("p (a b) -> p a b", a=4), func=AF.Identity),
                             reads=[pB[bank]], writes=[bYT[sl]])
                    for half in range(2):
                        for kc in range(8):
                            P.op("pe", lambda e: e.matmul(psb[half][:, :], lhsT=ynT[sl][:, kc, :], rhs=wo[:, kc, half * 512:(half + 1) * 512], start=(kc == 0), stop=(kc == 7)),
                                 reads=[bYT[sl], bWo], writes=[pB[half]], inc=(kc == 7))
                        P.op("dve", lambda e: e.tensor_tensor(out=xo[sl][:, half * 512:(half + 1) * 512], in0=psb[half][:, :], in1=gbc[:, w, half * 512:(half + 1) * 512], op=ALU.mult),
                             reads=[pB[half], bGbc], writes=[bXo[sl]])
                        P.op("pool", lambda e: e.tensor_tensor(out=xo[sl][:, half * 512:(half + 1) * 512], in0=xo[sl][:, half * 512:(half + 1) * 512], in1=xt[sl][:, half * 512:(half + 1) * 512], op=ALU.add),
                             reads=[bXo[sl], bXt[sl]], writes=[bXo[sl]])
                    P.dma("pool", xm[tg:tg + 128, :], xo[sl][:], reads=[bXo[sl]], writes=[bXm])
                P.barrier()
            if os.environ.get("MK_STOP") == "O":
                break
            with ExitStack() as st:
                F_ = lambda name, shape, dt=F32: sbt(st, name, shape, dt)
                NTL = 31 if last else 32
                NSL = 32 * 512
                BIG = 32768.0
                ntile = 32 if last else 34
                sel_all = F_("sel_all", [128, 34, 16]); g_all = F_("g_all", [128, 34, 16]); bSel = P.buf("sel_all"); bGa = P.buf("g_all")
                slotA_i = F_("slotA_i", [128, 34], I32); slotB_i = F_("slotB_i", [128, 34], I32); gA = F_("gA", [128, 34]); gB = F_("gB", [128, 34])
                widx_i = F_("widx_i", [128, 32], I32); bSlots = P.buf("slots")
                tokidx = F_("tokidx", [128, 34], I32); bTok = P.buf("tokidx")
                P.dma("sp", tokidx[:], I["tokidx"], writes=[bTok])
                P.op("pool", lambda e: e.memset(sel_all[:], 0.0), writes=[bSel])
                P.op("pool", lambda e: e.memset(g_all[:], 0.0), writes=[bGa])
                bTbl = P.buf("tbl_dram"); bHfD = P.buf("hfD_dram"); bYp = P.buf("ypair_dram")
                P.dma("sp", tbl, I["tblfill"], writes=[bTbl])
                with ExitStack() as s1:
                    G_ = lambda name, shape, dt=F32: sbt(s1, name, shape, dt)
                    wr = G_("wr", [128, 8, 16]); rbb = G_("rbb", [128, 16]); bMc = P.buf("mconst")
                    P.dma("sp", wr[:], I["w_router"].rearrange("(c p) e -> p c e", p=128), writes=[bMc])
                    P.dma("sp", rbb[:], I["rb_bc"], writes=[bMc])
                    bc2 = G_("bc2", [128, 4, 1024]); bBc2 = P.buf("bc2")
                    diag = [G_("mdiag%d" % i, [128, 128]) for i in range(2)]; bDiag = P.bufs(2, "mdiag")
                    k = 0
                    for gi, (srct, off, w) in enumerate(((modT, 24, 0), (modT, 24, 1), (gain2, 0, 0), (gain2, 0, 1))):
                        for half in range(2):
                            bank = 1 + (k % 2)
                            for j in range(4):
                                dc = half * 4 + j
                                dsl = (k * 4 + j) % 2
                                P.op("dve", lambda e: e.tensor_scalar(out=diag[dsl][:], in0=ident[:], scalar1=srct[:, off + dc, w:w + 1], scalar2=None, op0=ALU.mult),
                                     reads=[bMod, bC], writes=[bDiag[dsl]])
                                P.op("pe", lambda e: e.matmul(psb[bank][:, j * 128:(j + 1) * 128], lhsT=onesf[:], rhs=diag[dsl][:], start=(j == 0), stop=True),
                                     reads=[bDiag[dsl], bC], writes=[pB[bank]], inc=True)
                            P.op("act", lambda e: e.activation(out=bc2[:, gi, half * 512:(half + 1) * 512], in_=psb[bank][:], func=AF.Identity),
                                 reads=[pB[bank]], writes=[bBc2])
                            k += 1
                    mx = [G_("mx%d" % i, [128, 1024]) for i in range(4)]; bMx = P.bufs(4, "mx")
                    xn4 = G_("xn4", [128, 4, 1024]); bXn = P.bufs(4, "xn4")
                    hf32 = [G_("hf32_%d" % i, [128, 512]) for i in range(2)]; bH32 = P.bufs(2, "hf32")
                    htm = [G_("htm%d" % i, [128, 1024]) for i in range(4)]; bHtm = P.bufs(4, "htm")
                    hfb = [G_("hfb%d" % i, [128, 1024], BF16) for i in range(4)]; bHfb = P.bufs(4, "hfb")
                    ms = [G_("ms%d" % i, [128, 1]) for i in range(4)]; bMs = P.bufs(4, "ms")
                    sg = G_("sg", [128, 4, 16]); sl_ = G_("selv", [128, 4, 16]); p6 = G_("p6", [128, 16, 6]); gs = G_("gs", [128, 16]); gmx = G_("gmx", [128, 4])
                    isb = G_("isb", [128, 16]); thr = G_("thr", [128, 16]); msk = G_("msk", [128, 4, 16]); den = G_("den", [128, 4]); bRt = P.buf("router")
                    mti = 0
                    for bidx, (t0, nt, w) in enumerate(BLOCKS):
                        if last and w == 1:
                            continue
                        nb = nt * 128
                        T0 = t0 // 128
                        for t in range(nt):
                            sl = mti % 4
                            mti += 1
                            P.dma("sp", mx[sl][:], xm[t0 + t * 128:t0 + (t + 1) * 128, :], reads=[bXm], writes=[bMx[sl]])
                            P.op("act", lambda e: e.activation(out=junk2[:], in_=mx[sl][:], func=AF.Square, accum_out=ms[sl][:]), reads=[bMx[sl]], writes=[bJ2, bMs[sl]])
                            P.op("act", lambda e: e.activation(out=ms[sl][:], in_=ms[sl][:], func=AF.Sqrt, bias=epsT[:, 0:1], scale=1.0 / D), reads=[bMs[sl], bC], writes=[bMs[sl]])
                            P.op("dve", lambda e: e.reciprocal(out=ms[sl][:], in_=ms[sl][:]), reads=[bMs[sl]], writes=[bMs[sl]])
                            P.op("dve", lambda e: e.tensor_scalar(out=xn4[:, t, :], in0=mx[sl][:], scalar1=ms[sl][:, 0:1], scalar2=None, op0=ALU.mult), reads=[bMx[sl], bMs[sl]], writes=[bXn[t]])
                            P.op("dve", lambda e: e.tensor_tensor(out=htm[sl][:], in0=xn4[:, t, :], in1=bc2[:, 2 + w, :], op=ALU.mult), reads=[bXn[t], bBc2], writes=[bHtm[sl]])
                            P.op("pool", lambda e: e.tensor_tensor(out=hfb[sl][:], in0=htm[sl][:], in1=bc2[:, w, :], op=ALU.add), reads=[bHtm[sl], bBc2], writes=[bHfb[sl]])
                            P.dma("pool", hfD[t0 + t * 128:t0 + (t + 1) * 128, :], hfb[sl][:], reads=[bHfb[sl]], writes=[bHfD])
                        def m1_T(dc):
                            bank = 6 + dc % 2
                            for t in range(nt):
                                P.op("pe", lambda e: e.transpose(out=psb[bank][:, t * 128:(t + 1) * 128], in_=xn4[:, t, dc * 128:(dc + 1) * 128], identity=ident[:]),
                                     reads=[bXn[t], bC], writes=[pB[bank]], inc=(t == nt - 1))
                            P.op("act", lambda e: e.activation(out=hf32[dc % 2][:, :nb], in_=psb[bank][:, :nb], func=AF.Identity, bias=modT[:, 24 + dc, w:w + 1], scale=gain2[:, dc, w:w + 1]),
                                 reads=[pB[bank], bMod], writes=[bH32[dc % 2]])

                        def m1_R(dc):
                            for t in range(nt):
                                P.op("pe", lambda e: e.matmul(psb[5][:, t * 16:(t + 1) * 16], lhsT=hf32[dc % 2][:, t * 128:(t + 1) * 128], rhs=wr[:, dc, :], start=(dc == 0 and t == 0), stop=(dc == 7)),
                                     reads=[bH32[dc % 2], bMc], writes=[pB[5]], inc=(t == nt - 1))

                        m1_T(0)
                        for dc in range(8):
                            if dc + 1 < 8:
                                m1_T(dc + 1)
                            m1_R(dc)
                        G = nt * 4
                        P.op("act", lambda e: e.activation(out=sg[:, 0:nt, :], in_=psb[5][:, 0:nt * 16].rearrange("p (t e) -> p t e", e=16), func=AF.Sigmoid), reads=[pB[5]], writes=[bRt])
                        P.op("dve", lambda e: e.tensor_tensor(out=sl_[:, 0:nt, :], in0=sg[:, 0:nt, :], in1=rbb[:].unsqueeze(1).to_broadcast([128, nt, 16]), op=ALU.add), reads=[bRt, bMc], writes=[bRt])
                        sv = sl_[:, 0:nt, :].rearrange("p t (g k) -> p (t g) k", k=4)
                        for (opx, dst) in ((ALU.add, gs), (ALU.min, thr)):
                            P.op("dve", lambda e: e.tensor_tensor(out=p6[:, 0:G, 0:2], in0=sv[:, :, 0:2], in1=sv[:, :, 2:4], op=opx), reads=[bRt], writes=[bRt])
                            P.op("dve", lambda e: e.tensor_tensor(out=p6[:, 0:G, 2:5], in0=sv[:, :, 0:3], in1=sv[:, :, 1:4], op=opx), reads=[bRt], writes=[bRt])
                            P.op("dve", lambda e: e.tensor_tensor(out=p6[:, 0:G, 5:6], in0=sv[:, :, 0:1], in1=sv[:, :, 3:4], op=opx), reads=[bRt], writes=[bRt])
                            P.op("dve", lambda e: e.tensor_reduce(out=dst[:, 0:G], in_=p6[:, 0:G, :], axis=AX.X, op=ALU.max), reads=[bRt], writes=[bRt])
                        P.op("dve", lambda e: e.tensor_reduce(out=gmx[:, 0:nt], in_=gs[:, 0:G].rearrange("p (t g) -> p t g", g=4), axis=AX.X, op=ALU.max), reads=[bRt], writes=[bRt])
                        P.op("dve", lambda e: e.tensor_tensor(out=isb[:, 0:G].rearrange("p (t g) -> p t g", g=4), in0=gs[:, 0:G].rearrange("p (t g) -> p t g", g=4),
                                                               in1=gmx[:, 0:nt].unsqueeze(2).to_broadcast([128, nt, 4]), op=ALU.is_ge), reads=[bRt], writes=[bRt])
                        mv = msk[:, 0:nt, :].rearrange("p t (g k) -> p (t g) k", k=4)
                        P.op("dve", lambda e: e.tensor_tensor(out=mv, in0=sv, in1=thr[:, 0:G].unsqueeze(2).to_broadcast([128, G, 4]), op=ALU.is_ge), reads=[bRt], writes=[bRt])
                        P.op("dve", lambda e: e.tensor_tensor(out=mv, in0=mv, in1=isb[:, 0:G].unsqueeze(2).to_broadcast([128, G, 4]), op=ALU.mult), reads=[bRt], writes=[bRt])
                        P.op("dve", lambda e: e.tensor_copy(out=sel_all[:, T0:T0 + nt, :], in_=msk[:, 0:nt, :]), reads=[bRt], writes=[bSel])
                        P.op("dve", lambda e: e.tensor_tensor(out=msk[:, 0:nt, :], in0=msk[:, 0:nt, :], in1=sg[:, 0:nt, :], op=ALU.mult), reads=[bRt], writes=[bRt])
                        P.op("dve", lambda e: e.tensor_reduce(out=den[:, 0:nt], in_=msk[:, 0:nt, :], axis=AX.X, op=ALU.add), reads=[bRt], writes=[bRt])
                        P.op("dve", lambda e: e.reciprocal(out=den[:, 0:nt], in_=den[:, 0:nt]), reads=[bRt], writes=[bRt])
                        P.op("dve", lambda e: e.tensor_tensor(out=g_all[:, T0:T0 + nt, :], in0=msk[:, 0:nt, :], in1=den[:, 0:nt].unsqueeze(2).to_broadcast([128, nt, 16]), op=ALU.mult), reads=[bRt], writes=[bGa])
                    P.barrier()
                with ExitStack() as s2:
                    G_ = lambda name, shape, dt=F32: sbt(s2, name, shape, dt)
                    tri = G_("tri", [128, 128]); k9 = G_("k9", [128, 16, 9]); k32 = G_("k32", [128, 32, 16]); pidx = G_("pidx", [128, 1]); bK = P.buf("sortconst")
                    P.dma("sp", tri[:], I["tri"], writes=[bK])
                    P.dma("sp", k9[:], I["k9"], writes=[bK])
                    P.dma("sp", k32[:], I["k32"], writes=[bK])
                    P.dma("sp", pidx[:], I["pidx"], writes=[bK])
                    cs = G_("cs", [128, 35, 16]); bCs = P.buf("cs")
                    rk = G_("rk", [128, 34, 16]); slot = G_("slot", [128, 34, 16]); s3 = G_("s3", [128, 34, 16]); bSm = P.buf("sortmath")
                    cnt = G_("cnt", [128, 16]); c9 = G_("c9", [128, 16, 9]); nte = G_("nte", [128, 16]); padc = G_("padc", [128, 16]); offend = G_("offend", [128, 16]); offm = G_("offm", [128, 16])
                    c32 = G_("c32", [128, 32, 16]); ek = G_("ek", [128, 32]); sA = G_("sA", [128, 34]); sB = G_("sB", [128, 34]); gsum = G_("gsum", [128, 34])
                    P.op("dve", lambda e: e.memset(cs[:, 0, :], 0.0), writes=[bCs])
                    for t in range(1, 35):
                        P.op("dve", lambda e: e.tensor_tensor(out=cs[:, t, :], in0=cs[:, t - 1, :], in1=sel_all[:, t - 1, :], op=ALU.add), reads=[bSel, bCs], writes=[bCs])
                    fl = lambda ap: ap.rearrange("p t e -> p (t e)")
                    P.op("pe", lambda e: e.matmul(psb[0][:, 0:512], lhsT=tri[:], rhs=fl(sel_all[:, 0:32, :]), start=True, stop=False), reads=[bK, bSel], writes=[pB[0]], inc=False)
                    P.op("pe", lambda e: e.matmul(psb[0][:, 0:512], lhsT=onesf[:], rhs=fl(cs[:, 0:32, :]), start=False, stop=True), reads=[bC, bCs], writes=[pB[0]])
                    P.op("pe", lambda e: e.matmul(psb[1][:, 0:32], lhsT=tri[:], rhs=fl(sel_all[:, 32:34, :]), start=True, stop=False), reads=[bK, bSel], writes=[pB[1]], inc=False)
                    P.op("pe", lambda e: e.matmul(psb[1][:, 0:32], lhsT=onesf[:], rhs=fl(cs[:, 32:34, :]), start=False, stop=True), reads=[bC, bCs], writes=[pB[1]])
                    P.op("pe", lambda e: e.matmul(psb[2][:, 0:16], lhsT=onesf[:], rhs=cs[:, 34, :], start=True, stop=True), reads=[bC, bCs], writes=[pB[2]])
                    P.op("act", lambda e: e.activation(out=fl(rk[:, 0:32, :]), in_=psb[0][:, 0:512], func=AF.Identity), reads=[pB[0]], writes=[bSm])
                    P.op("act", lambda e: e.activation(out=fl(rk[:, 32:34, :]), in_=psb[1][:, 0:32], func=AF.Identity), reads=[pB[1]], writes=[bSm])
                    P.op("act", lambda e: e.activation(out=cnt[:], in_=psb[2][:, 0:16], func=AF.Identity), reads=[pB[2]], writes=[bSm])
                    D_ = lambda fn, rd=(): P.op("dve", fn, reads=[bSm, bK, bSel, bGa] + list(rd), writes=[bSm])
                    D_(lambda e: e.tensor_tensor(out=c9[:], in0=cnt[:].unsqueeze(2).to_broadcast([128, 16, 9]), in1=k9[:], op=ALU.is_gt))
                    D_(lambda e: e.tensor_reduce(out=nte[:], in_=c9[:], axis=AX.X, op=ALU.add))
                    D_(lambda e: e.tensor_scalar(out=padc[:], in0=nte[:], scalar1=512.0, scalar2=None, op0=ALU.mult))
                    D_(lambda e: e.tensor_copy(out=offend[:, 0:1], in_=padc[:, 0:1]))
                    for ex in range(1, 16):
                        D_(lambda e: e.tensor_tensor(out=offend[:, ex:ex + 1], in0=offend[:, ex - 1:ex], in1=padc[:, ex:ex + 1], op=ALU.add))
                    D_(lambda e: e.tensor_tensor(out=offm[:], in0=offend[:], in1=padc[:], op=ALU.subtract))
                    D_(lambda e: e.tensor_tensor(out=s3[:], in0=rk[:], in1=offm[:].unsqueeze(1).to_broadcast([128, 34, 16]), op=ALU.add))
                    D_(lambda e: e.tensor_tensor(out=s3[:], in0=s3[:], in1=sel_all[:], op=ALU.mult))
                    D_(lambda e: e.tensor_reduce(out=sB[:], in_=s3[:], axis=AX.X, op=ALU.max))
                    D_(lambda e: e.tensor_scalar(out=slot[:], in0=sel_all[:], scalar1=-BIG, scalar2=BIG, op0=ALU.mult, op1=ALU.add))
                    D_(lambda e: e.tensor_tensor(out=slot[:], in0=slot[:], in1=s3[:], op=ALU.add))
                    D_(lambda e: e.tensor_reduce(out=sA[:], in_=slot[:], axis=AX.X, op=ALU.min))
                    D_(lambda e: e.tensor_tensor(out=slot[:], in0=slot[:], in1=sA[:].unsqueeze(2).to_broadcast([128, 34, 16]), op=ALU.is_equal))
                    D_(lambda e: e.tensor_tensor(out=slot[:], in0=slot[:], in1=g_all[:], op=ALU.mult))
                    P.op("dve", lambda e: e.tensor_reduce(out=gA[:], in_=slot[:], axis=AX.X, op=ALU.add), reads=[bSm], writes=[bSlots])
                    D_(lambda e: e.tensor_reduce(out=gsum[:], in_=g_all[:], axis=AX.X, op=ALU.add))
                    P.op("dve", lambda e: e.tensor_tensor(out=gB[:], in0=gsum[:], in1=gA[:], op=ALU.subtract), reads=[bSm, bSlots], writes=[bSlots])
                    P.op("dve", lambda e: e.tensor_copy(out=slotA_i[:], in_=sA[:]), reads=[bSm], writes=[bSlots])
                    P.op("dve", lambda e: e.tensor_copy(out=slotB_i[:], in_=sB[:]), reads=[bSm], writes=[bSlots])
                    D_(lambda e: e.tensor_tensor(out=c32[:], in0=k32[:], in1=offend[:].unsqueeze(1).to_broadcast([128, 32, 16]), op=ALU.is_ge))
                    D_(lambda e: e.tensor_reduce(out=ek[:], in_=c32[:], axis=AX.X, op=ALU.add))
                    D_(lambda e: e.tensor_scalar(out=ek[:], in0=ek[:], scalar1=15.0, scalar2=None, op0=ALU.min))
                    D_(lambda e: e.tensor_scalar(out=ek[:], in0=ek[:], scalar1=128.0, scalar2=pidx[:, 0:1], op0=ALU.mult, op1=ALU.add))
                    D_(lambda e: e.tensor_scalar(out=ek[:], in0=ek[:], scalar1=float(l * 2048), scalar2=None, op0=ALU.add))
                    P.op("dve", lambda e: e.tensor_copy(out=widx_i[:], in_=ek[:]), reads=[bSm], writes=[bSlots])
                    for t in range(ntile):
                        for si in (slotA_i, slotB_i):
                            P.idma(tbl[:, :], tokidx[:, t:t + 1], bass.IndirectOffsetOnAxis(ap=si[:, t:t + 1], axis=0), None, NSL - 1, reads=[bSlots, bTok, bTbl], writes=[])
                    if "dbg_sort" in dbg:
                        P.dma("sp", dbg_sort[:, 0:34], sA[:], reads=[bSm])
                        P.dma("sp", dbg_sort[:, 34:68], sB[:], reads=[bSm])
                        P.dma("sp", dbg_sort[:, 68:102], gA[:], reads=[bSlots])
                        P.dma("sp", dbg_sort[:, 102:136], gB[:], reads=[bSlots])
                        P.dma("sp", dbg_sort[:, 136:168], ek[:], reads=[bSm])
                        P.dma("sp", dbg_sort[:, 168:184], cnt[:], reads=[bSm])
                    P.barrier()
                with ExitStack() as s3_:
                    G_ = lambda name, shape, dt=F32: sbt(s3_, name, shape, dt)
                    stg = [G_("stg%d" % i, [128, 4096]) for i in range(3)]; bStg = P.bufs(3, "stg")
                    wgb = [G_("wgb%d" % i, [128, 8, 4, 128], BF16) for i in range(2)]; bWg = P.bufs(2, "wgb"); bWgD = P.bufs(2, "wgbD"); bWgP = P.bufs(2, "wgbP")
                    wub = [G_("wub%d" % i, [128, 8, 4, 128], BF16) for i in range(2)]; bWu = P.bufs(2, "wub"); bWuP = P.bufs(2, "wubP")
                    wdb = [G_("wdb%d" % i, [128, 4, 1024], BF16) for i in range(2)]; bWdA = P.bufs(2, "wdbA"); bWdB = P.bufs(2, "wdbB")
                    idxk = [G_("idxk%d" % i, [128, 4], I32) for i in range(2)]; bIdx = P.bufs(2, "idxk")
                    xg = [G_("xg%d" % i, [128, 4, 1024], BF16) for i in range(2)]; bXg = [P.bufs(4, "xg%d_" % i) for i in range(2)]
                    hT_ = [G_("hTm%d" % i, [128, 8, 512], BF16) for i in range(2)]; bHT = [P.bufs(4, "hTm%d_" % i) for i in range(2)]
                    aT = [G_("aT%d" % i, [128, 4, 512], BF16) for i in range(2)]; bAT = P.bufs(2, "aT")
                    sgu = [G_("sgu%d" % i, [128, 512]) for i in range(2)]; bSg = P.bufs(2, "sgu")
                    yp = [G_("yp%d" % i, [128, 1024]) for i in range(2)]; bYpA = P.bufs(2, "ypA"); bYpB = P.bufs(2, "ypB")
                    for i in range(2):
                        P.op("pool", lambda e: e.memset(xg[i][:], 0.0), writes=bXg[i])
                    wviews = (I["exp_w_gate"].rearrange("l e (p j) f -> (l e p) (j f)", j=8),
                              I["exp_w_up"].rearrange("l e (p j) f -> (l e p) (j f)", j=8),
                              I["exp_w_down"].rearrange("l e (p j) d -> (l e p) (j d)", j=4))

                    def prefetch(k):
                        ks = k % 2
                        P.dma("sp", idxk[ks][:], tbl[k * 512:(k + 1) * 512, :].rearrange("(p j) o -> p (j o)", p=128), reads=[bTbl], writes=[bIdx[ks]])
                        for j in range(4):
                            P.idma(xg[ks][:, j, :], hfD[:, :], None, bass.IndirectOffsetOnAxis(ap=idxk[ks][:, j:j + 1], axis=0), NTOK - 1, reads=[bIdx[ks], bHfD], writes=[bXg[ks][j]])
                        for m in range(3):
                            P.idma(stg[m][:], wviews[m], None, bass.IndirectOffsetOnAxis(ap=widx_i[:, k:k + 1], axis=0), 4095, reads=[bSlots], writes=[bStg[m]])

                    def casts(k):
                        ks = k % 2
                        gv = stg[0][:].rearrange("p (jd m jf) -> p jd m jf", jd=8, jf=4)
                        uv = stg[1][:].rearrange("p (jd m jf) -> p jd m jf", jd=8, jf=4)
                        dv = stg[2][:].rearrange("p (jf d) -> p jf d", jf=4)
                        for jf in (0, 1):
                            P.op("act", lambda e: e.activation(out=wgb[ks][:, :, jf, :], in_=gv[:, :, :, jf], func=AF.Identity), reads=[bStg[0]], writes=[bWg[ks]])
                        P.op("dve", lambda e: e.tensor_copy(out=wgb[ks][:, :, 2, :], in_=gv[:, :, :, 2]), reads=[bStg[0]], writes=[bWgD[ks]])
                        P.op("pool", lambda e: e.tensor_copy(out=wgb[ks][:, :, 3, :], in_=gv[:, :, :, 3]), reads=[bStg[0]], writes=[bWgP[ks]])
                        for jf in (0, 1, 2):
                            P.op("dve", lambda e: e.tensor_copy(out=wub[ks][:, :, jf, :], in_=uv[:, :, :, jf]), reads=[bStg[1]], writes=[bWu[ks]])
                        P.op("pool", lambda e: e.tensor_copy(out=wub[ks][:, :, 3, :], in_=uv[:, :, :, 3]), reads=[bStg[1]], writes=[bWuP[ks]])
                        P.op("act", lambda e: e.activation(out=wdb[ks][:, 0:2, :], in_=dv[:, 0:2, :], func=AF.Identity), reads=[bStg[2]], writes=[bWdA[ks]])
                        P.op("dve", lambda e: e.tensor_copy(out=wdb[ks][:, 2:4, :], in_=dv[:, 2:4, :]), reads=[bStg[2]], writes=[bWdB[ks]])

                    prefetch(0)
                    tcount = 0
                    for k in range(NTL):
                        ks = k % 2
                        for s in range(4):
                            bank = 4 + (tcount % 2)
                            pT = psb[bank][:].bitcast(BF16)
                            xv = xg[ks][:, s, :].rearrange("p (m j) -> p j m", j=8)
                            for jd in range(8):
                                P.op("pe", lambda e: e.transpose(out=pT[:, jd * 128:(jd + 1) * 128], in_=xv[:, jd, :], identity=identb[:]),
                                     reads=[bXg[ks][s], bC], writes=[pB[bank]], inc=(jd == 7))
                            dst = hT_[ks][:, :, s * 128:(s + 1) * 128]
                            src = pT[:, 0:1024].rearrange("p (j m) -> p j m", j=8)
                            if tcount % 2 == 0:
                                P.op("act", lambda e: e.activation(out=dst, in_=src, func=AF.Identity), reads=[pB[bank]], writes=[bHT[ks][s]])
                            else:
                                P.op("dve", lambda e: e.tensor_copy(out=dst, in_=src), reads=[pB[bank]], writes=[bHT[ks][s]])
                            tcount += 1
                        casts(k)
                        if k + 1 < NTL:
                            prefetch(k + 1)
                        for jf in range(4):
                            pg = (jf % 2) * 2
                            for (bank, wt, bw) in ((pg, wgb, [bWg[ks], bWgD[ks], bWgP[ks]]), (pg + 1, wub, [bWu[ks], bWuP[ks]])):
                                for jd in range(8):
                                    P.op("pe", lambda e: e.matmul(psb[bank][:, :], lhsT=wt[ks][:, jd, jf, :], rhs=hT_[ks][:, jd, :], start=(jd == 0), stop=(jd == 7)),
                                         reads=bw + bHT[ks], writes=[pB[bank]], inc=(jd == 7))
                            ssl = jf % 2
                            P.op("act", lambda e: e.activation(out=sgu[ssl][:], in_=psb[pg][:, :], func=AF.Silu), reads=[pB[pg]], writes=[bSg[ssl]])
                            P.op("dve", lambda e: e.tensor_tensor(out=aT[ks][:, jf, :], in0=psb[pg + 1][:, :], in1=sgu[ssl][:], op=ALU.mult), reads=[pB[pg + 1], bSg[ssl]], writes=[bAT[ks]])
                        for s in range(4):
                            ys = s % 2
                            b0 = 6 if s % 2 == 0 else 0
                            for dh in range(2):
                                bank = b0 + dh
                                for jf in range(4):
                                    P.op("pe", lambda e: e.matmul(psb[bank][:, :], lhsT=aT[ks][:, jf, s * 128:(s + 1) * 128], rhs=wdb[ks][:, jf, dh * 512:(dh + 1) * 512], start=(jf == 0), stop=(jf == 3)),
                                         reads=[bAT[ks], bWdA[ks], bWdB[ks]], writes=[pB[bank]], inc=(jf == 3))
                            P.op("act", lambda e: e.activation(out=yp[ys][:, 0:512], in_=psb[b0][:, :], func=AF.Identity), reads=[pB[b0]], writes=[bYpA[ys]])
                            P.op("dve", lambda e: e.tensor_copy(out=yp[ys][:, 512:1024], in_=psb[b0 + 1][:, :]), reads=[pB[b0 + 1]], writes=[bYpB[ys]])
                            P.dma("sp", ypair[k * 512:(k + 1) * 512, :].rearrange("(p j) d -> p j d", j=4)[:, s, :], yp[ys][:], reads=[bYpA[ys], bYpB[ys]], writes=[bYp])
                    P.barrier()
                with ExitStack() as s4_:
                    G_ = lambda name, shape, dt=F32: sbt(s4_, name, shape, dt)
                    ya = [G_("ya%d" % i, [128, 1024]) for i in range(3)]; bYa = P.bufs(3, "ya")
                    yb = [G_("yb%d" % i, [128, 1024]) for i in range(3)]; bYb = P.bufs(3, "yb")
                    mx = [G_("emx%d" % i, [128, 1024]) for i in range(3)]; bMx = P.bufs(3, "emx")
                    ms = [G_("ems%d" % i, [128, 1]) for i in range(3)]; bMs = P.bufs(3, "ems")
                    fng = G_("fng", [128, 1024]); bFn = P.buf("fng")
                    if last:
                        P.dma("sp", fng[:], I["fng_bc"], writes=[bFn])
                    for t in range(ntile):
                        sl = t % 3
                        w = 0 if t < 32 else 1
                        tg = t * 128
                        P.idma(ya[sl][:], ypair[:, :], None, bass.IndirectOffsetOnAxis(ap=slotA_i[:, t:t + 1], axis=0), NSL - 1, reads=[bSlots, bYp], writes=[bYa[sl]])
                        P.idma(yb[sl][:], ypair[:, :], None, bass.IndirectOffsetOnAxis(ap=slotB_i[:, t:t + 1], axis=0), NSL - 1, reads=[bSlots, bYp], writes=[bYb[sl]])
                        P.dma("sp", mx[sl][:], xm[tg:tg + 128, :], reads=[bXm], writes=[bMx[sl]])
                        P.op("dve", lambda e: e.tensor_scalar(out=ya[sl][:], in0=ya[sl][:], scalar1=gA[:, t:t + 1], scalar2=None, op0=ALU.mult), reads=[bYa[sl], bSlots], writes=[bYa[sl]])
                        P.op("dve", lambda e: e.scalar_tensor_tensor(out=ya[sl][:], in0=yb[sl][:], scalar=gB[:, t:t + 1], in1=ya[sl][:], op0=ALU.mult, op1=ALU.add), reads=[bYa[sl], bYb[sl], bSlots], writes=[bYa[sl]])
                        P.op("dve", lambda e: e.tensor_tensor(out=ya[sl][:], in0=ya[sl][:], in1=gbc[:, 2 + w, :], op=ALU.mult), reads=[bYa[sl], bGbc], writes=[bYa[sl]])
                        P.op("dve", lambda e: e.tensor_tensor(out=ya[sl][:], in0=ya[sl][:], in1=mx[sl][:], op=ALU.add), reads=[bYa[sl], bMx[sl]], writes=[bYa[sl]])
                        if last:
                            P.op("act", lambda e: e.activation(out=junk2[:], in_=ya[sl][:], func=AF.Square, accum_out=ms[sl][:]), reads=[bYa[sl]], writes=[bJ2, bMs[sl]])
                            P.op("act", lambda e: e.activation(out=ms[sl][:], in_=ms[sl][:], func=AF.Sqrt, bias=epsT[:, 0:1], scale=1.0 / D), reads=[bMs[sl], bC], writes=[bMs[sl]])
                            P.op("dve", lambda e: e.reciprocal(out=ms[sl][:], in_=ms[sl][:]), reads=[bMs[sl]], writes=[bMs[sl]])
                            P.op("dve", lambda e: e.scalar_tensor_tensor(out=ya[sl][:], in0=ya[sl][:], scalar=ms[sl][:, 0:1], in1=fng[:], op0=ALU.mult, op1=ALU.mult), reads=[bYa[sl], bMs[sl], bFn], writes=[bYa[sl]])
                            P.dma("act", out[tg:tg + 128, :], ya[sl][:], reads=[bYa[sl]], writes=[bOut])
                        else:
                            P.dma("act", xs1[tg:tg + 128, :], ya[sl][:], reads=[bYa[sl]], writes=[bXs1])
                    P.barrier()
                P.barrier()
            if os.environ.get("MK_STOP") == "M":
                break
        P.barrier()
    return nc, list(I.keys())


_PROG = {}


def _dt_of(a):
    if a.dtype == np.int32:
        return I32
    return BF16 if a.dtype == ml_dtypes.bfloat16 else F32


def kernel(**inputs):
    inp = {k: np.asarray(v) for k, v in inputs.items()}
    shared = prep_shared(inp)
    debug = tuple(os.environ.get("MK_DEBUG", "").split(",")) if os.environ.get("MK_DEBUG") else ()
    key = (debug, os.environ.get("MK_STOP"))
    if key not in _PROG:
        shapes = {k: (v.shape, _dt_of(v)) for k, v in shared.items()}
        _PROG[key] = build_program(shapes, debug)
    nc, used = _PROG[key]
    x = np.ascontiguousarray(inp["x"], dtype=np.float32)
    ctx = np.ascontiguousarray(inp["ctx"], dtype=np.float32)
    c = np.asarray(inp["c"], np.float32)
    cc = _cols(inp["c_ctx"])
    in_maps = []
    for b in range(8):
        m = {k: shared[k] for k in used}
        m["x"] = x[b]
        m["ctx"] = ctx[b]
        m["ccols"] = np.ascontiguousarray(np.stack([_cols(c[b]), cc], axis=-1))
        in_maps.append(m)
    res = run_bass_kernel_spmd(nc, in_maps, core_ids=list(range(8)))
    if debug:
        kernel.last = res.results
    return np.stack([np.asarray(r["out"], dtype=np.float32) for r in res.results], axis=0)
```
